# Optimizing a Trainium2 kernel written in Bass

```python
import math
import jax
import jax.numpy as jnp
from jax import lax
import numpy as np

D_MODEL = 1024
BATCH = 16
SEQ = 4096
DEPTH = 1

N_HEADS_A = 8
HEAD_DIM_A = 64
ROPE_DIM_A = HEAD_DIM_A // 4
NOPE_DIM_A = HEAD_DIM_A - ROPE_DIM_A
KV_RANK = 128
IDX_HEADS = 8
IDX_DIM = 32
IDX_ROPE_DIM = IDX_DIM // 4
TOPK_MAX = 256
N_HEADS_B = 8
HEAD_DIM_B = 64
WIDTH_A = N_HEADS_A * HEAD_DIM_A
WIDTH_B = N_HEADS_B * HEAD_DIM_B
MIX_WIDTH = WIDTH_A + WIDTH_B
Q_BLOCK = 128
ROPE_THETA = 500000.0
PEER_HEADS = 8
N_KEYS = 128
N_EXPERTS = N_KEYS * N_KEYS
PEER_KEY_DIM = 128
PEER_TOPK = 16
PEER_CHUNK = 128
EPS = 1e-6
IN_SIZES = (WIDTH_A, KV_RANK, ROPE_DIM_A, IDX_HEADS * IDX_DIM, IDX_DIM, IDX_HEADS,
            WIDTH_B, WIDTH_B, WIDTH_B, N_HEADS_B)
IN_COLS = sum(IN_SIZES)

kernel_name = 'hymba_dsa_fox_peer_block'


def rms_norm(x, g):
    xf = x.astype(jnp.float32)
    y = xf * lax.rsqrt(jnp.mean(xf * xf, axis=-1, keepdims=True) + EPS)
    return (y * g.astype(jnp.float32)).astype(x.dtype)


def rope_tables(pos, rot_dim):
    inv = ROPE_THETA ** (-jnp.arange(0, rot_dim, 2, dtype=jnp.float32) / rot_dim)
    ang = pos.astype(jnp.float32)[..., None] * inv
    return jnp.cos(ang), jnp.sin(ang)


def apply_partial_rope(x, cos, sin):
    half = cos.shape[-1]
    cos = cos.astype(x.dtype)
    sin = sin.astype(x.dtype)
    x1 = x[..., :half]
    x2 = x[..., half:2 * half]
    return jnp.concatenate([x1 * cos - x2 * sin, x2 * cos + x1 * sin, x[..., 2 * half:]], axis=-1)


def to_blocks(a):
    b, s = a.shape[:2]
    return a.reshape(b, s // Q_BLOCK, Q_BLOCK, *a.shape[2:]).swapaxes(0, 1)


def from_blocks(a):
    nb, b, q = a.shape[:3]
    return a.swapaxes(0, 1).reshape(b, nb * q, *a.shape[3:])


def dsa_attention(q, latent, k_rope, iq, ik, iw, cos_a, sin_a, cos_i, sin_i, w_uk, w_uv):
    b, s = q.shape[:2]
    topk = min(TOPK_MAX, s // 4)
    q = apply_partial_rope(q, cos_a[:, :, None], sin_a[:, :, None])
    q_rope, q_nope = q[..., :ROPE_DIM_A], q[..., ROPE_DIM_A:]
    k_rope = apply_partial_rope(k_rope, cos_a, sin_a)
    q_lat = jnp.einsum('bshn,rhn->bshr', q_nope, w_uk)
    iq = apply_partial_rope(iq, cos_i[:, :, None], sin_i[:, :, None])
    ik = apply_partial_rope(ik, cos_i, sin_i)
    key_pos = jnp.arange(s)
    t_blocks = key_pos.reshape(s // Q_BLOCK, Q_BLOCK)
    gather_rows = jax.vmap(lambda table, ids: table[ids])
    scale = HEAD_DIM_A ** -0.5

    def block(args):
        q_lat_b, q_rope_b, iq_b, iw_b, t_b = args
        dots = jnp.einsum('bqhd,bsd->bqhs', iq_b, ik,
                          preferred_element_type=jnp.float32) * (IDX_DIM ** -0.5)
        w_h = iw_b.astype(jnp.float32) * (IDX_HEADS ** -0.5)
        score = jnp.einsum('bqhs,bqh->bqs', jax.nn.relu(dots), w_h)
        causal = key_pos[None, :] <= t_b[:, None]
        score = jnp.where(causal[None], score, -jnp.inf)
        _, sel = lax.top_k(score, topk)
        valid = sel <= t_b[None, :, None]
        lat_s = gather_rows(latent, sel)
        kr_s = gather_rows(k_rope, sel)
        logits = (jnp.einsum('bqhr,bqkr->bqhk', q_lat_b, lat_s, preferred_element_type=jnp.float32)
                  + jnp.einsum('bqhd,bqkd->bqhk', q_rope_b, kr_s, preferred_element_type=jnp.float32)) * scale
        logits = jnp.where(valid[:, :, None, :], logits, -jnp.inf)
        p = jax.nn.softmax(logits, axis=-1).astype(lat_s.dtype)
        return jnp.einsum('bqhk,bqkr->bqhr', p, lat_s)

    o = lax.map(block, (to_blocks(q_lat), to_blocks(q_rope), to_blocks(iq), to_blocks(iw), t_blocks))
    o = from_blocks(o)
    return jnp.einsum('bshr,rhd->bshd', o, w_uv).reshape(b, s, WIDTH_A)


def forgetting_attention(q, k, v, f_logit):
    b, s = q.shape[:2]
    cum = jnp.cumsum(jax.nn.log_sigmoid(f_logit.astype(jnp.float32)), axis=1)
    cum_k = cum.transpose(0, 2, 1)
    key_pos = jnp.arange(s)
    t_blocks = key_pos.reshape(s // Q_BLOCK, Q_BLOCK)
    scale = HEAD_DIM_B ** -0.5

    def block(args):
        q_b, cum_b, t_b = args
        logits = jnp.einsum('bqhd,bshd->bhqs', q_b, k, preferred_element_type=jnp.float32) * scale
        logits = logits + cum_b.transpose(0, 2, 1)[..., None] - cum_k[:, :, None, :]
        causal = key_pos[None, :] <= t_b[:, None]
        logits = jnp.where(causal, logits, -jnp.inf)
        p = jax.nn.softmax(logits, axis=-1).astype(v.dtype)
        return jnp.einsum('bhqs,bshd->bqhd', p, v)

    o = from_blocks(lax.map(block, (to_blocks(q), to_blocks(cum), t_blocks)))
    return o.reshape(b, s, WIDTH_B)


def peer_ffn(h, w_q, keys1, keys2, u, v):
    b, s, d = h.shape
    half = PEER_KEY_DIM // 2
    k1 = keys1.astype(jnp.float32)
    k2 = keys2.astype(jnp.float32)

    def chunk(hc):
        q = (hc @ w_q).reshape(-1, PEER_HEADS, PEER_KEY_DIM).astype(jnp.float32)
        s1 = jnp.einsum('chd,nd->chn', q[..., :half], k1)
        s2 = jnp.einsum('chd,nd->chn', q[..., half:], k2)
        v1, i1 = lax.top_k(s1, PEER_TOPK)
        v2, i2 = lax.top_k(s2, PEER_TOPK)
        cand_s = (v1[..., :, None] + v2[..., None, :]).reshape(*v1.shape[:-1], PEER_TOPK * PEER_TOPK)
        cand_i = (i1[..., :, None] * N_KEYS + i2[..., None, :]).reshape(*i1.shape[:-1], PEER_TOPK * PEER_TOPK)
        best_s, best_pos = lax.top_k(cand_s, PEER_TOPK)
        expert = jnp.take_along_axis(cand_i, best_pos, axis=-1)
        g = jax.nn.softmax(best_s, axis=-1)
        act = jax.nn.gelu(jnp.einsum('cd,chkd->chk', hc, u[expert],
                                     preferred_element_type=jnp.float32), approximate=False)
        wgt = (g * act).astype(hc.dtype)
        return jnp.einsum('chk,chkd->cd', wgt, v[expert])

    out = lax.map(chunk, h.reshape(-1, PEER_CHUNK, d))
    return out.reshape(b, s, d)


def setup_inputs(seed: int = 0) -> dict:
    key = jax.random.key(seed)
    ks = jax.random.split(key, 21)
    f32 = jnp.float32
    L, D = DEPTH, D_MODEL

    def nrm(k, shape, sc):
        return jax.random.normal(k, shape, f32) * sc

    def gain(k, shape):
        return 1.0 + 0.05 * jax.random.normal(k, shape, f32)

    x = nrm(ks[0], (BATCH, SEQ, D), 1.0)
    c = nrm(ks[1], (BATCH, D), 1.0)
    positions = (jnp.arange(SEQ, dtype=jnp.int32)[None, :]
                 + jax.random.randint(ks[2], (BATCH, 1), 0, 1024, dtype=jnp.int32))
    w_ada = nrm(ks[3], (L, D, 6 * D), 0.2 * D ** -0.5)
    b_ada = nrm(ks[4], (L, 6 * D), 0.1)
    g_mix = gain(ks[5], (L, D))
    w_in = nrm(ks[6], (L, D, IN_COLS), D ** -0.5)
    g_kv = gain(ks[7], (L, KV_RANK))
    w_uk = nrm(ks[8], (L, KV_RANK, N_HEADS_A, NOPE_DIM_A), KV_RANK ** -0.5)
    w_uv = nrm(ks[9], (L, KV_RANK, N_HEADS_A, HEAD_DIM_A), KV_RANK ** -0.5)
    b_forget = jax.random.uniform(ks[10], (L, N_HEADS_B), f32, 2.0, 6.0)
    g_out_a = gain(ks[11], (L, WIDTH_A))
    g_out_b = gain(ks[12], (L, WIDTH_B))
    w_out = nrm(ks[13], (L, MIX_WIDTH, D), MIX_WIDTH ** -0.5)
    g_ffn = gain(ks[14], (L, D))
    w_peer_q = nrm(ks[15], (L, D, PEER_HEADS * PEER_KEY_DIM), D ** -0.5)
    peer_keys1 = nrm(ks[16], (L, N_KEYS, PEER_KEY_DIM // 2), (PEER_KEY_DIM // 2) ** -0.5)
    peer_keys2 = nrm(ks[17], (L, N_KEYS, PEER_KEY_DIM // 2), (PEER_KEY_DIM // 2) ** -0.5)
    peer_u = nrm(ks[18], (L, N_EXPERTS, D), D ** -0.5)
    peer_v = nrm(ks[19], (L, N_EXPERTS, D), 0.5)
    g_final = gain(ks[20], (D,))
    return {'x': x, 'c': c, 'positions': positions, 'w_ada': w_ada, 'b_ada': b_ada,
            'g_mix': g_mix, 'w_in': w_in, 'g_kv': g_kv, 'w_uk': w_uk, 'w_uv': w_uv,
            'b_forget': b_forget, 'g_out_a': g_out_a, 'g_out_b': g_out_b, 'w_out': w_out,
            'g_ffn': g_ffn, 'w_peer_q': w_peer_q, 'peer_keys1': peer_keys1,
            'peer_keys2': peer_keys2, 'peer_u': peer_u, 'peer_v': peer_v, 'g_final': g_final}


def reference(x, c, positions, w_ada, b_ada, g_mix, w_in, g_kv, w_uk, w_uv, b_forget,
              g_out_a, g_out_b, w_out, g_ffn, w_peer_q, peer_keys1, peer_keys2, peer_u,
              peer_v, g_final):
    b, s, d = x.shape
    cos_a, sin_a = rope_tables(positions, ROPE_DIM_A)
    cos_i, sin_i = rope_tables(positions, IDX_ROPE_DIM)
    c_act = jax.nn.silu(c)
    split_at = []
    acc = 0
    for n in IN_SIZES[:-1]:
        acc += n
        split_at.append(acc)
    for l in range(DEPTH):
        mod = (c_act @ w_ada[l] + b_ada[l])[:, None, :]
        shift1, scale1, gate1, shift2, scale2, gate2 = jnp.split(mod, 6, axis=-1)
        h = rms_norm(x, g_mix[l]) * (1.0 + scale1) + shift1
        proj = h @ w_in[l]
        q_a, lat, k_r, iq, ik, iw, q_b, k_b, v_b, f_b = jnp.split(proj, split_at, axis=-1)
        o_a = dsa_attention(q_a.reshape(b, s, N_HEADS_A, HEAD_DIM_A), rms_norm(lat, g_kv[l]), k_r,
                            iq.reshape(b, s, IDX_HEADS, IDX_DIM), ik, iw,
                            cos_a, sin_a, cos_i, sin_i, w_uk[l], w_uv[l])
        o_b = forgetting_attention(q_b.reshape(b, s, N_HEADS_B, HEAD_DIM_B),
                                   k_b.reshape(b, s, N_HEADS_B, HEAD_DIM_B),
                                   v_b.reshape(b, s, N_HEADS_B, HEAD_DIM_B),
                                   f_b + b_forget[l])
        mixed = jnp.concatenate([rms_norm(o_a, g_out_a[l]), rms_norm(o_b, g_out_b[l])], axis=-1)
        x = x + gate1 * (mixed @ w_out[l])
        h2 = rms_norm(x, g_ffn[l]) * (1.0 + scale2) + shift2
        x = x + gate2 * peer_ffn(h2, w_peer_q[l], peer_keys1[l], peer_keys2[l], peer_u[l], peer_v[l])
    return rms_norm(x, g_final)
```

```python
import math
import numpy as np
from contextlib import ExitStack
import concourse.bass as bass
import concourse.mybir as mybir
from concourse.bass_utils import run_bass_kernel_spmd

F32 = mybir.dt.float32
BF16 = mybir.dt.bfloat16
I32 = mybir.dt.int32
AF = mybir.ActivationFunctionType
ALU = mybir.AluOpType
AX = mybir.AxisListType

ENGS = ('pe', 'act', 'dve', 'pool', 'sp')
EPOCH = 30000
N_EPOCH = 12
N_DMA = 24

NEG = -30000.0
NEGS = -1.0e30
SEQ = 4096
NT = 32
NB = 2
D = 1024
NCOL = 2496
THETA = 500000.0


class Buf:
    __slots__ = ('w', 'r', 'name')

    def __init__(self, name=''):
        self.w = None
        self.r = []
        self.name = name


class Sched:
    def __init__(self, nc, ctx):
        self.nc = nc
        self.q = {e: [] for e in ENGS}
        self.sems = {}
        for e in ENGS:
            if e == 'sp':
                continue
            for k in range(N_EPOCH):
                self.sems[(e, k)] = ctx.enter_context(nc.semaphore(f"s_{e}{k}"))
        for k in range(N_DMA):
            self.sems[('dma', k)] = ctx.enter_context(nc.semaphore(f"s_dma{k}"))
        self.epoch = {e: 0 for e in ENGS}
        self.cnt = {e: 0 for e in ENGS}
        self.dcnt = [0] * N_DMA
        self.dnext = 0
        self.known = {e: {} for e in ENGS}
        self.nops = 0

    def _need(self, eng, deps):
        waits = []
        kn = self.known[eng]
        best = {}
        for (s, v) in deps:
            if best.get(s, 0) < v:
                best[s] = v
        for s, v in best.items():
            if kn.get(s, 0) < v:
                kn[s] = v
                waits.append((s, v))
        return waits

    @staticmethod
    def _deps(R, W):
        deps = []
        for b in R:
            if b.w is not None:
                deps.append(b.w)
        for b in W:
            if b.w is not None:
                deps.append(b.w)
            deps.extend(b.r)
        return deps

    def op(self, eng, fn, R=(), W=()):
        deps = self._deps(R, W)
        if eng == 'pe':
            deps = [d for d in deps if d[0][0] != 'pe']
        waits = self._need(eng, deps)
        if self.cnt[eng] >= EPOCH:
            self.epoch[eng] += 1
            self.cnt[eng] = 0
        self.cnt[eng] += 1
        tok = ((eng, self.epoch[eng]), self.cnt[eng])
        self.q[eng].append((waits, fn, tok[0], 1))
        for b in R:
            b.r.append(tok)
        for b in W:
            b.w = tok
            b.r = []
        self.nops += 1
        return tok

    def dma(self, q, out, in_, R=(), W=(), **kw):
        slot = self.dnext
        self.dnext = (self.dnext + 1) % N_DMA
        s = ('dma', slot)
        deps = self._deps(R, W)
        if self.dcnt[slot] > 0:
            deps.append((s, self.dcnt[slot]))
        waits = self._need(q, deps)
        self.dcnt[slot] += 16
        tok = (s, self.dcnt[slot])

        def fn(e, out=out, in_=in_, kw=kw):
            return e.dma_start(out=out, in_=in_, **kw)
        self.q[q].append((waits, fn, s, 16))
        for b in R:
            b.r.append(tok)
        for b in W:
            b.w = tok
            b.r = []
        self.nops += 1
        return tok

    def barrier(self):
        toks = []
        for e in ENGS:
            if e == 'sp':
                continue
            if self.cnt[e] > 0:
                toks.append(((e, self.epoch[e]), self.cnt[e]))
        for k in range(N_DMA):
            if self.dcnt[k] > 0:
                toks.append((('dma', k), self.dcnt[k]))
        for e in ENGS:
            waits = self._need(e, toks)
            if waits:
                self.q[e].append((waits, None, None, 0))

    def emit(self):
        nc = self.nc
        sems = self.sems
        qs = self.q

        def replay(e, lst):
            for (waits, fn, s, inc) in lst:
                for (ws, v) in waits:
                    e.wait_ge(sems[ws], v)
                if fn is None:
                    continue
                fn(e).then_inc(sems[s], inc)

        with nc.Block() as block:
            @block.tensor
            def _(e):
                replay(e, qs['pe'])

            @block.scalar
            def _(e):
                replay(e, qs['act'])

            @block.vector
            def _(e):
                replay(e, qs['dve'])

            @block.gpsimd
            def _(e):
                replay(e, qs['pool'])

            @block.sync
            def _(e):
                replay(e, qs['sp'])


class T:
    def __init__(self, t, name=''):
        self.t = t
        self.b = Buf(name)

    def __getitem__(self, k):
        return self.t[k]


def build_nc(stage=99, ntile_dbg=None, nb_dbg=None):
    nc = bass.Bass("TRN2", target_bir_lowering=False)

    def din(name, shape, dt):
        return nc.dram_tensor(name, shape, dt, kind="ExternalInput").ap()

    def dscr(name, shape, dt):
        return nc.dram_tensor(name, shape, dt, kind="Internal").ap()

    x = din("x", [NB * SEQ, D], F32)
    cT = din("cT", [128, 16], F32)
    posT = din("posT", [NB, 128, NT], I32)
    w_ada = din("w_ada", [D, 6 * D], F32)
    b_adaT = din("b_adaT", [128, 48], F32)
    gvec = din("gvec", [128, 24], F32)
    g_final = din("g_final", [1, D], F32)
    g_kv = din("g_kv", [1, 128], F32)
    b_forget = din("b_forget", [1, 8], F32)
    w_in = din("w_in", [D, NCOL], F32)
    w_uk = din("w_uk", [128, 384], F32)
    w_uv = din("w_uv", [128, 512], F32)
    w_out = din("w_out", [D, D], F32)
    w_pq = din("w_pq", [D, D], F32)
    kT = din("kT", [128, 128], F32)
    uT = din("uT", [D, 16384], F32)
    pv = din("pv", [16384, D], F32)
    y = nc.dram_tensor("y", [NB * SEQ, D], F32, kind="ExternalOutput").ap()

    QAT = dscr("QAT", [NB, 128, 4, SEQ], BF16)
    KATs = dscr("KATs", [NB, 128, 4, SEQ], BF16)
    VAs = dscr("VAs", [NB, 128, NT, 512], BF16)
    IQT = dscr("IQT", [NB, 128, 3, SEQ], BF16)
    IKT = dscr("IKT", [NB, 128, SEQ], BF16)
    SGN = dscr("SGN", [NB, 128, NT, 8], F32)
    QBT = dscr("QBT", [NB, 128, 4, SEQ], BF16)
    KBTs = dscr("KBTs", [NB, 128, 4, SEQ], BF16)
    VBs = dscr("VBs", [NB, 128, NT, 512], BF16)
    YA = dscr("YA", [NB * SEQ, D], F32)
    X1 = dscr("X1", [NB * SEQ, D], F32)
    NCUM = dscr("NCUM", [NB, 8, SEQ], F32)
    uTb = dscr("uTb", [D, 16384], BF16)
    pvb = dscr("pvb", [16384, D], BF16)
    DBG = {}
    dbg_stage = stage

    with ExitStack() as ctx:
        S = Sched(nc, ctx)
        counter = [0]

        def sb(ctxx, shape, dt, name=None):
            counter[0] += 1
            nm = f"{name or 't'}_{counter[0]}"
            return T(ctxx.enter_context(nc.sbuf_tensor(nm, shape, dt)), nm)

        def psb(ctxx, shape, dt, name=None):
            counter[0] += 1
            nm = f"{name or 'p'}_{counter[0]}"
            return T(ctxx.enter_context(nc.psum_tensor(nm, shape, dt)), nm)

        def bs(*ts):
            return [t.b for t in ts]

        def ts_(out, in0, s1, s2, op0, op1=None, accum_out=None):
            if op1 is None:
                return lambda e: e.tensor_scalar(out=out, in0=in0, scalar1=s1, scalar2=None, op0=op0)
            if accum_out is not None:
                return lambda e: e.tensor_scalar(out=out, in0=in0, scalar1=s1, scalar2=s2, op0=op0, op1=op1,
                                                 accum_out=accum_out)
            return lambda e: e.tensor_scalar(out=out, in0=in0, scalar1=s1, scalar2=s2, op0=op0, op1=op1)

        def tt_(out, in0, in1, op):
            return lambda e: e.tensor_tensor(out=out, in0=in0, in1=in1, op=op)

        def stt_(out, in0, scalar, in1, op0, op1):
            return lambda e: e.scalar_tensor_tensor(out=out, in0=in0, scalar=scalar, in1=in1, op0=op0, op1=op1)

        def cp_(out, in_):
            return lambda e: e.tensor_copy(out=out, in_=in_)

        def act_(out, in_, func, bias=None, scale=None, accum_out=None):
            kw = {}
            if bias is not None:
                kw['bias'] = bias
            if scale is not None:
                kw['scale'] = scale
            if accum_out is not None:
                kw['accum_out'] = accum_out
            return lambda e: e.activation(out=out, in_=in_, func=func, **kw)

        def max_(out, in_):
            return lambda e: e.max(out=out, in_=in_)

        def mr_(out, repl, vals, imm):
            return lambda e: e.match_replace(out=out, in_to_replace=repl, in_values=vals, imm_value=imm)

        def red_(out, in_, op):
            return lambda e: e.tensor_reduce(out=out, in_=in_, axis=AX.X, op=op)

        def recip_(out, in_):
            return lambda e: e.reciprocal(out=out, in_=in_)

        def memset_(ap, v):
            return lambda e: e.memset(ap, v)

        def mm_(out, lhsT, rhs, start, stop, sgc=False):
            if sgc:
                return lambda e: e.matmul(out, lhsT=lhsT, rhs=rhs, start=start, stop=stop, skip_group_check=True)
            return lambda e: e.matmul(out, lhsT=lhsT, rhs=rhs, start=start, stop=stop)

        def tr_(out, in_, ident):
            return lambda e: e.transpose(out=out, in_=in_, identity=ident)

        G = ctx
        idi = sb(G, [128, 128], I32)
        pidi = sb(G, [128, 1], I32)
        idf = sb(G, [128, 128], F32)
        pidf = sb(G, [128, 1], F32)
        ident_f = sb(G, [128, 128], F32, "ident_f")
        ident_b = sb(G, [128, 128], BF16, "ident_b")
        cmaskL = sb(G, [128, 128], F32, "cmaskL")
        cmaskS = sb(G, [128, 128], F32, "cmaskS")
        tri_f = sb(G, [128, 128], F32, "tri_f")
        ones_f = sb(G, [128, 128], F32, "ones_f")
        invf = sb(G, [128, 24], F32, "invf")
        cact = sb(G, [128, 16], F32, "cact")
        badaT = sb(G, [128, 48], F32, "badaT")
        gv = sb(G, [128, 24], F32, "gv")
        mod = sb(G, [128, 48, 2], F32, "mod")
        mv = [sb(G, [128, 48], F32, f"mv{b}") for b in range(NB)]
        gkvbc = sb(G, [128, 128], F32, "gkvbc")
        bfbc = sb(G, [128, 8], F32, "bfbc")
        lsall = sb(G, [128, NT, 8], F32, "lsall")
        dgt = sb(G, [128, 128], F32, "dgt")
        rt = sb(G, [128, NT, 24], F32, "rt")
        small = sb(G, [128, 64], F32, "small")
        stgf = sb(G, [128, 1024], F32, "stgf")
        castrot = [0]

        def load_cast(dst_ap, dst_t, src_ap, n):
            S.dma('sp', stgf[:, 0:n], src_ap, W=bs(stgf))
            castrot[0] += 1
            if castrot[0] % 2 == 0:
                S.op('act', act_(dst_ap, stgf[:, 0:n], AF.Copy), R=bs(stgf), W=bs(dst_t))
            else:
                S.op('dve', cp_(dst_ap, stgf[:, 0:n]), R=bs(stgf), W=bs(dst_t))

        PS = [psb(G, [128, 512], F32, f"PS{i}") for i in range(8)]

        def psbf(i):
            return PS[i][:].bitcast(BF16)

        S.op('pool', lambda e: e.iota(idi[:], pattern=[[1, 128]], base=0, channel_multiplier=0), W=bs(idi))
        S.op('pool', lambda e: e.iota(pidi[:], pattern=[[0, 1]], base=0, channel_multiplier=1), W=bs(pidi))
        S.op('dve', cp_(idf[:], idi[:]), R=bs(idi), W=bs(idf))
        S.op('dve', cp_(pidf[:], pidi[:]), R=bs(pidi), W=bs(pidf))
        S.op('dve', ts_(ident_f[:], idf[:], pidf[:, 0:1], None, ALU.is_equal), R=bs(idf, pidf), W=bs(ident_f))
        S.op('dve', cp_(ident_b[:], ident_f[:]), R=bs(ident_f), W=bs(ident_b))
        S.op('dve', ts_(cmaskL[:], idf[:], pidf[:, 0:1], NEG, ALU.is_gt, ALU.mult), R=bs(idf, pidf), W=bs(cmaskL))
        S.op('dve', ts_(cmaskS[:], idf[:], pidf[:, 0:1], NEGS, ALU.is_gt, ALU.mult), R=bs(idf, pidf), W=bs(cmaskS))
        S.op('dve', ts_(tri_f[:], idf[:], pidf[:, 0:1], None, ALU.is_ge), R=bs(idf, pidf), W=bs(tri_f))
        S.op('dve', memset_(ones_f[:], 1.0), W=bs(ones_f))
        fr = [THETA ** (-(2.0 * k) / 16.0) for k in range(8)] + [THETA ** (-(2.0 * k) / 8.0) for k in range(4)]
        for rep in range(2):
            for k in range(12):
                S.op('dve', (lambda e, c=rep * 12 + k, v=float(np.float32(fr[k])): e.memset(invf[:, c:c + 1], v)),
                     W=bs(invf))
        S.dma('sp', cact[:], cT, W=bs(cact))
        S.dma('sp', badaT[:], b_adaT, W=bs(badaT))
        S.dma('sp', gv[:], gvec, W=bs(gv))
        S.dma('sp', gkvbc[:], g_kv.partition_broadcast(128), W=bs(gkvbc))
        S.dma('sp', bfbc[:], b_forget.partition_broadcast(128), W=bs(bfbc))
        S.op('act', act_(cact[:], cact[:], AF.Silu), R=bs(cact), W=bs(cact))

        with ExitStack() as P0:
            wada = [sb(P0, [128, 8, D], F32, f"wada{i}") for i in range(2)]
            for g in range(6):
                wt_ = wada[g % 2]
                S.dma('sp', wt_[:], w_ada[:, g * D:(g + 1) * D].rearrange("(k p) n -> p k n", p=128), W=bs(wt_))
                for jc in range(8):
                    j = g * 8 + jc
                    for k in range(8):
                        S.op('pe', mm_(PS[0][:, j * 2:(j + 1) * 2], wt_[:, k, jc * 128:(jc + 1) * 128],
                                       cact[:, k * 2:(k + 1) * 2], k == 0, k == 7), R=bs(wt_, cact), W=bs(PS[0]))
            S.op('dve', tt_(mod[:], PS[0][:, 0:96].rearrange("p (j b) -> p j b", b=2),
                            badaT[:].unsqueeze(2).to_broadcast([128, 48, 2]), ALU.add), R=bs(PS[0], badaT), W=bs(mod))
            for b in range(NB):
                m = mv[b]
                S.op('dve', cp_(m[:, 0:8], mod[:, 0:8, b]), R=bs(mod), W=bs(m))
                S.op('dve', stt_(m[:, 8:16], mod[:, 8:16, b], 1.0, gv[:, 0:8], ALU.add, ALU.mult), R=bs(mod, gv), W=bs(m))
                S.op('dve', cp_(m[:, 16:24], mod[:, 16:24, b]), R=bs(mod), W=bs(m))
                S.op('dve', cp_(m[:, 24:32], mod[:, 24:32, b]), R=bs(mod), W=bs(m))
                S.op('dve', stt_(m[:, 32:40], mod[:, 32:40, b], 1.0, gv[:, 8:16], ALU.add, ALU.mult), R=bs(mod, gv), W=bs(m))
                S.op('dve', cp_(m[:, 40:48], mod[:, 40:48, b]), R=bs(mod), W=bs(m))
        S.barrier()

        if stage >= 4:
            with ExitStack() as P0:
                CW = 8192
                cf = [sb(P0, [128, CW], F32, f"cf{i}") for i in range(2)]
                cb = [sb(P0, [128, CW], BF16, f"cb{i}") for i in range(2)]
                i = 0
                for (src, dst) in ((uT, uTb), (pv, pvb)):
                    sv = src.rearrange("(p a) n -> p (a n)", p=128)
                    dv = dst.rearrange("(p a) n -> p (a n)", p=128)
                    tot = sv.shape[1]
                    for c in range(tot // CW):
                        f_, t_ = cf[i % 2], cb[i % 2]
                        S.dma('sp', f_[:], sv[:, c * CW:(c + 1) * CW], W=bs(f_))
                        if i % 3 == 0:
                            S.op('act', act_(t_[:], f_[:], AF.Copy), R=bs(f_), W=bs(t_))
                        elif i % 3 == 1:
                            S.op('dve', cp_(t_[:], f_[:]), R=bs(f_), W=bs(t_))
                        else:
                            S.op('pool', cp_(t_[:], f_[:]), R=bs(f_), W=bs(t_))
                        S.dma('sp', dv[:, c * CW:(c + 1) * CW], t_[:], R=bs(t_))
                        i += 1
            S.barrier()

        def build_gate_bc(m, src0, dst):
            for k in range(8):
                S.op('dve', ts_(dgt[:], ident_f[:], m[:, src0 + k:src0 + k + 1], None, ALU.mult),
                     R=bs(ident_f, m), W=bs(dgt))
                pb = PS[1 + k // 4]
                S.op('pe', mm_(pb[:, (k % 4) * 128:(k % 4 + 1) * 128], ones_f[:], dgt[:], True, True),
                     R=bs(ones_f, dgt), W=bs(pb))
            S.op('act', act_(dst[:, 0:512], PS[1][:], AF.Copy), R=bs(PS[1]), W=bs(dst))
            S.op('act', act_(dst[:, 512:1024], PS[2][:], AF.Copy), R=bs(PS[2]), W=bs(dst))

        def rms_rstd(ctxx, src_ap, n, rstd_ap, junk_ap, src_b, junk_b, sm_b, eps=1e-6):
            S.op('act', act_(junk_ap, src_ap, AF.Square, accum_out=small[:, 60:61]), R=src_b, W=junk_b + bs(small))
            S.op('dve', ts_(small[:, 61:62], small[:, 60:61], 1.0 / n, eps, ALU.mult, ALU.add), R=bs(small), W=bs(small))
            S.op('act', act_(small[:, 62:63], small[:, 61:62], AF.Sqrt), R=bs(small), W=bs(small))
            S.op('dve', recip_(rstd_ap, small[:, 62:63]), R=bs(small), W=sm_b)

        tiles = list(range(NT)) if ntile_dbg is None else list(range(ntile_dbg))
        for b in range(NB if ntile_dbg is None else (nb_dbg or 1)):
            m = mv[b]
            with ExitStack() as P0:
                posi = sb(P0, [128, NT], I32)
                posf = sb(P0, [128, NT], F32)
                ang = sb(P0, [128, NT, 24], F32)
                kf = sb(P0, [128, NT, 24], F32)
                ki = sb(P0, [128, NT, 24], I32)
                S.dma('sp', posi[:], posT[b], W=bs(posi))
                S.op('dve', cp_(posf[:], posi[:]), R=bs(posi), W=bs(posf))
                S.op('dve', tt_(ang[:], posf[:].unsqueeze(2).to_broadcast([128, NT, 24]),
                                invf[:].unsqueeze(1).to_broadcast([128, NT, 24]), ALU.mult), R=bs(posf, invf), W=bs(ang))
                S.op('dve', ts_(ang[:, :, 12:24], ang[:, :, 12:24], math.pi / 2, None, ALU.add), R=bs(ang), W=bs(ang))
                S.op('dve', ts_(kf[:], ang[:], 1.0 / (2 * math.pi), None, ALU.mult), R=bs(ang), W=bs(kf))
                S.op('dve', cp_(ki[:], kf[:]), R=bs(kf), W=bs(ki))
                S.op('dve', cp_(kf[:], ki[:]), R=bs(ki), W=bs(kf))
                C1 = 6.28125
                C2 = 2 * math.pi - C1
                S.op('dve', stt_(ang[:].rearrange("p a b -> p (a b)"), kf[:].rearrange("p a b -> p (a b)"), -C1,
                                 ang[:].rearrange("p a b -> p (a b)"), ALU.mult, ALU.add), R=bs(kf, ang), W=bs(ang))
                S.op('dve', stt_(ang[:].rearrange("p a b -> p (a b)"), kf[:].rearrange("p a b -> p (a b)"), -C2,
                                 ang[:].rearrange("p a b -> p (a b)"), ALU.mult, ALU.add), R=bs(kf, ang), W=bs(ang))
                S.op('dve', ts_(ang[:], ang[:], -math.pi, math.pi, ALU.max, ALU.min), R=bs(ang), W=bs(ang))
                S.op('act', act_(rt[:], ang[:], AF.Sin), R=bs(ang), W=bs(rt))
            S.barrier()

            with ExitStack() as PP:
                winb = sb(PP, [128, 8, NCOL], BF16, "winb")
                wukb = sb(PP, [128, 384], BF16, "wukb")
                wuvb = sb(PP, [128, 512], BF16, "wuvb")
                for k in range(8):
                    for (c0, c1) in ((0, 1024), (1024, 2048), (2048, NCOL)):
                        load_cast(winb[:, k, c0:c1], winb, w_in[k * 128:(k + 1) * 128, c0:c1], c1 - c0)
                load_cast(wukb[:], wukb, w_uk, 384)
                load_cast(wuvb[:], wuvb, w_uv, 512)
                xt = [sb(PP, [128, D], F32, f"xt{i}") for i in range(2)]
                junk = sb(PP, [128, D], F32, "junk")
                xh = sb(PP, [128, D], F32, "xh")
                hT = sb(PP, [128, 8, 128], BF16, "hT")
                pr = sb(PP, [128, NCOL], F32, "pr")
                qa_b = sb(PP, [128, 512], BF16, "qa_b")
                qb_b = sb(PP, [128, 512], BF16, "qb_b")
                kb_b = sb(PP, [128, 512], BF16, "kb_b")
                vb_b = sb(PP, [128, 512], BF16, "vb_b")
                va_b = sb(PP, [128, 512], BF16, "va_b")
                ka_b = sb(PP, [128, 8, 64], BF16, "ka_b")
                kr_b = sb(PP, [128, 16], BF16, "kr_b")
                iq_f = sb(PP, [128, 8, 32], F32, "iq_f")
                iq_b = sb(PP, [128, 3, 4, 32], BF16, "iq_b")
                S.op('dve', memset_(iq_b[:], 0.0), W=bs(iq_b))
                ik_b = sb(PP, [128, 4, 32], BF16, "ik_b")
                ik_f = sb(PP, [128, 32], F32, "ik_f")
                latn = sb(PP, [128, 128], BF16, "latn")
                latnT = sb(PP, [128, 128], BF16, "latnT")
                rtmp = [sb(PP, [128, 8, 8], F32, f"rtmp{i}") for i in range(4)]
                wabs = sb(PP, [128, 8], F32, "wabs")
                sgn_t = sb(PP, [128, 8], F32, "sgn_t")
                fz = sb(PP, [128, 8], F32, "fz")
                oT = sb(PP, [128, 4, 128], BF16, "oT")
                oT2 = sb(PP, [128, 4, 128], BF16, "oT2")
                oT3 = sb(PP, [128, 4, 128], BF16, "oT3")
                oT4 = sb(PP, [128, 4, 128], BF16, "oT4")
                ncs = sb(PP, [8, 512], F32, "ncs")

                def rope(dst3, src3, H, half, sinap, cosap, Rb, Wb):
                    cb_ = cosap.unsqueeze(1).to_broadcast([128, H, half])
                    sb_ = sinap.unsqueeze(1).to_broadcast([128, H, half])
                    x1 = src3[:, :, 0:half]
                    x2 = src3[:, :, half:2 * half]
                    t = [r[:, 0:H, 0:half] for r in rtmp]
                    S.op('dve', tt_(t[0], x1, cb_, ALU.mult), R=Rb + bs(rt), W=bs(rtmp[0]))
                    S.op('dve', tt_(t[1], x2, sb_, ALU.mult), R=Rb + bs(rt), W=bs(rtmp[1]))
                    S.op('dve', tt_(t[2], x2, cb_, ALU.mult), R=Rb + bs(rt), W=bs(rtmp[2]))
                    S.op('dve', tt_(t[3], x1, sb_, ALU.mult), R=Rb + bs(rt), W=bs(rtmp[3]))
                    S.op('dve', tt_(dst3[:, :, 0:half], t[0], t[1], ALU.subtract), R=bs(rtmp[0], rtmp[1]), W=Wb)
                    S.op('dve', tt_(dst3[:, :, half:2 * half], t[2], t[3], ALU.add), R=bs(rtmp[2], rtmp[3]), W=Wb)

                for tt in tiles:
                    r0 = b * SEQ + tt * 128
                    X = xt[tt % 2]
                    S.dma('sp', X[:], x[r0:r0 + 128, :], W=bs(X))
                    rms_rstd(PP, X[:], D, small[:, 0:1], junk[:], bs(X), bs(junk), bs(small))
                    S.op('dve', ts_(xh[:], X[:], small[:, 0:1], None, ALU.mult), R=bs(X, small), W=bs(xh))
                    for k in range(8):
                        pb = PS[k // 4]
                        S.op('pe', tr_(pb[:, (k % 4) * 128:(k % 4 + 1) * 128], xh[:, k * 128:(k + 1) * 128], ident_f[:]),
                             R=bs(xh, ident_f), W=bs(pb))
                    for k in range(8):
                        pb = PS[k // 4]
                        src = pb[:, (k % 4) * 128:(k % 4 + 1) * 128]
                        if k % 2 == 0:
                            S.op('dve', ts_(hT[:, k, :], src, m[:, 8 + k:9 + k], m[:, k:k + 1], ALU.mult, ALU.add),
                                 R=bs(pb, m), W=bs(hT))
                        else:
                            S.op('act', act_(hT[:, k, :], src, AF.Identity, bias=m[:, k:k + 1], scale=m[:, 8 + k:9 + k]),
                                 R=bs(pb, m), W=bs(hT))
                    cols = [(0, 512), (512, 1024), (1024, 1536), (1536, 2048), (2048, NCOL)]
                    for ci, (c0, c1) in enumerate(cols):
                        pb = PS[2 + ci]
                        for k in range(8):
                            S.op('pe', mm_(pb[:, 0:c1 - c0], hT[:, k, :], winb[:, k, c0:c1], k == 0, k == 7),
                                 R=bs(hT, winb), W=bs(pb))
                        if ci % 2 == 0:
                            S.op('act', act_(pr[:, c0:c1], pb[:, 0:c1 - c0], AF.Copy), R=bs(pb), W=bs(pr))
                        else:
                            S.op('dve', cp_(pr[:, c0:c1], pb[:, 0:c1 - c0]), R=bs(pb), W=bs(pr))
                    sinA, sinI = rt[:, tt, 0:8], rt[:, tt, 8:12]
                    cosA, cosI = rt[:, tt, 12:20], rt[:, tt, 20:24]
                    S.op('act', act_(qa_b[:], pr[:, 0:512], AF.Copy), R=bs(pr), W=bs(qa_b))
                    rope(qa_b[:].rearrange("p (h d) -> p h d", h=8), pr[:, 0:512].rearrange("p (h d) -> p h d", h=8),
                         8, 8, sinA, cosA, bs(pr), bs(qa_b))
                    rope(kr_b[:].unsqueeze(1), pr[:, 640:656].unsqueeze(1), 1, 8, sinA, cosA, bs(pr), bs(kr_b))
                    rms_rstd(PP, pr[:, 512:640], 128, small[:, 1:2], junk[:, 0:128], bs(pr), bs(junk), bs(small))
                    S.op('dve', stt_(latn[:], pr[:, 512:640], small[:, 1:2], gkvbc[:], ALU.mult, ALU.mult),
                         R=bs(pr, small, gkvbc), W=bs(latn))
                    S.op('pe', tr_(psbf(7)[:, 0:128], latn[:], ident_b[:]), R=bs(latn, ident_b), W=bs(PS[7]))
                    S.op('act', act_(latnT[:], psbf(7)[:, 0:128], AF.Copy), R=bs(PS[7]), W=bs(latnT))
                    S.op('pe', mm_(PS[0][:, 0:384], latnT[:], wukb[:], True, True), R=bs(latnT, wukb), W=bs(PS[0]))
                    S.op('pe', mm_(PS[1][:, 0:512], latnT[:], wuvb[:], True, True), R=bs(latnT, wuvb), W=bs(PS[1]))
                    S.op('act', act_(ka_b[:, :, 16:64], PS[0][:, 0:384].rearrange("p (h d) -> p h d", h=8), AF.Copy),
                         R=bs(PS[0]), W=bs(ka_b))
                    S.op('dve', cp_(ka_b[:, :, 0:16], kr_b[:].unsqueeze(1).to_broadcast([128, 8, 16])),
                         R=bs(kr_b), W=bs(ka_b))
                    S.op('dve', cp_(va_b[:], PS[1][:, 0:512]), R=bs(PS[1]), W=bs(va_b))
                    S.dma('sp', VAs[b, :, tt, :], va_b[:], R=bs(va_b))
                    S.op('act', act_(wabs[:], pr[:, 944:952], AF.Abs, scale=(8.0 ** -0.5) * (32.0 ** -0.5)),
                         R=bs(pr), W=bs(wabs))
                    S.op('dve', ts_(sgn_t[:], pr[:, 944:952], 0.0, 2.0, ALU.is_ge, ALU.mult), R=bs(pr), W=bs(sgn_t))
                    S.op('dve', ts_(sgn_t[:], sgn_t[:], -1.0, None, ALU.add), R=bs(sgn_t), W=bs(sgn_t))
                    S.dma('sp', SGN[b, :, tt, :], sgn_t[:], R=bs(sgn_t))
                    S.op('dve', cp_(iq_f[:], pr[:, 656:912].rearrange("p (h d) -> p h d", h=8)), R=bs(pr), W=bs(iq_f))
                    rope(iq_f[:], pr[:, 656:912].rearrange("p (h d) -> p h d", h=8), 8, 4, sinI, cosI, bs(pr), bs(iq_f))
                    for k3 in range(3):
                        nh = 3 if k3 < 2 else 2
                        S.op('dve', tt_(iq_b[:, k3, 0:nh, :], iq_f[:, 3 * k3:3 * k3 + nh, :],
                                        wabs[:, 3 * k3:3 * k3 + nh].unsqueeze(2).to_broadcast([128, nh, 32]), ALU.mult),
                             R=bs(iq_f, wabs), W=bs(iq_b))
                    S.op('dve', cp_(ik_f[:], pr[:, 912:944]), R=bs(pr), W=bs(ik_f))
                    rope(ik_f[:].unsqueeze(1), pr[:, 912:944].unsqueeze(1), 1, 4, sinI, cosI, bs(pr), bs(ik_f))
                    S.op('dve', cp_(ik_b[:], ik_f[:].unsqueeze(1).to_broadcast([128, 4, 32])), R=bs(ik_f), W=bs(ik_b))
                    S.op('act', act_(qb_b[:], pr[:, 952:1464], AF.Copy), R=bs(pr), W=bs(qb_b))
                    S.op('dve', cp_(kb_b[:], pr[:, 1464:1976]), R=bs(pr), W=bs(kb_b))
                    S.op('act', act_(vb_b[:], pr[:, 1976:2488], AF.Copy), R=bs(pr), W=bs(vb_b))
                    S.dma('sp', VBs[b, :, tt, :], vb_b[:], R=bs(vb_b))
                    S.op('dve', tt_(fz[:], pr[:, 2488:2496], bfbc[:], ALU.add), R=bs(pr, bfbc), W=bs(fz))
                    S.op('act', act_(fz[:], fz[:], AF.Exp, scale=-1.0), R=bs(fz), W=bs(fz))
                    S.op('act', act_(lsall[:, tt, :], fz[:], AF.Ln, bias=1.0), R=bs(fz), W=bs(lsall))
                    jobs = [(qa_b, 4, QAT[b], oT, 0), (ka_b, 4, KATs[b], oT2, 1), (qb_b, 4, QBT[b], oT3, 2),
                            (kb_b, 4, KBTs[b], oT4, 3)]
                    for (src, nblk, dst, stg, pbi) in jobs:
                        sv = src[:] if len(src.t.shape) == 2 else src[:].rearrange("p h d -> p (h d)")
                        pbb = psbf(pbi)
                        for k in range(nblk):
                            S.op('pe', tr_(pbb[:, k * 128:(k + 1) * 128], sv[:, k * 128:(k + 1) * 128], ident_b[:]),
                                 R=bs(src, ident_b), W=bs(PS[pbi]))
                        eng = 'act' if pbi % 2 == 0 else 'dve'
                        if eng == 'act':
                            S.op('act', act_(stg[:].rearrange("p a t -> p (a t)"), pbb[:, 0:512], AF.Copy), R=bs(PS[pbi]), W=bs(stg))
                        else:
                            S.op('dve', cp_(stg[:].rearrange("p a t -> p (a t)"), pbb[:, 0:512]), R=bs(PS[pbi]), W=bs(stg))
                        S.dma('sp', dst[:, :, tt * 128:(tt + 1) * 128], stg[:], R=bs(stg))
                    pbb = psbf(4)
                    for k in range(3):
                        S.op('pe', tr_(pbb[:, k * 128:(k + 1) * 128], iq_b[:, k, :, :].rearrange("p a d -> p (a d)"), ident_b[:]),
                             R=bs(iq_b, ident_b), W=bs(PS[4]))
                    S.op('pe', tr_(pbb[:, 384:512], ik_b[:].rearrange("p a d -> p (a d)"), ident_b[:]),
                         R=bs(ik_b, ident_b), W=bs(PS[4]))
                    S.op('act', act_(oT[:].rearrange("p a t -> p (a t)"), pbb[:, 0:512], AF.Copy), R=bs(PS[4]), W=bs(oT))
                    S.dma('sp', IQT[b, :, :, tt * 128:(tt + 1) * 128], oT[:, 0:3, :], R=bs(oT))
                    S.dma('sp', IKT[b, :, tt * 128:(tt + 1) * 128], oT[:, 3, :], R=bs(oT))
                ntl = len(tiles)
                for T0 in range(0, ntl, 4):
                    pb = PS[5 + (T0 // 4) % 2]
                    for Tt in range(T0, min(T0 + 4, ntl)):
                        oc = (Tt - T0) * 128
                        for J in range(Tt + 1):
                            S.op('pe', mm_(pb[0:8, oc:oc + 128], lsall[:, J, :], (tri_f if J == Tt else ones_f)[:],
                                           J == 0, J == Tt), R=bs(lsall, tri_f, ones_f), W=bs(pb))
                    n = (min(T0 + 4, ntl) - T0) * 128
                    S.op('act', act_(ncs[0:8, 0:n], pb[0:8, 0:n], AF.Copy, scale=8.0), R=bs(pb), W=bs(ncs))
                    S.dma('sp', NCUM[b, :, T0 * 128:T0 * 128 + n], ncs[0:8, 0:n], R=bs(ncs))
            S.barrier()
            if dbg_stage == 1:
                continue

            for grp in (0, 1):
                with ExitStack() as PA:
                    KT = sb(PA, [128, 4, SEQ], BF16, "KT")
                    VV = sb(PA, [128, NT, 512], BF16, "VV")
                    L_all = len(tiles) * 128
                    for p4 in range(4):
                        S.dma('sp', KT[:, p4, 0:L_all], (KATs if grp == 0 else KBTs)[b, :, p4, 0:L_all], W=bs(KT))
                    S.dma('sp', VV[:, 0:len(tiles), :], (VAs if grp == 0 else VBs)[b, :, 0:len(tiles), :], W=bs(VV))
                    woutb = sb(PA, [128, 4, D], BF16, "woutb")
                    for k in range(4):
                        load_cast(woutb[:, k, :], woutb, w_out[grp * 512 + k * 128:grp * 512 + (k + 1) * 128, :], D)
                    QT = [sb(PA, [128, 4, 128], BF16, f"QT{i}") for i in range(2)]
                    lgs = [sb(PA, [128, SEQ], F32, f"lg{i}") for i in range(2)]
                    PT = [sb(PA, [128, 1024], BF16, f"PT{i}") for i in range(2)]
                    oall = sb(PA, [128, 512], F32, "oall")
                    onb = sb(PA, [128, 512], BF16, "onb")
                    mixT = sb(PA, [128, 4, 128], BF16, "mixT")
                    junk2 = sb(PA, [128, 512], F32, "junk2")
                    yt = sb(PA, [128, D], F32, "yt")
                    stat = sb(PA, [128, 32], F32, "stat")
                    if grp == 0:
                        IK = sb(PA, [128, SEQ], BF16, "IK")
                        S.dma('sp', IK[:, 0:L_all], IKT[b, :, 0:L_all], W=bs(IK))
                        IQ = [sb(PA, [128, 3, 128], BF16, f"IQ{i}") for i in range(2)]
                        sg = [sb(PA, [128, 8], F32, f"sg{i}") for i in range(2)]
                        sc = sb(PA, [128, SEQ], F32, "sc")
                        wk = lgs[1]
                        Pm1 = sb(PA, [128, SEQ], BF16, "Pm1")
                        Pms = [(sc[:].bitcast(BF16), sc), (Pm1[:], Pm1)]
                        mb = sb(PA, [128, SEQ], BF16, "mb")
                        rl = [sb(PA, [128, 512], F32, f"rl{i}") for i in range(2)]
                        m8 = sb(PA, [128, 8], F32, "m8")
                        tau = sb(PA, [128, 1], F32, "tau")
                    else:
                        Pm0 = sb(PA, [128, SEQ], BF16, "Pm0")
                        Pm1 = sb(PA, [128, SEQ], BF16, "Pm1")
                        Pms = [(Pm0[:], Pm0), (Pm1[:], Pm1)]
                        fbts = [sb(PA, [128, SEQ], F32, f"fbt{i}") for i in range(2)]
                        xres = sb(PA, [128, D], F32, "xres")
                        yat = sb(PA, [128, D], F32, "yat")
                        g1bc = sb(PA, [128, D], F32, "g1bc")
                        build_gate_bc(m, 16, g1bc)
                    rotA = [0]
                    rotB = [0]
                    rotT = [0]
                    statb = [Buf() for _ in range(8)]

                    def nextA():
                        rotA[0] = (rotA[0] + 1) % 3
                        return 1 + rotA[0]

                    def nextB():
                        rotB[0] = (rotB[0] + 1) % 4
                        return 4 + rotB[0]

                    for qt in tiles:
                        r0 = b * SEQ + qt * 128
                        L = (qt + 1) * 128
                        nch = (L + 511) // 512
                        Q = QT[qt % 2]
                        S.dma('sp', Q[:], (QAT if grp == 0 else QBT)[b, :, :, qt * 128:(qt + 1) * 128], W=bs(Q))
                        if grp == 0:
                            iq = IQ[qt % 2]
                            sgt = sg[qt % 2]
                            S.dma('sp', iq[:], IQT[b, :, :, qt * 128:(qt + 1) * 128], W=bs(iq))
                            S.dma('sp', sgt[:], SGN[b, :, qt, :], W=bs(sgt))
                            for h in range(8):
                                pr0 = 32 * (h % 3)
                                for c in range(nch):
                                    c0, c1 = c * 512, min(L, (c + 1) * 512)
                                    pb = PS[nextB()]
                                    S.op('pe', mm_(pb[:, 0:c1 - c0], iq[pr0:pr0 + 32, h // 3, :], IK[pr0:pr0 + 32, c0:c1],
                                                   True, True), R=bs(iq, IK), W=bs(pb))
                                    r_ = rl[(h + c) % 2]
                                    S.op('act', act_(r_[:, 0:c1 - c0], pb[:, 0:c1 - c0], AF.Relu), R=bs(pb), W=bs(r_))
                                    if h == 0:
                                        S.op('dve', ts_(sc[:, c0:c1], r_[:, 0:c1 - c0], sgt[:, 0:1], None, ALU.mult),
                                             R=bs(r_, sgt), W=bs(sc))
                                    else:
                                        S.op('dve', stt_(sc[:, c0:c1], r_[:, 0:c1 - c0], sgt[:, h:h + 1], sc[:, c0:c1],
                                                         ALU.mult, ALU.add), R=bs(r_, sgt, sc), W=bs(sc))
                            S.op('dve', tt_(sc[:, L - 128:L], sc[:, L - 128:L], cmaskS[:], ALU.add), R=bs(sc, cmaskS), W=bs(sc))
                            if qt >= 2:
                                for r in range(32):
                                    src_ = sc if r == 0 else wk
                                    S.op('dve', max_(m8[:], src_[:, 0:L]), R=bs(src_), W=bs(m8))
                                    if r < 31:
                                        S.op('dve', mr_(wk[:, 0:L], m8[:], src_[:, 0:L], NEGS), R=bs(m8, src_), W=bs(wk))
                                S.op('dve', ts_(tau[:], m8[:, 7:8], -1.0e29, None, ALU.max), R=bs(m8), W=bs(tau))
                            else:
                                S.op('dve', memset_(tau[:], -1.0e29), W=bs(tau))
                            S.op('dve', ts_(mb[:, 0:L], sc[:, 0:L], tau[:, 0:1], NEG, ALU.is_lt, ALU.mult), R=bs(sc, tau), W=bs(mb))
                        def head_S1(h):
                            p4, hf = h // 2, (h % 2) * 64
                            lg = lgs[h % 2]
                            if grp == 1:
                                fbt = fbts[h % 2]
                                S.dma('sp', fbt[:, 0:L], NCUM[b, h:h + 1, 0:L].partition_broadcast(128), W=bs(fbt))
                                bias_t = fbt
                            else:
                                bias_t = mb
                            for c in range(nch):
                                c0, c1 = c * 512, min(L, (c + 1) * 512)
                                pb = PS[nextB()]
                                S.op('pe', mm_(pb[:, 0:c1 - c0], Q[hf:hf + 64, p4, :], KT[hf:hf + 64, p4, c0:c1], True, True),
                                     R=bs(Q, KT), W=bs(pb))
                                S.op('dve', tt_(lg[:, c0:c1], pb[:, 0:c1 - c0], bias_t[:, c0:c1], ALU.add),
                                     R=bs(pb, bias_t), W=bs(lg))
                            if grp == 1:
                                S.op('pool', tt_(lg[:, L - 128:L], lg[:, L - 128:L], cmaskL[:], ALU.add), R=bs(lg, cmaskL), W=bs(lg))
                            S.op('dve', red_(stat[:, 16 + h:17 + h], lg[:, 0:L], ALU.max), R=bs(lg), W=[statb[h]])
                            S.op('dve', ts_(stat[:, 24 + h:25 + h], stat[:, 16 + h:17 + h], -0.125, None, ALU.mult), R=[statb[h]], W=[statb[h]])

                        def head_S2(h):
                            lg = lgs[h % 2]
                            Pm_ap, Pm_b = Pms[h % 2]
                            S.op('act', act_(Pm_ap[:, 0:L], lg[:, 0:L], AF.Exp, bias=stat[:, 24 + h:25 + h], scale=0.125,
                                             accum_out=stat[:, h:h + 1]), R=bs(lg) + [statb[h]], W=bs(Pm_b) + [statb[h]])
                            nkb = qt + 1
                            for g0 in range(0, nkb, 8):
                                g1 = min(nkb, g0 + 8)
                                pi = nextA()
                                pbb = psbf(pi)
                                for kb in range(g0, g1):
                                    S.op('pe', tr_(pbb[:, (kb - g0) * 128:(kb - g0 + 1) * 128], Pm_ap[:, kb * 128:(kb + 1) * 128], ident_b[:]),
                                         R=bs(Pm_b, ident_b), W=bs(PS[pi]))
                                rotT[0] += 1
                                ptt = PT[rotT[0] % 2]
                                n = (g1 - g0) * 128
                                if rotT[0] % 2 == 0:
                                    S.op('act', act_(ptt[:, 0:n], pbb[:, 0:n], AF.Copy), R=bs(PS[pi]), W=bs(ptt))
                                else:
                                    S.op('pool' if False else 'dve', cp_(ptt[:, 0:n], pbb[:, 0:n]), R=bs(PS[pi]), W=bs(ptt))
                                for kb in range(g0, g1):
                                    S.op('pe', mm_(PS[0][:, h * 64:(h + 1) * 64], ptt[:, (kb - g0) * 128:(kb - g0 + 1) * 128],
                                                   VV[:, kb, h * 64:(h + 1) * 64], kb == 0, kb == nkb - 1),
                                         R=bs(ptt, VV), W=bs(PS[0]))

                        head_S1(0)
                        for h in range(8):
                            if h + 1 < 8:
                                head_S1(h + 1)
                            head_S2(h)
                        S.op('dve', recip_(stat[:, 8:16], stat[:, 0:8]), R=statb, W=bs(stat))
                        S.op('dve', tt_(oall[:].rearrange("p (h d) -> p h d", h=8), PS[0][:].rearrange("p (h d) -> p h d", h=8),
                                        stat[:, 8:16].unsqueeze(2).to_broadcast([128, 8, 64]), ALU.mult), R=bs(PS[0], stat), W=bs(oall))
                        rms_rstd(PA, oall[:], 512, small[:, 2:3], junk2[:], bs(oall), bs(junk2), bs(small))
                        S.op('dve', ts_(onb[:], oall[:], small[:, 2:3], None, ALU.mult), R=bs(oall, small), W=bs(onb))
                        pbb = psbf(1)
                        for k in range(4):
                            S.op('pe', tr_(pbb[:, k * 128:(k + 1) * 128], onb[:, k * 128:(k + 1) * 128], ident_b[:]),
                                 R=bs(onb, ident_b), W=bs(PS[1]))
                        for k in range(4):
                            S.op('dve', ts_(mixT[:, k, :], pbb[:, k * 128:(k + 1) * 128], gv[:, 16 + grp * 4 + k:17 + grp * 4 + k], None, ALU.mult),
                                 R=bs(PS[1], gv), W=bs(mixT))
                        for dh in range(2):
                            for k in range(4):
                                S.op('pe', mm_(PS[2 + dh][:], mixT[:, k, :], woutb[:, k, dh * 512:(dh + 1) * 512], k == 0, k == 3),
                                     R=bs(mixT, woutb), W=bs(PS[2 + dh]))
                        if grp == 0:
                            S.op('act', act_(yt[:, 0:512], PS[2][:], AF.Copy), R=bs(PS[2]), W=bs(yt))
                            S.op('dve', cp_(yt[:, 512:1024], PS[3][:]), R=bs(PS[3]), W=bs(yt))
                            S.dma('sp', YA[r0:r0 + 128, :], yt[:], R=bs(yt))
                        else:
                            S.dma('sp', yat[:], YA[r0:r0 + 128, :], W=bs(yat))
                            S.dma('sp', xres[:], x[r0:r0 + 128, :], W=bs(xres))
                            for dh in range(2):
                                sl = slice(dh * 512, (dh + 1) * 512)
                                S.op('dve', tt_(yt[:, sl], PS[2 + dh][:], yat[:, sl], ALU.add), R=bs(PS[2 + dh], yat), W=bs(yt))
                            S.op('pool', tt_(yt[:], yt[:], g1bc[:], ALU.mult), R=bs(yt, g1bc), W=bs(yt))
                            S.op('pool', tt_(yt[:], yt[:], xres[:], ALU.add), R=bs(yt, xres), W=bs(yt))
                            S.dma('sp', X1[r0:r0 + 128, :], yt[:], R=bs(yt))
                S.barrier()
            if dbg_stage == 3:
                nrow = len(tiles) * 128
                S.dma('sp', y[b * SEQ:b * SEQ + nrow, :], X1[b * SEQ:b * SEQ + nrow, :])
                S.barrier()
            if dbg_stage <= 4:
                continue

            with ExitStack() as PE_:
                TG = 1
                TT = TG * 128
                NI = 4
                EB = NI * 128
                nblk = 16384 // EB
                wpqb = sb(PE_, [128, 8, D], BF16, "wpqb")
                for k in range(8):
                    load_cast(wpqb[:, k, :], wpqb, w_pq[k * 128:(k + 1) * 128, :], D)
                kTb = sb(PE_, [128, 128], BF16, "kTb")
                load_cast(kTb[:], kTb, kT, 128)
                g2bc = sb(PE_, [128, D], F32, "g2bc")
                gfbc = sb(PE_, [128, D], F32, "gfbc")
                build_gate_bc(m, 40, g2bc)
                S.dma('sp', gfbc[:], g_final.partition_broadcast(128), W=bs(gfbc))
                x1t = [sb(PE_, [128, D], F32, f"x1t{i}") for i in range(TG)]
                xh = sb(PE_, [128, D], F32, "xhE")
                junk = xh
                h2T = sb(PE_, [128, 8, TT], BF16, "h2T")
                pqT = sb(PE_, [128, 8, 128], BF16, "pqT")
                s1 = [sb(PE_, [128, 8, 128], F32, f"s1_{i}") for i in range(TG)]
                s2 = [sb(PE_, [128, 8, 128], F32, f"s2_{i}") for i in range(TG)]
                w1 = sb(PE_, [128, 8, 128], F32, "w1")
                v1 = sb(PE_, [128, 8, 16], F32, "v1")
                v2 = sb(PE_, [128, 8, 16], F32, "v2")
                cand = sb(PE_, [128, 8, 256], F32, "cand")
                best = sb(PE_, [128, 8, 16], F32, "best")
                et = sb(PE_, [128, 8, 16], F32, "et")
                zs = [sb(PE_, [128, 32], F32, f"zs{i}") for i in range(TG)]
                Sg = [sb(PE_, [128, NI, 128], F32, f"Sg{i}") for i in range(4)]
                Ee = [sb(PE_, [128, EB], F32, f"Ee{i}") for i in range(2)]
                Gh2 = [[sb(PE_, [128, 8, EB], BF16, f"Gh{i}_{j}") for i in range(TG)] for j in range(2)]
                gAT = [sb(PE_, [128, NI * 128], BF16, f"gAT{i}") for i in range(2)]
                WTt = [sb(PE_, [128, 128], BF16, f"WT{i}") for i in range(4)]
                uch = [sb(PE_, [128, 8, EB], BF16, f"uch{i}") for i in range(2)]
                vch = [sb(PE_, [128, NI, D], BF16, f"vch{i}") for i in range(2)]
                po = sb(PE_, [128, D], F32, "po")
                rot = {'sg': 0, 'ee': 0, 'ga': 0, 'wt': 0, 'pa': 0, 'pg': 0, 'w': 0}
                PAb = [Buf(), Buf()]
                PGb = [Buf() for _ in range(4)]
                for g0 in range(0, len(tiles), TG):
                    grp_tiles = tiles[g0:g0 + TG]
                    for ti, tt in enumerate(grp_tiles):
                        r0 = b * SEQ + tt * 128
                        X = x1t[ti]
                        S.dma('sp', X[:], X1[r0:r0 + 128, :], W=bs(X))
                        rms_rstd(PE_, X[:], D, small[:, 3:4], junk[:], bs(X), bs(junk), bs(small))
                        S.op('dve', ts_(xh[:], X[:], small[:, 3:4], None, ALU.mult), R=bs(X, small), W=bs(xh))
                        for k in range(8):
                            pb = PS[k // 4]
                            S.op('pe', tr_(pb[:, (k % 4) * 128:(k % 4 + 1) * 128], xh[:, k * 128:(k + 1) * 128], ident_f[:]),
                                 R=bs(xh, ident_f), W=bs(pb))
                        for k in range(8):
                            pb = PS[k // 4]
                            src = pb[:, (k % 4) * 128:(k % 4 + 1) * 128]
                            S.op('act', act_(h2T[:, k, ti * 128:(ti + 1) * 128], src, AF.Identity, bias=m[:, 24 + k:25 + k],
                                             scale=m[:, 32 + k:33 + k]), R=bs(pb, m), W=bs(h2T))
                        for h in range(8):
                            pb = PS[2 + h // 4]
                            for k in range(8):
                                S.op('pe', mm_(pb[:, (h % 4) * 128:(h % 4 + 1) * 128], wpqb[:, k, h * 128:(h + 1) * 128],
                                               h2T[:, k, ti * 128:(ti + 1) * 128], k == 0, k == 7), R=bs(wpqb, h2T), W=bs(pb))
                        S.op('act', act_(pqT[:, 0:4, :].rearrange("p a t -> p (a t)"), PS[2][:], AF.Copy), R=bs(PS[2]), W=bs(pqT))
                        S.op('dve', cp_(pqT[:, 4:8, :].rearrange("p a t -> p (a t)"), PS[3][:]), R=bs(PS[3]), W=bs(pqT))
                        for h in range(8):
                            pa, pb2 = PS[4 + h // 4], PS[6 + h // 4]
                            S.op('pe', mm_(pa[:, (h % 4) * 128:(h % 4 + 1) * 128], pqT[0:64, h, :], kTb[0:64, :], True, True),
                                 R=bs(pqT, kTb), W=bs(pa))
                            S.op('pe', mm_(pb2[:, (h % 4) * 128:(h % 4 + 1) * 128], pqT[64:128, h, :], kTb[64:128, :], True, True),
                                 R=bs(pqT, kTb), W=bs(pb2))
                        s1t, s2t, zst = s1[ti], s2[ti], zs[ti]
                        S.op('act', act_(s1t[:, 0:4, :].rearrange("p a t -> p (a t)"), PS[4][:], AF.Copy), R=bs(PS[4]), W=bs(s1t))
                        S.op('dve', cp_(s1t[:, 4:8, :].rearrange("p a t -> p (a t)"), PS[5][:]), R=bs(PS[5]), W=bs(s1t))
                        S.op('act', act_(s2t[:, 0:4, :].rearrange("p a t -> p (a t)"), PS[6][:], AF.Copy), R=bs(PS[6]), W=bs(s2t))
                        S.op('dve', cp_(s2t[:, 4:8, :].rearrange("p a t -> p (a t)"), PS[7][:]), R=bs(PS[7]), W=bs(s2t))
                        for (sx, vx) in ((s1t, v1), (s2t, v2)):
                            for h in range(8):
                                S.op('dve', max_(vx[:, h, 0:8], sx[:, h, :]), R=bs(sx), W=bs(vx))
                                S.op('dve', mr_(w1[:, h, :], vx[:, h, 0:8], sx[:, h, :], NEGS), R=bs(sx, vx), W=bs(w1))
                                S.op('dve', max_(vx[:, h, 8:16], w1[:, h, :]), R=bs(w1), W=bs(vx))
                        for h in range(8):
                            S.op('dve', tt_(cand[:, h, :].rearrange("p (a c) -> p a c", a=16),
                                             v1[:, h, :].unsqueeze(2).to_broadcast([128, 16, 16]),
                                             v2[:, h, :].unsqueeze(1).to_broadcast([128, 16, 16]), ALU.add), R=bs(v1, v2), W=bs(cand))
                        for h in range(8):
                            S.op('dve', max_(best[:, h, 0:8], cand[:, h, :]), R=bs(cand), W=bs(best))
                            S.op('dve', mr_(cand[:, h, :], best[:, h, 0:8], cand[:, h, :], NEGS), R=bs(best, cand), W=bs(cand))
                            S.op('dve', max_(best[:, h, 8:16], cand[:, h, :]), R=bs(cand), W=bs(best))
                        S.op('dve', tt_(et[:], best[:], best[:, :, 0:1].to_broadcast([128, 8, 16]), ALU.subtract), R=bs(best), W=bs(et))
                        S.op('act', act_(et[:], et[:], AF.Exp), R=bs(et), W=bs(et))
                        S.op('dve', red_(zst[:, 0:8], et[:], ALU.add), R=bs(et), W=bs(zst))
                        S.op('act', act_(zst[:, 8:16], zst[:, 0:8], AF.Ln), R=bs(zst), W=bs(zst))
                        S.op('dve', tt_(zst[:, 16:24], zst[:, 8:16], best[:, :, 0], ALU.add), R=bs(zst, best), W=bs(zst))
                        S.op('dve', ts_(zst[:, 16:24], zst[:, 16:24], -1.0, None, ALU.mult), R=bs(zst), W=bs(zst))
                        S.op('dve', cp_(zst[:, 24:32], best[:, :, 15]), R=bs(best), W=bs(zst))
                    ntg = len(grp_tiles)
                    NTT = ntg * 128
                    pend = []

                    def flush_pend(keep):
                        while len(pend) > keep:
                            fn_, R_, W_ = pend.pop(0)
                            S.op('dve', fn_, R=R_, W=W_)

                    def emit_gate_unit(ib, ti, h):
                        Gh = Gh2[ib % 2]
                        u_ = rot['sg']
                        rot['sg'] += 1
                        pX = PS[6 + u_ % 2]
                        in0 = s1[ti][:, h, ib * NI:(ib + 1) * NI].unsqueeze(2).to_broadcast([128, NI, 128])
                        in1 = s2[ti][:, h, :].unsqueeze(1).to_broadcast([128, NI, 128])
                        nb_ap = zs[ti][:, 16 + h:17 + h]
                        tau_ap = zs[ti][:, 24 + h:25 + h]
                        if u_ % 4 != 0:
                            sg_ = Sg[rot['w'] % 4]
                            rot['w'] += 1
                            S.op('pool', tt_(sg_[:], in0, in1, ALU.add), R=bs(s1[ti], s2[ti]), W=bs(sg_))
                            S.op('act', act_(pX[:], sg_[:].rearrange("p a c -> p (a c)"), AF.Exp, bias=nb_ap),
                                 R=bs(sg_, zs[ti]), W=bs(pX))
                            pend.append((stt_(Gh[ti][:, h, :], sg_[:].rearrange("p a c -> p (a c)"), tau_ap, pX[:],
                                              ALU.is_ge, ALU.mult), bs(sg_, zs[ti], pX), bs(Gh[ti])))
                        else:
                            ee_ = Ee[rot['ee'] % 2]
                            rot['ee'] += 1
                            S.op('dve', tt_(pX[:].rearrange("p (a c) -> p a c", a=NI), in0, in1, ALU.add),
                                 R=bs(s1[ti], s2[ti]), W=bs(pX))
                            S.op('act', act_(ee_[:], pX[:], AF.Exp, bias=nb_ap), R=bs(pX, zs[ti]), W=bs(ee_))
                            pend.append((stt_(Gh[ti][:, h, :], pX[:], tau_ap, ee_[:], ALU.is_ge, ALU.mult),
                                         bs(pX, zs[ti], ee_), bs(Gh[ti])))
                        flush_pend(1)

                    units = [(ti, h) for ti in range(ntg) for h in range(8)]
                    st = {}

                    def issue_loads(ib):
                        e0 = ib * EB
                        uc = uch[ib % 2]
                        vcb = vch[ib % 2]
                        S.dma('sp', uc[:], uTb[:, e0:e0 + EB].rearrange("(k p) n -> p k n", p=128), W=bs(uc))
                        S.dma('sp', vcb[:], pvb[e0:e0 + EB, :].rearrange("(a p) n -> p a n", p=128), W=bs(vcb))

                    def stage_A(ib):
                        uc = uch[ib % 2]
                        pAt = PS[2 + ib % 2]
                        for blk in range(NI):
                            bsl = slice(blk * 128, (blk + 1) * 128)
                            for k in range(8):
                                S.op('pe', mm_(pAt[:, blk * 128:(blk + 1) * 128], uc[:, k, bsl], h2T[:, k, 0:128], k == 0, k == 7),
                                     R=bs(uc, h2T), W=bs(pAt))
                        ga = gAT[ib % 2]
                        S.op('act', act_(ga[:], pAt[:, 0:NI * 128], AF.Gelu), R=bs(pAt), W=bs(ga))

                    def stage_G(ib, blk):
                        Gh = Gh2[ib % 2]
                        bsl = slice(blk * 128, (blk + 1) * 128)
                        ga = gAT[ib % 2]
                        pGt = PS[4 + rot['pg'] % 2]
                        rot['pg'] += 1
                        pG = pGt[:, 0:128]
                        for h in range(8):
                            S.op('pe', mm_(pG, Gh[0][:, h, bsl], ident_b[:], h == 0, h == 7), R=bs(Gh[0], ident_b), W=bs(pGt))
                        wt_ = WTt[rot['wt'] % 4]
                        rot['wt'] += 1
                        S.op('dve', tt_(wt_[:], pG, ga[:, bsl], ALU.mult), R=bs(pGt, ga), W=bs(wt_))
                        st[(ib, blk)] = wt_

                    def stage_O(ib, blk):
                        vcb = vch[ib % 2]
                        first = (ib == 0 and blk == 0)
                        last = (ib == nblk - 1 and blk == NI - 1)
                        wt_ = st.pop((ib, blk))
                        for dh in range(2):
                            S.op('pe', mm_(PS[dh][:], wt_[:], vcb[:, blk, dh * 512:(dh + 1) * 512], first, last),
                                 R=bs(wt_, vcb), W=bs(PS[dh]))

                    assert TG == 1
                    for (ti, h) in units:
                        emit_gate_unit(0, ti, h)
                    issue_loads(0)
                    stage_A(0)
                    upb = (len(units) + NI - 1) // NI
                    prev = None
                    for ib in range(nblk):
                        for blk in range(NI):
                            if blk == 0:
                                flush_pend(0)
                            stage_G(ib, blk)
                            if prev is not None:
                                stage_O(*prev)
                            prev = (ib, blk)
                            if ib + 1 < nblk:
                                if blk == 0:
                                    issue_loads(ib + 1)
                                for (ti, h) in units[blk * upb:(blk + 1) * upb]:
                                    emit_gate_unit(ib + 1, ti, h)
                                if blk == 1:
                                    stage_A(ib + 1)
                    flush_pend(0)
                    stage_O(*prev)
                    for ti, tt in enumerate(grp_tiles):
                        r0 = b * SEQ + tt * 128
                        X = x1t[ti]
                        for dh in range(2):
                            sl = slice(dh * 512, (dh + 1) * 512)
                            S.op('dve', tt_(po[:, sl], PS[ti * 2 + dh][:], g2bc[:, sl], ALU.mult), R=bs(PS[ti * 2 + dh], g2bc), W=bs(po))
                        S.op('pool', tt_(po[:], po[:], X[:], ALU.add), R=bs(po, X), W=bs(po))
                        rms_rstd(PE_, po[:], D, small[:, 4:5], junk[:], bs(po), bs(junk), bs(small))
                        S.op('dve', stt_(po[:], po[:], small[:, 4:5], gfbc[:], ALU.mult, ALU.mult), R=bs(po, small, gfbc), W=bs(po))
                        S.dma('sp', y[r0:r0 + 128, :], po[:], R=bs(po))
            S.barrier()
        S.barrier()
        S.emit()
    return nc


def _prep_inputs(inp, core):
    f = np.float32
    bsl = slice(core * NB, (core + 1) * NB)
    c = np.asarray(inp['c'], f)[bsl]
    cT = np.ascontiguousarray(c.reshape(NB, 8, 128).transpose(2, 1, 0)).reshape(128, 16)
    pos = np.asarray(inp['positions'], np.int32)[bsl]
    posT = np.ascontiguousarray(pos.reshape(NB, NT, 128).transpose(0, 2, 1))
    gcat = np.concatenate([np.asarray(inp['g_out_a'], f)[0], np.asarray(inp['g_out_b'], f)[0]])
    gvec = np.concatenate([np.asarray(inp['g_mix'], f)[0].reshape(8, 128).T,
                           np.asarray(inp['g_ffn'], f)[0].reshape(8, 128).T,
                           gcat.reshape(8, 128).T], axis=1)
    kT = np.concatenate([np.asarray(inp['peer_keys1'], f)[0].T, np.asarray(inp['peer_keys2'], f)[0].T], axis=0)
    return {
        "x": np.ascontiguousarray(np.asarray(inp['x'], f)[bsl].reshape(NB * SEQ, D)),
        "cT": np.ascontiguousarray(cT),
        "posT": posT,
        "w_ada": np.ascontiguousarray(np.asarray(inp['w_ada'], f)[0]),
        "b_adaT": np.ascontiguousarray(np.asarray(inp['b_ada'], f)[0].reshape(48, 128).T),
        "gvec": np.ascontiguousarray(gvec),
        "g_final": np.ascontiguousarray(np.asarray(inp['g_final'], f).reshape(1, D)),
        "g_kv": np.ascontiguousarray(np.asarray(inp['g_kv'], f)[0].reshape(1, 128)),
        "b_forget": np.ascontiguousarray(np.asarray(inp['b_forget'], f)[0].reshape(1, 8)),
        "w_in": np.ascontiguousarray(np.asarray(inp['w_in'], f)[0]),
        "w_uk": np.ascontiguousarray(np.asarray(inp['w_uk'], f)[0].reshape(128, 384)),
        "w_uv": np.ascontiguousarray(np.asarray(inp['w_uv'], f)[0].reshape(128, 512)),
        "w_out": np.ascontiguousarray(np.asarray(inp['w_out'], f)[0]),
        "w_pq": np.ascontiguousarray(np.asarray(inp['w_peer_q'], f)[0]),
        "kT": np.ascontiguousarray(kT),
    }


def kernel(**inputs):
    f = np.float32
    nc = build_nc()
    uT = np.ascontiguousarray(np.asarray(inputs['peer_u'], f)[0].T)
    pv = np.ascontiguousarray(np.asarray(inputs['peer_v'], f)[0])
    in_maps = []
    for core in range(8):
        d = _prep_inputs(inputs, core)
        d["uT"] = uT
        d["pv"] = pv
        in_maps.append(d)
    res = run_bass_kernel_spmd(nc, in_maps, core_ids=list(range(8)))
    out = np.concatenate([np.asarray(r["y"], f).reshape(NB, SEQ, D) for r in res.results], axis=0)
    return out
```

```python
import math
import numpy as np
from contextlib import ExitStack
import concourse.bass as bass
import concourse.mybir as mybir
from concourse.bass_utils import run_bass_kernel_spmd

F32 = mybir.dt.float32
BF16 = mybir.dt.bfloat16
I32 = mybir.dt.int32
AF = mybir.ActivationFunctionType
ALU = mybir.AluOpType
AX = mybir.AxisListType

ENGS = ('pe', 'act', 'dve', 'pool', 'sp')
EPOCH = 30000
N_EPOCH = 12
N_DMA = 24

NEG = -30000.0
NEGS = -1.0e30
SEQ = 4096
NT = 32
NB = 2
D = 1024
NCOL = 2496
THETA = 500000.0


class Buf:
    __slots__ = ('w', 'r', 'name')

    def __init__(self, name=''):
        self.w = None
        self.r = []
        self.name = name


class Sched:
    def __init__(self, nc, ctx):
        self.nc = nc
        self.q = {e: [] for e in ENGS}
        self.sems = {}
        for e in ENGS:
            if e == 'sp':
                continue
            for k in range(N_EPOCH):
                self.sems[(e, k)] = ctx.enter_context(nc.semaphore(f"s_{e}{k}"))
        for k in range(N_DMA):
            self.sems[('dma', k)] = ctx.enter_context(nc.semaphore(f"s_dma{k}"))
        self.epoch = {e: 0 for e in ENGS}
        self.cnt = {e: 0 for e in ENGS}
        self.dcnt = [0] * N_DMA
        self.dnext = 0
        self.known = {e: {} for e in ENGS}
        self.nops = 0

    def _need(self, eng, deps):
        waits = []
        kn = self.known[eng]
        best = {}
        for (s, v) in deps:
            if best.get(s, 0) < v:
                best[s] = v
        for s, v in best.items():
            if kn.get(s, 0) < v:
                kn[s] = v
                waits.append((s, v))
        return waits

    @staticmethod
    def _deps(R, W):
        deps = []
        for b in R:
            if b.w is not None:
                deps.append(b.w)
        for b in W:
            if b.w is not None:
                deps.append(b.w)
            deps.extend(b.r)
        return deps

    def op(self, eng, fn, R=(), W=()):
        deps = self._deps(R, W)
        if eng == 'pe':
            deps = [d for d in deps if d[0][0] != 'pe']
        waits = self._need(eng, deps)
        if self.cnt[eng] >= EPOCH:
            self.epoch[eng] += 1
            self.cnt[eng] = 0
        self.cnt[eng] += 1
        tok = ((eng, self.epoch[eng]), self.cnt[eng])
        self.q[eng].append((waits, fn, tok[0], 1))
        for b in R:
            b.r.append(tok)
        for b in W:
            b.w = tok
            b.r = []
        self.nops += 1
        return tok

    def dma(self, q, out, in_, R=(), W=(), **kw):
        slot = self.dnext
        self.dnext = (self.dnext + 1) % N_DMA
        s = ('dma', slot)
        deps = self._deps(R, W)
        if self.dcnt[slot] > 0:
            deps.append((s, self.dcnt[slot]))
        waits = self._need(q, deps)
        self.dcnt[slot] += 16
        tok = (s, self.dcnt[slot])

        def fn(e, out=out, in_=in_, kw=kw):
            return e.dma_start(out=out, in_=in_, **kw)
        self.q[q].append((waits, fn, s, 16))
        for b in R:
            b.r.append(tok)
        for b in W:
            b.w = tok
            b.r = []
        self.nops += 1
        return tok

    def barrier(self):
        toks = []
        for e in ENGS:
            if e == 'sp':
                continue
            if self.cnt[e] > 0:
                toks.append(((e, self.epoch[e]), self.cnt[e]))
        for k in range(N_DMA):
            if self.dcnt[k] > 0:
                toks.append((('dma', k), self.dcnt[k]))
        for e in ENGS:
            waits = self._need(e, toks)
            if waits:
                self.q[e].append((waits, None, None, 0))

    def emit(self):
        nc = self.nc
        sems = self.sems
        qs = self.q

        def replay(e, lst):
            for (waits, fn, s, inc) in lst:
                for (ws, v) in waits:
                    e.wait_ge(sems[ws], v)
                if fn is None:
                    continue
                fn(e).then_inc(sems[s], inc)

        with nc.Block() as block:
            @block.tensor
            def _(e):
                replay(e, qs['pe'])

            @block.scalar
            def _(e):
                replay(e, qs['act'])

            @block.vector
            def _(e):
                replay(e, qs['dve'])

            @block.gpsimd
            def _(e):
                replay(e, qs['pool'])

            @block.sync
            def _(e):
                replay(e, qs['sp'])


class T:
    def __init__(self, t, name=''):
        self.t = t
        self.b = Buf(name)

    def __getitem__(self, k):
        return self.t[k]


def build_nc(stage=99, ntile_dbg=None, nb_dbg=None):
    nc = bass.Bass("TRN2", target_bir_lowering=False)

    def din(name, shape, dt):
        return nc.dram_tensor(name, shape, dt, kind="ExternalInput").ap()

    def dscr(name, shape, dt):
        return nc.dram_tensor(name, shape, dt, kind="Internal").ap()

    x = din("x", [NB * SEQ, D], F32)
    cT = din("cT", [128, 16], F32)
    posT = din("posT", [NB, 128, NT], I32)
    w_ada = din("w_ada", [D, 6 * D], F32)
    b_adaT = din("b_adaT", [128, 48], F32)
    gvec = din("gvec", [128, 24], F32)
    g_final = din("g_final", [1, D], F32)
    g_kv = din("g_kv", [1, 128], F32)
    b_forget = din("b_forget", [1, 8], F32)
    w_in = din("w_in", [D, NCOL], F32)
    w_uk = din("w_uk", [128, 384], F32)
    w_uv = din("w_uv", [128, 512], F32)
    w_out = din("w_out", [D, D], F32)
    w_pq = din("w_pq", [D, D], F32)
    kT = din("kT", [128, 128], F32)
    uT = din("uT", [D, 16384], F32)
    pv = din("pv", [16384, D], F32)
    y = nc.dram_tensor("y", [NB * SEQ, D], F32, kind="ExternalOutput").ap()

    QAT = dscr("QAT", [NB, 128, 4, SEQ], BF16)
    KATs = dscr("KATs", [NB, 128, 4, SEQ], BF16)
    VAs = dscr("VAs", [NB, 128, NT, 512], BF16)
    IQT = dscr("IQT", [NB, 128, 3, SEQ], BF16)
    IKT = dscr("IKT", [NB, 128, SEQ], BF16)
    SGN = dscr("SGN", [NB, 128, NT, 8], F32)
    QBT = dscr("QBT", [NB, 128, 4, SEQ], BF16)
    KBTs = dscr("KBTs", [NB, 128, 4, SEQ], BF16)
    VBs = dscr("VBs", [NB, 128, NT, 512], BF16)
    YA = dscr("YA", [NB * SEQ, D], F32)
    X1 = dscr("X1", [NB * SEQ, D], F32)
    NCUM = dscr("NCUM", [NB, 8, SEQ], F32)
    uTb = dscr("uTb", [D, 16384], BF16)
    pvb = dscr("pvb", [16384, D], BF16)
    DBG = {}
    dbg_stage = stage

    with ExitStack() as ctx:
        S = Sched(nc, ctx)
        counter = [0]

        def sb(ctxx, shape, dt, name=None):
            counter[0] += 1
            nm = f"{name or 't'}_{counter[0]}"
            return T(ctxx.enter_context(nc.sbuf_tensor(nm, shape, dt)), nm)

        def psb(ctxx, shape, dt, name=None):
            counter[0] += 1
            nm = f"{name or 'p'}_{counter[0]}"
            return T(ctxx.enter_context(nc.psum_tensor(nm, shape, dt)), nm)

        def bs(*ts):
            return [t.b for t in ts]

        def ts_(out, in0, s1, s2, op0, op1=None, accum_out=None):
            if op1 is None:
                return lambda e: e.tensor_scalar(out=out, in0=in0, scalar1=s1, scalar2=None, op0=op0)
            if accum_out is not None:
                return lambda e: e.tensor_scalar(out=out, in0=in0, scalar1=s1, scalar2=s2, op0=op0, op1=op1,
                                                 accum_out=accum_out)
            return lambda e: e.tensor_scalar(out=out, in0=in0, scalar1=s1, scalar2=s2, op0=op0, op1=op1)

        def tt_(out, in0, in1, op):
            return lambda e: e.tensor_tensor(out=out, in0=in0, in1=in1, op=op)

        def stt_(out, in0, scalar, in1, op0, op1):
            return lambda e: e.scalar_tensor_tensor(out=out, in0=in0, scalar=scalar, in1=in1, op0=op0, op1=op1)

        def cp_(out, in_):
            return lambda e: e.tensor_copy(out=out, in_=in_)

        def act_(out, in_, func, bias=None, scale=None, accum_out=None):
            kw = {}
            if bias is not None:
                kw['bias'] = bias
            if scale is not None:
                kw['scale'] = scale
            if accum_out is not None:
                kw['accum_out'] = accum_out
            return lambda e: e.activation(out=out, in_=in_, func=func, **kw)

        def max_(out, in_):
            return lambda e: e.max(out=out, in_=in_)

        def mr_(out, repl, vals, imm):
            return lambda e: e.match_replace(out=out, in_to_replace=repl, in_values=vals, imm_value=imm)

        def red_(out, in_, op):
            return lambda e: e.tensor_reduce(out=out, in_=in_, axis=AX.X, op=op)

        def recip_(out, in_):
            return lambda e: e.reciprocal(out=out, in_=in_)

        def memset_(ap, v):
            return lambda e: e.memset(ap, v)

        def mm_(out, lhsT, rhs, start, stop, sgc=False):
            if sgc:
                return lambda e: e.matmul(out, lhsT=lhsT, rhs=rhs, start=start, stop=stop, skip_group_check=True)
            return lambda e: e.matmul(out, lhsT=lhsT, rhs=rhs, start=start, stop=stop)

        def tr_(out, in_, ident):
            return lambda e: e.transpose(out=out, in_=in_, identity=ident)

        G = ctx
        idi = sb(G, [128, 128], I32)
        pidi = sb(G, [128, 1], I32)
        idf = sb(G, [128, 128], F32)
        pidf = sb(G, [128, 1], F32)
        ident_f = sb(G, [128, 128], F32, "ident_f")
        ident_b = sb(G, [128, 128], BF16, "ident_b")
        cmaskL = sb(G, [128, 128], F32, "cmaskL")
        cmaskS = sb(G, [128, 128], F32, "cmaskS")
        tri_f = sb(G, [128, 128], F32, "tri_f")
        ones_f = sb(G, [128, 128], F32, "ones_f")
        invf = sb(G, [128, 24], F32, "invf")
        cact = sb(G, [128, 16], F32, "cact")
        badaT = sb(G, [128, 48], F32, "badaT")
        gv = sb(G, [128, 24], F32, "gv")
        mod = sb(G, [128, 48, 2], F32, "mod")
        mv = [sb(G, [128, 48], F32, f"mv{b}") for b in range(NB)]
        gkvbc = sb(G, [128, 128], F32, "gkvbc")
        bfbc = sb(G, [128, 8], F32, "bfbc")
        lsall = sb(G, [128, NT, 8], F32, "lsall")
        dgt = sb(G, [128, 128], F32, "dgt")
        rt = sb(G, [128, NT, 24], F32, "rt")
        small = sb(G, [128, 64], F32, "small")
        stgf = sb(G, [128, 1024], F32, "stgf")
        castrot = [0]

        def load_cast(dst_ap, dst_t, src_ap, n):
            S.dma('sp', stgf[:, 0:n], src_ap, W=bs(stgf))
            castrot[0] += 1
            if castrot[0] % 2 == 0:
                S.op('act', act_(dst_ap, stgf[:, 0:n], AF.Copy), R=bs(stgf), W=bs(dst_t))
            else:
                S.op('dve', cp_(dst_ap, stgf[:, 0:n]), R=bs(stgf), W=bs(dst_t))

        PS = [psb(G, [128, 512], F32, f"PS{i}") for i in range(8)]

        def psbf(i):
            return PS[i][:].bitcast(BF16)

        S.op('pool', lambda e: e.iota(idi[:], pattern=[[1, 128]], base=0, channel_multiplier=0), W=bs(idi))
        S.op('pool', lambda e: e.iota(pidi[:], pattern=[[0, 1]], base=0, channel_multiplier=1), W=bs(pidi))
        S.op('dve', cp_(idf[:], idi[:]), R=bs(idi), W=bs(idf))
        S.op('dve', cp_(pidf[:], pidi[:]), R=bs(pidi), W=bs(pidf))
        S.op('dve', ts_(ident_f[:], idf[:], pidf[:, 0:1], None, ALU.is_equal), R=bs(idf, pidf), W=bs(ident_f))
        S.op('dve', cp_(ident_b[:], ident_f[:]), R=bs(ident_f), W=bs(ident_b))
        S.op('dve', ts_(cmaskL[:], idf[:], pidf[:, 0:1], NEG, ALU.is_gt, ALU.mult), R=bs(idf, pidf), W=bs(cmaskL))
        S.op('dve', ts_(cmaskS[:], idf[:], pidf[:, 0:1], NEGS, ALU.is_gt, ALU.mult), R=bs(idf, pidf), W=bs(cmaskS))
        S.op('dve', ts_(tri_f[:], idf[:], pidf[:, 0:1], None, ALU.is_ge), R=bs(idf, pidf), W=bs(tri_f))
        S.op('dve', memset_(ones_f[:], 1.0), W=bs(ones_f))
        fr = [THETA ** (-(2.0 * k) / 16.0) for k in range(8)] + [THETA ** (-(2.0 * k) / 8.0) for k in range(4)]
        for rep in range(2):
            for k in range(12):
                S.op('dve', (lambda e, c=rep * 12 + k, v=float(np.float32(fr[k])): e.memset(invf[:, c:c + 1], v)),
                     W=bs(invf))
        S.dma('sp', cact[:], cT, W=bs(cact))
        S.dma('sp', badaT[:], b_adaT, W=bs(badaT))
        S.dma('sp', gv[:], gvec, W=bs(gv))
        S.dma('sp', gkvbc[:], g_kv.partition_broadcast(128), W=bs(gkvbc))
        S.dma('sp', bfbc[:], b_forget.partition_broadcast(128), W=bs(bfbc))
        S.op('act', act_(cact[:], cact[:], AF.Silu), R=bs(cact), W=bs(cact))

        with ExitStack() as P0:
            wada = [sb(P0, [128, 8, D], F32, f"wada{i}") for i in range(2)]
            for g in range(6):
                wt_ = wada[g % 2]
                S.dma('sp', wt_[:], w_ada[:, g * D:(g + 1) * D].rearrange("(k p) n -> p k n", p=128), W=bs(wt_))
                for jc in range(8):
                    j = g * 8 + jc
                    for k in range(8):
                        S.op('pe', mm_(PS[0][:, j * 2:(j + 1) * 2], wt_[:, k, jc * 128:(jc + 1) * 128],
                                       cact[:, k * 2:(k + 1) * 2], k == 0, k == 7), R=bs(wt_, cact), W=bs(PS[0]))
            S.op('dve', tt_(mod[:], PS[0][:, 0:96].rearrange("p (j b) -> p j b", b=2),
                            badaT[:].unsqueeze(2).to_broadcast([128, 48, 2]), ALU.add), R=bs(PS[0], badaT), W=bs(mod))
            for b in range(NB):
                m = mv[b]
                S.op('dve', cp_(m[:, 0:8], mod[:, 0:8, b]), R=bs(mod), W=bs(m))
                S.op('dve', stt_(m[:, 8:16], mod[:, 8:16, b], 1.0, gv[:, 0:8], ALU.add, ALU.mult), R=bs(mod, gv), W=bs(m))
                S.op('dve', cp_(m[:, 16:24], mod[:, 16:24, b]), R=bs(mod), W=bs(m))
                S.op('dve', cp_(m[:, 24:32], mod[:, 24:32, b]), R=bs(mod), W=bs(m))
                S.op('dve', stt_(m[:, 32:40], mod[:, 32:40, b], 1.0, gv[:, 8:16], ALU.add, ALU.mult), R=bs(mod, gv), W=bs(m))
                S.op('dve', cp_(m[:, 40:48], mod[:, 40:48, b]), R=bs(mod), W=bs(m))
        S.barrier()

        if stage >= 4:
            with ExitStack() as P0:
                CW = 8192
                cf = [sb(P0, [128, CW], F32, f"cf{i}") for i in range(2)]
                cb = [sb(P0, [128, CW], BF16, f"cb{i}") for i in range(2)]
                i = 0
                for (src, dst) in ((uT, uTb), (pv, pvb)):
                    sv = src.rearrange("(p a) n -> p (a n)", p=128)
                    dv = dst.rearrange("(p a) n -> p (a n)", p=128)
                    tot = sv.shape[1]
                    for c in range(tot // CW):
                        f_, t_ = cf[i % 2], cb[i % 2]
                        S.dma('sp', f_[:], sv[:, c * CW:(c + 1) * CW], W=bs(f_))
                        if i % 3 == 0:
                            S.op('act', act_(t_[:], f_[:], AF.Copy), R=bs(f_), W=bs(t_))
                        elif i % 3 == 1:
                            S.op('dve', cp_(t_[:], f_[:]), R=bs(f_), W=bs(t_))
                        else:
                            S.op('pool', cp_(t_[:], f_[:]), R=bs(f_), W=bs(t_))
                        S.dma('sp', dv[:, c * CW:(c + 1) * CW], t_[:], R=bs(t_))
                        i += 1
            S.barrier()

        def build_gate_bc(m, src0, dst):
            for k in range(8):
                S.op('dve', ts_(dgt[:], ident_f[:], m[:, src0 + k:src0 + k + 1], None, ALU.mult),
                     R=bs(ident_f, m), W=bs(dgt))
                pb = PS[1 + k // 4]
                S.op('pe', mm_(pb[:, (k % 4) * 128:(k % 4 + 1) * 128], ones_f[:], dgt[:], True, True),
                     R=bs(ones_f, dgt), W=bs(pb))
            S.op('act', act_(dst[:, 0:512], PS[1][:], AF.Copy), R=bs(PS[1]), W=bs(dst))
            S.op('act', act_(dst[:, 512:1024], PS[2][:], AF.Copy), R=bs(PS[2]), W=bs(dst))

        def rms_rstd(ctxx, src_ap, n, rstd_ap, junk_ap, src_b, junk_b, sm_b, eps=1e-6):
            S.op('act', act_(junk_ap, src_ap, AF.Square, accum_out=small[:, 60:61]), R=src_b, W=junk_b + bs(small))
            S.op('dve', ts_(small[:, 61:62], small[:, 60:61], 1.0 / n, eps, ALU.mult, ALU.add), R=bs(small), W=bs(small))
            S.op('act', act_(small[:, 62:63], small[:, 61:62], AF.Sqrt), R=bs(small), W=bs(small))
            S.op('dve', recip_(rstd_ap, small[:, 62:63]), R=bs(small), W=sm_b)

        tiles = list(range(NT)) if ntile_dbg is None else list(range(ntile_dbg))
        for b in range(NB if ntile_dbg is None else (nb_dbg or 1)):
            m = mv[b]
            with ExitStack() as P0:
                posi = sb(P0, [128, NT], I32)
                posf = sb(P0, [128, NT], F32)
                ang = sb(P0, [128, NT, 24], F32)
                kf = sb(P0, [128, NT, 24], F32)
                ki = sb(P0, [128, NT, 24], I32)
                S.dma('sp', posi[:], posT[b], W=bs(posi))
                S.op('dve', cp_(posf[:], posi[:]), R=bs(posi), W=bs(posf))
                S.op('dve', tt_(ang[:], posf[:].unsqueeze(2).to_broadcast([128, NT, 24]),
                                invf[:].unsqueeze(1).to_broadcast([128, NT, 24]), ALU.mult), R=bs(posf, invf), W=bs(ang))
                S.op('dve', ts_(ang[:, :, 12:24], ang[:, :, 12:24], math.pi / 2, None, ALU.add), R=bs(ang), W=bs(ang))
                S.op('dve', ts_(kf[:], ang[:], 1.0 / (2 * math.pi), None, ALU.mult), R=bs(ang), W=bs(kf))
                S.op('dve', cp_(ki[:], kf[:]), R=bs(kf), W=bs(ki))
                S.op('dve', cp_(kf[:], ki[:]), R=bs(ki), W=bs(kf))
                C1 = 6.28125
                C2 = 2 * math.pi - C1
                S.op('dve', stt_(ang[:].rearrange("p a b -> p (a b)"), kf[:].rearrange("p a b -> p (a b)"), -C1,
                                 ang[:].rearrange("p a b -> p (a b)"), ALU.mult, ALU.add), R=bs(kf, ang), W=bs(ang))
                S.op('dve', stt_(ang[:].rearrange("p a b -> p (a b)"), kf[:].rearrange("p a b -> p (a b)"), -C2,
                                 ang[:].rearrange("p a b -> p (a b)"), ALU.mult, ALU.add), R=bs(kf, ang), W=bs(ang))
                S.op('dve', ts_(ang[:], ang[:], -math.pi, math.pi, ALU.max, ALU.min), R=bs(ang), W=bs(ang))
                S.op('act', act_(rt[:], ang[:], AF.Sin), R=bs(ang), W=bs(rt))
            S.barrier()

            with ExitStack() as PP:
                winb = sb(PP, [128, 8, NCOL], BF16, "winb")
                wukb = sb(PP, [128, 384], BF16, "wukb")
                wuvb = sb(PP, [128, 512], BF16, "wuvb")
                for k in range(8):
                    for (c0, c1) in ((0, 1024), (1024, 2048), (2048, NCOL)):
                        load_cast(winb[:, k, c0:c1], winb, w_in[k * 128:(k + 1) * 128, c0:c1], c1 - c0)
                load_cast(wukb[:], wukb, w_uk, 384)
                load_cast(wuvb[:], wuvb, w_uv, 512)
                xt = [sb(PP, [128, D], F32, f"xt{i}") for i in range(2)]
                junk = sb(PP, [128, D], F32, "junk")
                xh = sb(PP, [128, D], F32, "xh")
                hT = sb(PP, [128, 8, 128], BF16, "hT")
                pr = sb(PP, [128, NCOL], F32, "pr")
                qa_b = sb(PP, [128, 512], BF16, "qa_b")
                qb_b = sb(PP, [128, 512], BF16, "qb_b")
                kb_b = sb(PP, [128, 512], BF16, "kb_b")
                vb_b = sb(PP, [128, 512], BF16, "vb_b")
                va_b = sb(PP, [128, 512], BF16, "va_b")
                ka_b = sb(PP, [128, 8, 64], BF16, "ka_b")
                kr_b = sb(PP, [128, 16], BF16, "kr_b")
                iq_f = sb(PP, [128, 8, 32], F32, "iq_f")
                iq_b = sb(PP, [128, 3, 4, 32], BF16, "iq_b")
                S.op('dve', memset_(iq_b[:], 0.0), W=bs(iq_b))
                ik_b = sb(PP, [128, 4, 32], BF16, "ik_b")
                ik_f = sb(PP, [128, 32], F32, "ik_f")
                latn = sb(PP, [128, 128], BF16, "latn")
                latnT = sb(PP, [128, 128], BF16, "latnT")
                rtmp = [sb(PP, [128, 8, 8], F32, f"rtmp{i}") for i in range(4)]
                wabs = sb(PP, [128, 8], F32, "wabs")
                sgn_t = sb(PP, [128, 8], F32, "sgn_t")
                fz = sb(PP, [128, 8], F32, "fz")
                oT = sb(PP, [128, 4, 128], BF16, "oT")
                oT2 = sb(PP, [128, 4, 128], BF16, "oT2")
                oT3 = sb(PP, [128, 4, 128], BF16, "oT3")
                oT4 = sb(PP, [128, 4, 128], BF16, "oT4")
                ncs = sb(PP, [8, 512], F32, "ncs")

                def rope(dst3, src3, H, half, sinap, cosap, Rb, Wb):
                    cb_ = cosap.unsqueeze(1).to_broadcast([128, H, half])
                    sb_ = sinap.unsqueeze(1).to_broadcast([128, H, half])
                    x1 = src3[:, :, 0:half]
                    x2 = src3[:, :, half:2 * half]
                    t = [r[:, 0:H, 0:half] for r in rtmp]
                    S.op('dve', tt_(t[0], x1, cb_, ALU.mult), R=Rb + bs(rt), W=bs(rtmp[0]))
                    S.op('dve', tt_(t[1], x2, sb_, ALU.mult), R=Rb + bs(rt), W=bs(rtmp[1]))
                    S.op('dve', tt_(t[2], x2, cb_, ALU.mult), R=Rb + bs(rt), W=bs(rtmp[2]))
                    S.op('dve', tt_(t[3], x1, sb_, ALU.mult), R=Rb + bs(rt), W=bs(rtmp[3]))
                    S.op('dve', tt_(dst3[:, :, 0:half], t[0], t[1], ALU.subtract), R=bs(rtmp[0], rtmp[1]), W=Wb)
                    S.op('dve', tt_(dst3[:, :, half:2 * half], t[2], t[3], ALU.add), R=bs(rtmp[2], rtmp[3]), W=Wb)

                for tt in tiles:
                    r0 = b * SEQ + tt * 128
                    X = xt[tt % 2]
                    S.dma('sp', X[:], x[r0:r0 + 128, :], W=bs(X))
                    rms_rstd(PP, X[:], D, small[:, 0:1], junk[:], bs(X), bs(junk), bs(small))
                    S.op('dve', ts_(xh[:], X[:], small[:, 0:1], None, ALU.mult), R=bs(X, small), W=bs(xh))
                    for k in range(8):
                        pb = PS[k // 4]
                        S.op('pe', tr_(pb[:, (k % 4) * 128:(k % 4 + 1) * 128], xh[:, k * 128:(k + 1) * 128], ident_f[:]),
                             R=bs(xh, ident_f), W=bs(pb))
                    for k in range(8):
                        pb = PS[k // 4]
                        src = pb[:, (k % 4) * 128:(k % 4 + 1) * 128]
                        if k // 4 == 0:
                            S.op('dve', ts_(hT[:, k, :], src, m[:, 8 + k:9 + k], m[:, k:k + 1], ALU.mult, ALU.add),
                                 R=bs(pb, m), W=bs(hT))
                        else:
                            S.op('act', act_(hT[:, k, :], src, AF.Identity, bias=m[:, k:k + 1], scale=m[:, 8 + k:9 + k]),
                                 R=bs(pb, m), W=bs(hT))
                    cols = [(0, 512), (512, 1024), (1024, 1536), (1536, 2048), (2048, NCOL)]
                    for ci, (c0, c1) in enumerate(cols):
                        pb = PS[2 + ci]
                        for k in range(8):
                            S.op('pe', mm_(pb[:, 0:c1 - c0], hT[:, k, :], winb[:, k, c0:c1], k == 0, k == 7),
                                 R=bs(hT, winb), W=bs(pb))
                        if ci % 2 == 0:
                            S.op('act', act_(pr[:, c0:c1], pb[:, 0:c1 - c0], AF.Copy), R=bs(pb), W=bs(pr))
                        else:
                            S.op('dve', cp_(pr[:, c0:c1], pb[:, 0:c1 - c0]), R=bs(pb), W=bs(pr))
                    sinA, sinI = rt[:, tt, 0:8], rt[:, tt, 8:12]
                    cosA, cosI = rt[:, tt, 12:20], rt[:, tt, 20:24]
                    S.op('act', act_(qa_b[:], pr[:, 0:512], AF.Copy), R=bs(pr), W=bs(qa_b))
                    rope(qa_b[:].rearrange("p (h d) -> p h d", h=8), pr[:, 0:512].rearrange("p (h d) -> p h d", h=8),
                         8, 8, sinA, cosA, bs(pr), bs(qa_b))
                    rope(kr_b[:].unsqueeze(1), pr[:, 640:656].unsqueeze(1), 1, 8, sinA, cosA, bs(pr), bs(kr_b))
                    rms_rstd(PP, pr[:, 512:640], 128, small[:, 1:2], junk[:, 0:128], bs(pr), bs(junk), bs(small))
                    S.op('dve', stt_(latn[:], pr[:, 512:640], small[:, 1:2], gkvbc[:], ALU.mult, ALU.mult),
                         R=bs(pr, small, gkvbc), W=bs(latn))
                    S.op('pe', tr_(psbf(7)[:, 0:128], latn[:], ident_b[:]), R=bs(latn, ident_b), W=bs(PS[7]))
                    S.op('act', act_(latnT[:], psbf(7)[:, 0:128], AF.Copy), R=bs(PS[7]), W=bs(latnT))
                    S.op('pe', mm_(PS[0][:, 0:384], latnT[:], wukb[:], True, True), R=bs(latnT, wukb), W=bs(PS[0]))
                    S.op('pe', mm_(PS[1][:, 0:512], latnT[:], wuvb[:], True, True), R=bs(latnT, wuvb), W=bs(PS[1]))
                    S.op('act', act_(ka_b[:, :, 16:64], PS[0][:, 0:384].rearrange("p (h d) -> p h d", h=8), AF.Copy),
                         R=bs(PS[0]), W=bs(ka_b))
                    S.op('dve', cp_(ka_b[:, :, 0:16], kr_b[:].unsqueeze(1).to_broadcast([128, 8, 16])),
                         R=bs(kr_b), W=bs(ka_b))
                    S.op('dve', cp_(va_b[:], PS[1][:, 0:512]), R=bs(PS[1]), W=bs(va_b))
                    S.dma('sp', VAs[b, :, tt, :], va_b[:], R=bs(va_b))
                    S.op('act', act_(wabs[:], pr[:, 944:952], AF.Abs, scale=(8.0 ** -0.5) * (32.0 ** -0.5)),
                         R=bs(pr), W=bs(wabs))
                    S.op('dve', ts_(sgn_t[:], pr[:, 944:952], 0.0, 2.0, ALU.is_ge, ALU.mult), R=bs(pr), W=bs(sgn_t))
                    S.op('dve', ts_(sgn_t[:], sgn_t[:], -1.0, None, ALU.add), R=bs(sgn_t), W=bs(sgn_t))
                    S.dma('sp', SGN[b, :, tt, :], sgn_t[:], R=bs(sgn_t))
                    S.op('dve', cp_(iq_f[:], pr[:, 656:912].rearrange("p (h d) -> p h d", h=8)), R=bs(pr), W=bs(iq_f))
                    rope(iq_f[:], pr[:, 656:912].rearrange("p (h d) -> p h d", h=8), 8, 4, sinI, cosI, bs(pr), bs(iq_f))
                    for k3 in range(3):
                        nh = 3 if k3 < 2 else 2
                        S.op('dve', tt_(iq_b[:, k3, 0:nh, :], iq_f[:, 3 * k3:3 * k3 + nh, :],
                                        wabs[:, 3 * k3:3 * k3 + nh].unsqueeze(2).to_broadcast([128, nh, 32]), ALU.mult),
                             R=bs(iq_f, wabs), W=bs(iq_b))
                    S.op('dve', cp_(ik_f[:], pr[:, 912:944]), R=bs(pr), W=bs(ik_f))
                    rope(ik_f[:].unsqueeze(1), pr[:, 912:944].unsqueeze(1), 1, 4, sinI, cosI, bs(pr), bs(ik_f))
                    S.op('dve', cp_(ik_b[:], ik_f[:].unsqueeze(1).to_broadcast([128, 4, 32])), R=bs(ik_f), W=bs(ik_b))
                    S.op('act', act_(qb_b[:], pr[:, 952:1464], AF.Copy), R=bs(pr), W=bs(qb_b))
                    S.op('dve', cp_(kb_b[:], pr[:, 1464:1976]), R=bs(pr), W=bs(kb_b))
                    S.op('act', act_(vb_b[:], pr[:, 1976:2488], AF.Copy), R=bs(pr), W=bs(vb_b))
                    S.dma('sp', VBs[b, :, tt, :], vb_b[:], R=bs(vb_b))
                    S.op('dve', tt_(fz[:], pr[:, 2488:2496], bfbc[:], ALU.add), R=bs(pr, bfbc), W=bs(fz))
                    S.op('act', act_(fz[:], fz[:], AF.Exp, scale=-1.0), R=bs(fz), W=bs(fz))
                    S.op('act', act_(lsall[:, tt, :], fz[:], AF.Ln, bias=1.0), R=bs(fz), W=bs(lsall))
                    jobs = [(qa_b, 4, QAT[b], oT, 0), (ka_b, 4, KATs[b], oT2, 1), (qb_b, 4, QBT[b], oT3, 2),
                            (kb_b, 4, KBTs[b], oT4, 3)]
                    for (src, nblk, dst, stg, pbi) in jobs:
                        sv = src[:] if len(src.t.shape) == 2 else src[:].rearrange("p h d -> p (h d)")
                        pbb = psbf(pbi)
                        for k in range(nblk):
                            S.op('pe', tr_(pbb[:, k * 128:(k + 1) * 128], sv[:, k * 128:(k + 1) * 128], ident_b[:]),
                                 R=bs(src, ident_b), W=bs(PS[pbi]))
                        eng = 'act' if pbi % 2 == 0 else 'dve'
                        if eng == 'act':
                            S.op('act', act_(stg[:].rearrange("p a t -> p (a t)"), pbb[:, 0:512], AF.Copy), R=bs(PS[pbi]), W=bs(stg))
                        else:
                            S.op('dve', cp_(stg[:].rearrange("p a t -> p (a t)"), pbb[:, 0:512]), R=bs(PS[pbi]), W=bs(stg))
                        S.dma('sp', dst[:, :, tt * 128:(tt + 1) * 128], stg[:], R=bs(stg))
                    pbb = psbf(4)
                    for k in range(3):
                        S.op('pe', tr_(pbb[:, k * 128:(k + 1) * 128], iq_b[:, k, :, :].rearrange("p a d -> p (a d)"), ident_b[:]),
                             R=bs(iq_b, ident_b), W=bs(PS[4]))
                    S.op('pe', tr_(pbb[:, 384:512], ik_b[:].rearrange("p a d -> p (a d)"), ident_b[:]),
                         R=bs(ik_b, ident_b), W=bs(PS[4]))
                    S.op('act', act_(oT[:].rearrange("p a t -> p (a t)"), pbb[:, 0:512], AF.Copy), R=bs(PS[4]), W=bs(oT))
                    S.dma('sp', IQT[b, :, :, tt * 128:(tt + 1) * 128], oT[:, 0:3, :], R=bs(oT))
                    S.dma('sp', IKT[b, :, tt * 128:(tt + 1) * 128], oT[:, 3, :], R=bs(oT))
                ntl = len(tiles)
                for T0 in range(0, ntl, 4):
                    pb = PS[5 + (T0 // 4) % 2]
                    for Tt in range(T0, min(T0 + 4, ntl)):
                        oc = (Tt - T0) * 128
                        for J in range(Tt + 1):
                            S.op('pe', mm_(pb[0:8, oc:oc + 128], lsall[:, J, :], (tri_f if J == Tt else ones_f)[:],
                                           J == 0, J == Tt), R=bs(lsall, tri_f, ones_f), W=bs(pb))
                    n = (min(T0 + 4, ntl) - T0) * 128
                    S.op('act', act_(ncs[0:8, 0:n], pb[0:8, 0:n], AF.Copy, scale=8.0), R=bs(pb), W=bs(ncs))
                    S.dma('sp', NCUM[b, :, T0 * 128:T0 * 128 + n], ncs[0:8, 0:n], R=bs(ncs))
            S.barrier()
            if dbg_stage == 1:
                continue

            for grp in (0, 1):
                with ExitStack() as PA:
                    KT = sb(PA, [128, 4, SEQ], BF16, "KT")
                    VV = sb(PA, [128, NT, 512], BF16, "VV")
                    L_all = len(tiles) * 128
                    for p4 in range(4):
                        S.dma('sp', KT[:, p4, 0:L_all], (KATs if grp == 0 else KBTs)[b, :, p4, 0:L_all], W=bs(KT))
                    S.dma('sp', VV[:, 0:len(tiles), :], (VAs if grp == 0 else VBs)[b, :, 0:len(tiles), :], W=bs(VV))
                    woutb = sb(PA, [128, 4, D], BF16, "woutb")
                    for k in range(4):
                        load_cast(woutb[:, k, :], woutb, w_out[grp * 512 + k * 128:grp * 512 + (k + 1) * 128, :], D)
                    QT = [sb(PA, [128, 4, 128], BF16, f"QT{i}") for i in range(2)]
                    lgs = [sb(PA, [128, SEQ], F32, f"lg{i}") for i in range(2)]
                    PT = [sb(PA, [128, 1024], BF16, f"PT{i}") for i in range(2)]
                    oall = sb(PA, [128, 512], F32, "oall")
                    onb = sb(PA, [128, 512], BF16, "onb")
                    mixT = sb(PA, [128, 4, 128], BF16, "mixT")
                    junk2 = sb(PA, [128, 512], F32, "junk2")
                    yt = sb(PA, [128, D], F32, "yt")
                    stat = sb(PA, [128, 32], F32, "stat")
                    if grp == 0:
                        IK = sb(PA, [128, SEQ], BF16, "IK")
                        S.dma('sp', IK[:, 0:L_all], IKT[b, :, 0:L_all], W=bs(IK))
                        IQ = [sb(PA, [128, 3, 128], BF16, f"IQ{i}") for i in range(2)]
                        sg = [sb(PA, [128, 8], F32, f"sg{i}") for i in range(2)]
                        sc = sb(PA, [128, SEQ], F32, "sc")
                        wk = lgs[1]
                        Pm1 = sb(PA, [128, SEQ], BF16, "Pm1")
                        Pms = [(sc[:].bitcast(BF16), sc), (Pm1[:], Pm1)]
                        mb = sb(PA, [128, SEQ], BF16, "mb")
                        rl = [sb(PA, [128, 512], F32, f"rl{i}") for i in range(2)]
                        m8 = sb(PA, [128, 8], F32, "m8")
                        bis = sb(PA, [128, 8], F32, "bis")
                        tau = sb(PA, [128, 1], F32, "tau")
                    else:
                        Pm0 = sb(PA, [128, SEQ], BF16, "Pm0")
                        Pm1 = sb(PA, [128, SEQ], BF16, "Pm1")
                        Pms = [(Pm0[:], Pm0), (Pm1[:], Pm1)]
                        fbts = [sb(PA, [128, SEQ], F32, f"fbt{i}") for i in range(2)]
                        xres = sb(PA, [128, D], F32, "xres")
                        yat = sb(PA, [128, D], F32, "yat")
                        g1bc = sb(PA, [128, D], F32, "g1bc")
                        build_gate_bc(m, 16, g1bc)
                    rotA = [0]
                    rotB = [0]
                    rotT = [0]
                    statb = [Buf() for _ in range(8)]

                    def nextA():
                        rotA[0] = (rotA[0] + 1) % 3
                        return 1 + rotA[0]

                    def nextB():
                        rotB[0] = (rotB[0] + 1) % 4
                        return 4 + rotB[0]

                    for qt in tiles:
                        r0 = b * SEQ + qt * 128
                        L = (qt + 1) * 128
                        nch = (L + 511) // 512
                        Q = QT[qt % 2]
                        S.dma('sp', Q[:], (QAT if grp == 0 else QBT)[b, :, :, qt * 128:(qt + 1) * 128], W=bs(Q))
                        if grp == 0:
                            iq = IQ[qt % 2]
                            sgt = sg[qt % 2]
                            S.dma('sp', iq[:], IQT[b, :, :, qt * 128:(qt + 1) * 128], W=bs(iq))
                            S.dma('sp', sgt[:], SGN[b, :, qt, :], W=bs(sgt))
                            for h in range(8):
                                pr0 = 32 * (h % 3)
                                for c in range(nch):
                                    c0, c1 = c * 512, min(L, (c + 1) * 512)
                                    pb = PS[nextB()]
                                    S.op('pe', mm_(pb[:, 0:c1 - c0], iq[pr0:pr0 + 32, h // 3, :], IK[pr0:pr0 + 32, c0:c1],
                                                   True, True), R=bs(iq, IK), W=bs(pb))
                                    r_ = rl[(h + c) % 2]
                                    S.op('act', act_(r_[:, 0:c1 - c0], pb[:, 0:c1 - c0], AF.Relu), R=bs(pb), W=bs(r_))
                                    if h == 0:
                                        S.op('dve', ts_(sc[:, c0:c1], r_[:, 0:c1 - c0], sgt[:, 0:1], None, ALU.mult),
                                             R=bs(r_, sgt), W=bs(sc))
                                    else:
                                        S.op('dve', stt_(sc[:, c0:c1], r_[:, 0:c1 - c0], sgt[:, h:h + 1], sc[:, c0:c1],
                                                         ALU.mult, ALU.add), R=bs(r_, sgt, sc), W=bs(sc))
                            if qt >= 2:
                                S.op('dve', red_(bis[:, 5:6], sc[:, 0:L], ALU.max), R=bs(sc), W=bs(bis))
                                S.op('dve', red_(bis[:, 0:1], sc[:, 0:L], ALU.min), R=bs(sc), W=bs(bis))
                                S.op('dve', tt_(bis[:, 1:2], bis[:, 5:6], bis[:, 0:1], ALU.subtract), R=bs(bis), W=bs(bis))
                            S.op('dve', tt_(sc[:, L - 128:L], sc[:, L - 128:L], cmaskS[:], ALU.add), R=bs(sc, cmaskS), W=bs(sc))
                            if qt >= 2:
                                NBIS = 22
                                for k in range(NBIS):
                                    stp = 2.0 ** -(k + 1)
                                    S.op('dve', stt_(bis[:, 2:3], bis[:, 1:2], stp, bis[:, 0:1], ALU.mult, ALU.add), R=bs(bis), W=bs(bis))
                                    S.op('dve', ts_(wk[:, 0:L], sc[:, 0:L], bis[:, 2:3], 0.0, ALU.is_ge, ALU.add, accum_out=bis[:, 3:4]),
                                         R=bs(sc, bis), W=bs(wk, bis))
                                    S.op('dve', ts_(bis[:, 4:5], bis[:, 3:4], 255.5, stp, ALU.is_ge, ALU.mult), R=bs(bis), W=bs(bis))
                                    S.op('dve', stt_(bis[:, 0:1], bis[:, 4:5], bis[:, 1:2], bis[:, 0:1], ALU.mult, ALU.add), R=bs(bis), W=bs(bis))
                                S.op('dve', ts_(tau[:], bis[:, 0:1], -1.0e29, None, ALU.max), R=bs(bis), W=bs(tau))
                            else:
                                S.op('dve', memset_(tau[:], -1.0e29), W=bs(tau))
                            S.op('dve', ts_(mb[:, 0:L], sc[:, 0:L], tau[:, 0:1], NEG, ALU.is_lt, ALU.mult), R=bs(sc, tau), W=bs(mb))
                        def head_S1(h):
                            p4, hf = h // 2, (h % 2) * 64
                            lg = lgs[h % 2]
                            if grp == 1:
                                fbt = fbts[h % 2]
                                S.dma('sp', fbt[:, 0:L], NCUM[b, h:h + 1, 0:L].partition_broadcast(128), W=bs(fbt))
                                bias_t = fbt
                            else:
                                bias_t = mb
                            for c in range(nch):
                                c0, c1 = c * 512, min(L, (c + 1) * 512)
                                pb = PS[nextB()]
                                S.op('pe', mm_(pb[:, 0:c1 - c0], Q[hf:hf + 64, p4, :], KT[hf:hf + 64, p4, c0:c1], True, True),
                                     R=bs(Q, KT), W=bs(pb))
                                S.op('dve', tt_(lg[:, c0:c1], pb[:, 0:c1 - c0], bias_t[:, c0:c1], ALU.add),
                                     R=bs(pb, bias_t), W=bs(lg))
                            if grp == 1:
                                S.op('pool', tt_(lg[:, L - 128:L], lg[:, L - 128:L], cmaskL[:], ALU.add), R=bs(lg, cmaskL), W=bs(lg))
                            S.op('dve', red_(stat[:, 16 + h:17 + h], lg[:, 0:L], ALU.max), R=bs(lg), W=[statb[h]])
                            S.op('dve', ts_(stat[:, 24 + h:25 + h], stat[:, 16 + h:17 + h], -0.125, None, ALU.mult), R=[statb[h]], W=[statb[h]])

                        def head_S2(h):
                            lg = lgs[h % 2]
                            Pm_ap, Pm_b = Pms[h % 2]
                            S.op('act', act_(Pm_ap[:, 0:L], lg[:, 0:L], AF.Exp, bias=stat[:, 24 + h:25 + h], scale=0.125,
                                             accum_out=stat[:, h:h + 1]), R=bs(lg) + [statb[h]], W=bs(Pm_b) + [statb[h]])
                            nkb = qt + 1
                            for g0 in range(0, nkb, 8):
                                g1 = min(nkb, g0 + 8)
                                pi = nextA()
                                pbb = psbf(pi)
                                for kb in range(g0, g1):
                                    S.op('pe', tr_(pbb[:, (kb - g0) * 128:(kb - g0 + 1) * 128], Pm_ap[:, kb * 128:(kb + 1) * 128], ident_b[:]),
                                         R=bs(Pm_b, ident_b), W=bs(PS[pi]))
                                rotT[0] += 1
                                ptt = PT[rotT[0] % 2]
                                n = (g1 - g0) * 128
                                if rotT[0] % 2 == 0:
                                    S.op('act', act_(ptt[:, 0:n], pbb[:, 0:n], AF.Copy), R=bs(PS[pi]), W=bs(ptt))
                                else:
                                    S.op('pool' if False else 'dve', cp_(ptt[:, 0:n], pbb[:, 0:n]), R=bs(PS[pi]), W=bs(ptt))
                                for kb in range(g0, g1):
                                    S.op('pe', mm_(PS[0][:, h * 64:(h + 1) * 64], ptt[:, (kb - g0) * 128:(kb - g0 + 1) * 128],
                                                   VV[:, kb, h * 64:(h + 1) * 64], kb == 0, kb == nkb - 1),
                                         R=bs(ptt, VV), W=bs(PS[0]))

                        head_S1(0)
                        for h in range(8):
                            if h + 1 < 8:
                                head_S1(h + 1)
                            head_S2(h)
                        S.op('dve', recip_(stat[:, 8:16], stat[:, 0:8]), R=statb, W=bs(stat))
                        S.op('dve', tt_(oall[:].rearrange("p (h d) -> p h d", h=8), PS[0][:].rearrange("p (h d) -> p h d", h=8),
                                        stat[:, 8:16].unsqueeze(2).to_broadcast([128, 8, 64]), ALU.mult), R=bs(PS[0], stat), W=bs(oall))
                        rms_rstd(PA, oall[:], 512, small[:, 2:3], junk2[:], bs(oall), bs(junk2), bs(small))
                        S.op('dve', ts_(onb[:], oall[:], small[:, 2:3], None, ALU.mult), R=bs(oall, small), W=bs(onb))
                        pbb = psbf(1)
                        for k in range(4):
                            S.op('pe', tr_(pbb[:, k * 128:(k + 1) * 128], onb[:, k * 128:(k + 1) * 128], ident_b[:]),
                                 R=bs(onb, ident_b), W=bs(PS[1]))
                        for k in range(4):
                            S.op('dve', ts_(mixT[:, k, :], pbb[:, k * 128:(k + 1) * 128], gv[:, 16 + grp * 4 + k:17 + grp * 4 + k], None, ALU.mult),
                                 R=bs(PS[1], gv), W=bs(mixT))
                        for dh in range(2):
                            for k in range(4):
                                S.op('pe', mm_(PS[2 + dh][:], mixT[:, k, :], woutb[:, k, dh * 512:(dh + 1) * 512], k == 0, k == 3),
                                     R=bs(mixT, woutb), W=bs(PS[2 + dh]))
                        if grp == 0:
                            S.op('act', act_(yt[:, 0:512], PS[2][:], AF.Copy), R=bs(PS[2]), W=bs(yt))
                            S.op('dve', cp_(yt[:, 512:1024], PS[3][:]), R=bs(PS[3]), W=bs(yt))
                            S.dma('sp', YA[r0:r0 + 128, :], yt[:], R=bs(yt))
                        else:
                            S.dma('sp', yat[:], YA[r0:r0 + 128, :], W=bs(yat))
                            S.dma('sp', xres[:], x[r0:r0 + 128, :], W=bs(xres))
                            for dh in range(2):
                                sl = slice(dh * 512, (dh + 1) * 512)
                                S.op('dve', tt_(yt[:, sl], PS[2 + dh][:], yat[:, sl], ALU.add), R=bs(PS[2 + dh], yat), W=bs(yt))
                            S.op('pool', tt_(yt[:], yt[:], g1bc[:], ALU.mult), R=bs(yt, g1bc), W=bs(yt))
                            S.op('pool', tt_(yt[:], yt[:], xres[:], ALU.add), R=bs(yt, xres), W=bs(yt))
                            S.dma('sp', X1[r0:r0 + 128, :], yt[:], R=bs(yt))
                S.barrier()
            if dbg_stage == 3:
                nrow = len(tiles) * 128
                S.dma('sp', y[b * SEQ:b * SEQ + nrow, :], X1[b * SEQ:b * SEQ + nrow, :])
                S.barrier()
            if dbg_stage <= 4:
                continue

            with ExitStack() as PE_:
                TG = 1
                TT = TG * 128
                NI = 4
                EB = NI * 128
                nblk = 16384 // EB
                wpqb = sb(PE_, [128, 8, D], BF16, "wpqb")
                for k in range(8):
                    load_cast(wpqb[:, k, :], wpqb, w_pq[k * 128:(k + 1) * 128, :], D)
                kTb = sb(PE_, [128, 128], BF16, "kTb")
                load_cast(kTb[:], kTb, kT, 128)
                g2bc = sb(PE_, [128, D], F32, "g2bc")
                gfbc = sb(PE_, [128, D], F32, "gfbc")
                build_gate_bc(m, 40, g2bc)
                S.dma('sp', gfbc[:], g_final.partition_broadcast(128), W=bs(gfbc))
                x1t = [sb(PE_, [128, D], F32, f"x1t{i}") for i in range(TG)]
                xh = sb(PE_, [128, D], F32, "xhE")
                junk = xh
                h2T = sb(PE_, [128, 8, TT], BF16, "h2T")
                pqT = sb(PE_, [128, 8, 128], BF16, "pqT")
                s1 = [sb(PE_, [128, 8, 128], F32, f"s1_{i}") for i in range(TG)]
                s2 = [sb(PE_, [128, 8, 128], F32, f"s2_{i}") for i in range(TG)]
                w1 = sb(PE_, [128, 8, 128], F32, "w1")
                v1 = sb(PE_, [128, 8, 16], F32, "v1")
                v2 = sb(PE_, [128, 8, 16], F32, "v2")
                cand = sb(PE_, [128, 8, 256], F32, "cand")
                best = sb(PE_, [128, 8, 16], F32, "best")
                et = sb(PE_, [128, 8, 16], F32, "et")
                zs = [sb(PE_, [128, 32], F32, f"zs{i}") for i in range(TG)]
                Sg = [sb(PE_, [128, NI, 128], F32, f"Sg{i}") for i in range(4)]
                Ee = [sb(PE_, [128, EB], F32, f"Ee{i}") for i in range(2)]
                Gh2 = [[sb(PE_, [128, 8, EB], BF16, f"Gh{i}_{j}") for i in range(TG)] for j in range(2)]
                gAT = [sb(PE_, [128, NI * 128], BF16, f"gAT{i}") for i in range(2)]
                WTt = [sb(PE_, [128, 128], BF16, f"WT{i}") for i in range(4)]
                uch = [sb(PE_, [128, 8, EB], BF16, f"uch{i}") for i in range(2)]
                vch = [sb(PE_, [128, NI, D], BF16, f"vch{i}") for i in range(2)]
                po = sb(PE_, [128, D], F32, "po")
                rot = {'sg': 0, 'ee': 0, 'ga': 0, 'wt': 0, 'pa': 0, 'pg': 0, 'w': 0}
                PAb = [Buf(), Buf()]
                PGb = [Buf() for _ in range(4)]
                for g0 in range(0, len(tiles), TG):
                    grp_tiles = tiles[g0:g0 + TG]
                    for ti, tt in enumerate(grp_tiles):
                        r0 = b * SEQ + tt * 128
                        X = x1t[ti]
                        S.dma('sp', X[:], X1[r0:r0 + 128, :], W=bs(X))
                        rms_rstd(PE_, X[:], D, small[:, 3:4], junk[:], bs(X), bs(junk), bs(small))
                        S.op('dve', ts_(xh[:], X[:], small[:, 3:4], None, ALU.mult), R=bs(X, small), W=bs(xh))
                        for k in range(8):
                            pb = PS[k // 4]
                            S.op('pe', tr_(pb[:, (k % 4) * 128:(k % 4 + 1) * 128], xh[:, k * 128:(k + 1) * 128], ident_f[:]),
                                 R=bs(xh, ident_f), W=bs(pb))
                        for k in range(8):
                            pb = PS[k // 4]
                            src = pb[:, (k % 4) * 128:(k % 4 + 1) * 128]
                            S.op('act', act_(h2T[:, k, ti * 128:(ti + 1) * 128], src, AF.Identity, bias=m[:, 24 + k:25 + k],
                                             scale=m[:, 32 + k:33 + k]), R=bs(pb, m), W=bs(h2T))
                        for h in range(8):
                            pb = PS[2 + h // 4]
                            for k in range(8):
                                S.op('pe', mm_(pb[:, (h % 4) * 128:(h % 4 + 1) * 128], wpqb[:, k, h * 128:(h + 1) * 128],
                                               h2T[:, k, ti * 128:(ti + 1) * 128], k == 0, k == 7), R=bs(wpqb, h2T), W=bs(pb))
                        S.op('act', act_(pqT[:, 0:4, :].rearrange("p a t -> p (a t)"), PS[2][:], AF.Copy), R=bs(PS[2]), W=bs(pqT))
                        S.op('dve', cp_(pqT[:, 4:8, :].rearrange("p a t -> p (a t)"), PS[3][:]), R=bs(PS[3]), W=bs(pqT))
                        for h in range(8):
                            pa, pb2 = PS[4 + h // 4], PS[6 + h // 4]
                            S.op('pe', mm_(pa[:, (h % 4) * 128:(h % 4 + 1) * 128], pqT[0:64, h, :], kTb[0:64, :], True, True),
                                 R=bs(pqT, kTb), W=bs(pa))
                            S.op('pe', mm_(pb2[:, (h % 4) * 128:(h % 4 + 1) * 128], pqT[64:128, h, :], kTb[64:128, :], True, True),
                                 R=bs(pqT, kTb), W=bs(pb2))
                        s1t, s2t, zst = s1[ti], s2[ti], zs[ti]
                        S.op('act', act_(s1t[:, 0:4, :].rearrange("p a t -> p (a t)"), PS[4][:], AF.Copy), R=bs(PS[4]), W=bs(s1t))
                        S.op('dve', cp_(s1t[:, 4:8, :].rearrange("p a t -> p (a t)"), PS[5][:]), R=bs(PS[5]), W=bs(s1t))
                        S.op('act', act_(s2t[:, 0:4, :].rearrange("p a t -> p (a t)"), PS[6][:], AF.Copy), R=bs(PS[6]), W=bs(s2t))
                        S.op('dve', cp_(s2t[:, 4:8, :].rearrange("p a t -> p (a t)"), PS[7][:]), R=bs(PS[7]), W=bs(s2t))
                        for (sx, vx) in ((s1t, v1), (s2t, v2)):
                            for h in range(8):
                                S.op('dve', max_(vx[:, h, 0:8], sx[:, h, :]), R=bs(sx), W=bs(vx))
                                S.op('dve', mr_(w1[:, h, :], vx[:, h, 0:8], sx[:, h, :], NEGS), R=bs(sx, vx), W=bs(w1))
                                S.op('dve', max_(vx[:, h, 8:16], w1[:, h, :]), R=bs(w1), W=bs(vx))
                        for h in range(8):
                            S.op('dve', tt_(cand[:, h, :].rearrange("p (a c) -> p a c", a=16),
                                             v1[:, h, :].unsqueeze(2).to_broadcast([128, 16, 16]),
                                             v2[:, h, :].unsqueeze(1).to_broadcast([128, 16, 16]), ALU.add), R=bs(v1, v2), W=bs(cand))
                        for h in range(8):
                            S.op('dve', max_(best[:, h, 0:8], cand[:, h, :]), R=bs(cand), W=bs(best))
                            S.op('dve', mr_(cand[:, h, :], best[:, h, 0:8], cand[:, h, :], NEGS), R=bs(best, cand), W=bs(cand))
                            S.op('dve', max_(best[:, h, 8:16], cand[:, h, :]), R=bs(cand), W=bs(best))
                        S.op('dve', tt_(et[:], best[:], best[:, :, 0:1].to_broadcast([128, 8, 16]), ALU.subtract), R=bs(best), W=bs(et))
                        S.op('act', act_(et[:], et[:], AF.Exp), R=bs(et), W=bs(et))
                        S.op('dve', red_(zst[:, 0:8], et[:], ALU.add), R=bs(et), W=bs(zst))
                        S.op('act', act_(zst[:, 8:16], zst[:, 0:8], AF.Ln), R=bs(zst), W=bs(zst))
                        S.op('dve', tt_(zst[:, 16:24], zst[:, 8:16], best[:, :, 0], ALU.add), R=bs(zst, best), W=bs(zst))
                        S.op('dve', ts_(zst[:, 16:24], zst[:, 16:24], -1.0, None, ALU.mult), R=bs(zst), W=bs(zst))
                        S.op('dve', cp_(zst[:, 24:32], best[:, :, 15]), R=bs(best), W=bs(zst))
                    ntg = len(grp_tiles)
                    NTT = ntg * 128
                    pend = []

                    def flush_pend(keep):
                        while len(pend) > keep:
                            fn_, R_, W_ = pend.pop(0)
                            S.op('dve', fn_, R=R_, W=W_)

                    def emit_gate_unit(ib, ti, h):
                        Gh = Gh2[ib % 2]
                        u_ = rot['sg']
                        rot['sg'] += 1
                        pX = PS[6 + u_ % 2]
                        in0 = s1[ti][:, h, ib * NI:(ib + 1) * NI].unsqueeze(2).to_broadcast([128, NI, 128])
                        in1 = s2[ti][:, h, :].unsqueeze(1).to_broadcast([128, NI, 128])
                        nb_ap = zs[ti][:, 16 + h:17 + h]
                        tau_ap = zs[ti][:, 24 + h:25 + h]
                        if u_ % 4 != 0:
                            sg_ = Sg[rot['w'] % 4]
                            rot['w'] += 1
                            S.op('pool', tt_(sg_[:], in0, in1, ALU.add), R=bs(s1[ti], s2[ti]), W=bs(sg_))
                            S.op('act', act_(pX[:], sg_[:].rearrange("p a c -> p (a c)"), AF.Exp, bias=nb_ap),
                                 R=bs(sg_, zs[ti]), W=bs(pX))
                            pend.append((stt_(Gh[ti][:, h, :], sg_[:].rearrange("p a c -> p (a c)"), tau_ap, pX[:],
                                              ALU.is_ge, ALU.mult), bs(sg_, zs[ti], pX), bs(Gh[ti])))
                        else:
                            ee_ = Ee[rot['ee'] % 2]
                            rot['ee'] += 1
                            S.op('dve', tt_(pX[:].rearrange("p (a c) -> p a c", a=NI), in0, in1, ALU.add),
                                 R=bs(s1[ti], s2[ti]), W=bs(pX))
                            S.op('act', act_(ee_[:], pX[:], AF.Exp, bias=nb_ap), R=bs(pX, zs[ti]), W=bs(ee_))
                            pend.append((stt_(Gh[ti][:, h, :], pX[:], tau_ap, ee_[:], ALU.is_ge, ALU.mult),
                                         bs(pX, zs[ti], ee_), bs(Gh[ti])))
                        flush_pend(1)

                    units = [(ti, h) for ti in range(ntg) for h in range(8)]
                    st = {}

                    def issue_loads(ib):
                        e0 = ib * EB
                        uc = uch[ib % 2]
                        vcb = vch[ib % 2]
                        S.dma('sp', uc[:], uTb[:, e0:e0 + EB].rearrange("(k p) n -> p k n", p=128), W=bs(uc))
                        S.dma('sp', vcb[:], pvb[e0:e0 + EB, :].rearrange("(a p) n -> p a n", p=128), W=bs(vcb))

                    def stage_A(ib):
                        uc = uch[ib % 2]
                        pAt = PS[2 + ib % 2]
                        for blk in range(NI):
                            bsl = slice(blk * 128, (blk + 1) * 128)
                            for k in range(8):
                                S.op('pe', mm_(pAt[:, blk * 128:(blk + 1) * 128], uc[:, k, bsl], h2T[:, k, 0:128], k == 0, k == 7),
                                     R=bs(uc, h2T), W=bs(pAt))
                        ga = gAT[ib % 2]
                        S.op('act', act_(ga[:], pAt[:, 0:NI * 128], AF.Gelu), R=bs(pAt), W=bs(ga))

                    def stage_G(ib, blk):
                        Gh = Gh2[ib % 2]
                        bsl = slice(blk * 128, (blk + 1) * 128)
                        ga = gAT[ib % 2]
                        pGt = PS[4 + rot['pg'] % 2]
                        rot['pg'] += 1
                        pG = pGt[:, 0:128]
                        for h in range(8):
                            S.op('pe', mm_(pG, Gh[0][:, h, bsl], ident_b[:], h == 0, h == 7), R=bs(Gh[0], ident_b), W=bs(pGt))
                        wt_ = WTt[rot['wt'] % 4]
                        rot['wt'] += 1
                        S.op('dve', tt_(wt_[:], pG, ga[:, bsl], ALU.mult), R=bs(pGt, ga), W=bs(wt_))
                        st[(ib, blk)] = wt_

                    def stage_O(ib, blk):
                        vcb = vch[ib % 2]
                        first = (ib == 0 and blk == 0)
                        last = (ib == nblk - 1 and blk == NI - 1)
                        wt_ = st.pop((ib, blk))
                        for dh in range(2):
                            S.op('pe', mm_(PS[dh][:], wt_[:], vcb[:, blk, dh * 512:(dh + 1) * 512], first, last),
                                 R=bs(wt_, vcb), W=bs(PS[dh]))

                    assert TG == 1
                    for (ti, h) in units:
                        emit_gate_unit(0, ti, h)
                    issue_loads(0)
                    stage_A(0)
                    upb = (len(units) + NI - 1) // NI
                    prev = None
                    for ib in range(nblk):
                        for blk in range(NI):
                            if blk == 0:
                                flush_pend(0)
                            stage_G(ib, blk)
                            if prev is not None:
                                stage_O(*prev)
                            prev = (ib, blk)
                            if ib + 1 < nblk:
                                if blk == 0:
                                    issue_loads(ib + 1)
                                for (ti, h) in units[blk * upb:(blk + 1) * upb]:
                                    emit_gate_unit(ib + 1, ti, h)
                                if blk == 1:
                                    stage_A(ib + 1)
                    flush_pend(0)
                    stage_O(*prev)
                    for ti, tt in enumerate(grp_tiles):
                        r0 = b * SEQ + tt * 128
                        X = x1t[ti]
                        for dh in range(2):
                            sl = slice(dh * 512, (dh + 1) * 512)
                            S.op('dve', tt_(po[:, sl], PS[ti * 2 + dh][:], g2bc[:, sl], ALU.mult), R=bs(PS[ti * 2 + dh], g2bc), W=bs(po))
                        S.op('pool', tt_(po[:], po[:], X[:], ALU.add), R=bs(po, X), W=bs(po))
                        rms_rstd(PE_, po[:], D, small[:, 4:5], junk[:], bs(po), bs(junk), bs(small))
                        S.op('dve', stt_(po[:], po[:], small[:, 4:5], gfbc[:], ALU.mult, ALU.mult), R=bs(po, small, gfbc), W=bs(po))
                        S.dma('sp', y[r0:r0 + 128, :], po[:], R=bs(po))
            S.barrier()
        S.barrier()
        S.emit()
    return nc


def _prep_inputs(inp, core):
    f = np.float32
    bsl = slice(core * NB, (core + 1) * NB)
    c = np.asarray(inp['c'], f)[bsl]
    cT = np.ascontiguousarray(c.reshape(NB, 8, 128).transpose(2, 1, 0)).reshape(128, 16)
    pos = np.asarray(inp['positions'], np.int32)[bsl]
    posT = np.ascontiguousarray(pos.reshape(NB, NT, 128).transpose(0, 2, 1))
    gcat = np.concatenate([np.asarray(inp['g_out_a'], f)[0], np.asarray(inp['g_out_b'], f)[0]])
    gvec = np.concatenate([np.asarray(inp['g_mix'], f)[0].reshape(8, 128).T,
                           np.asarray(inp['g_ffn'], f)[0].reshape(8, 128).T,
                           gcat.reshape(8, 128).T], axis=1)
    kT = np.concatenate([np.asarray(inp['peer_keys1'], f)[0].T, np.asarray(inp['peer_keys2'], f)[0].T], axis=0)
    return {
        "x": np.ascontiguousarray(np.asarray(inp['x'], f)[bsl].reshape(NB * SEQ, D)),
        "cT": np.ascontiguousarray(cT),
        "posT": posT,
        "w_ada": np.ascontiguousarray(np.asarray(inp['w_ada'], f)[0]),
        "b_adaT": np.ascontiguousarray(np.asarray(inp['b_ada'], f)[0].reshape(48, 128).T),
        "gvec": np.ascontiguousarray(gvec),
        "g_final": np.ascontiguousarray(np.asarray(inp['g_final'], f).reshape(1, D)),
        "g_kv": np.ascontiguousarray(np.asarray(inp['g_kv'], f)[0].reshape(1, 128)),
        "b_forget": np.ascontiguousarray(np.asarray(inp['b_forget'], f)[0].reshape(1, 8)),
        "w_in": np.ascontiguousarray(np.asarray(inp['w_in'], f)[0]),
        "w_uk": np.ascontiguousarray(np.asarray(inp['w_uk'], f)[0].reshape(128, 384)),
        "w_uv": np.ascontiguousarray(np.asarray(inp['w_uv'], f)[0].reshape(128, 512)),
        "w_out": np.ascontiguousarray(np.asarray(inp['w_out'], f)[0]),
        "w_pq": np.ascontiguousarray(np.asarray(inp['w_peer_q'], f)[0]),
        "kT": np.ascontiguousarray(kT),
    }


def kernel(**inputs):
    f = np.float32
    nc = build_nc()
    uT = np.ascontiguousarray(np.asarray(inputs['peer_u'], f)[0].T)
    pv = np.ascontiguousarray(np.asarray(inputs['peer_v'], f)[0])
    in_maps = []
    for core in range(8):
        d = _prep_inputs(inputs, core)
        d["uT"] = uT
        d["pv"] = pv
        in_maps.append(d)
    res = run_bass_kernel_spmd(nc, in_maps, core_ids=list(range(8)))
    out = np.concatenate([np.asarray(r["y"], f).reshape(NB, SEQ, D) for r in res.results], axis=0)
    return out
```

```python
import math
import numpy as np
from contextlib import ExitStack
import concourse.bass as bass
import concourse.mybir as mybir
from concourse.bass_utils import run_bass_kernel_spmd

F32 = mybir.dt.float32
BF16 = mybir.dt.bfloat16
I32 = mybir.dt.int32
AF = mybir.ActivationFunctionType
ALU = mybir.AluOpType
AX = mybir.AxisListType

ENGS = ('pe', 'act', 'dve', 'pool', 'sp')
EPOCH = 30000
N_EPOCH = 12
N_DMA = 24

NEG = -30000.0
NEGS = -1.0e30
SEQ = 4096
NT = 32
NB = 2
D = 1024
NCOL = 2496
THETA = 500000.0


class Buf:
    __slots__ = ('w', 'r', 'name')

    def __init__(self, name=''):
        self.w = None
        self.r = []
        self.name = name


class Sched:
    def __init__(self, nc, ctx):
        self.nc = nc
        self.q = {e: [] for e in ENGS}
        self.sems = {}
        for e in ENGS:
            if e == 'sp':
                continue
            for k in range(N_EPOCH):
                self.sems[(e, k)] = ctx.enter_context(nc.semaphore(f"s_{e}{k}"))
        for k in range(N_DMA):
            self.sems[('dma', k)] = ctx.enter_context(nc.semaphore(f"s_dma{k}"))
        self.epoch = {e: 0 for e in ENGS}
        self.cnt = {e: 0 for e in ENGS}
        self.dcnt = [0] * N_DMA
        self.dnext = 0
        self.known = {e: {} for e in ENGS}
        self.nops = 0

    def _need(self, eng, deps):
        waits = []
        kn = self.known[eng]
        best = {}
        for (s, v) in deps:
            if best.get(s, 0) < v:
                best[s] = v
        for s, v in best.items():
            if kn.get(s, 0) < v:
                kn[s] = v
                waits.append((s, v))
        return waits

    @staticmethod
    def _deps(R, W):
        deps = []
        for b in R:
            if b.w is not None:
                deps.append(b.w)
        for b in W:
            if b.w is not None:
                deps.append(b.w)
            deps.extend(b.r)
        return deps

    def op(self, eng, fn, R=(), W=()):
        deps = self._deps(R, W)
        if eng == 'pe':
            deps = [d for d in deps if d[0][0] != 'pe']
        waits = self._need(eng, deps)
        if self.cnt[eng] >= EPOCH:
            self.epoch[eng] += 1
            self.cnt[eng] = 0
        self.cnt[eng] += 1
        tok = ((eng, self.epoch[eng]), self.cnt[eng])
        self.q[eng].append((waits, fn, tok[0], 1))
        for b in R:
            b.r.append(tok)
        for b in W:
            b.w = tok
            b.r = []
        self.nops += 1
        return tok

    def dma(self, q, out, in_, R=(), W=(), **kw):
        slot = self.dnext
        self.dnext = (self.dnext + 1) % N_DMA
        s = ('dma', slot)
        deps = self._deps(R, W)
        if self.dcnt[slot] > 0:
            deps.append((s, self.dcnt[slot]))
        waits = self._need(q, deps)
        self.dcnt[slot] += 16
        tok = (s, self.dcnt[slot])

        def fn(e, out=out, in_=in_, kw=kw):
            return e.dma_start(out=out, in_=in_, **kw)
        self.q[q].append((waits, fn, s, 16))
        for b in R:
            b.r.append(tok)
        for b in W:
            b.w = tok
            b.r = []
        self.nops += 1
        return tok

    def barrier(self):
        toks = []
        for e in ENGS:
            if e == 'sp':
                continue
            if self.cnt[e] > 0:
                toks.append(((e, self.epoch[e]), self.cnt[e]))
        for k in range(N_DMA):
            if self.dcnt[k] > 0:
                toks.append((('dma', k), self.dcnt[k]))
        for e in ENGS:
            waits = self._need(e, toks)
            if waits:
                self.q[e].append((waits, None, None, 0))

    def emit(self):
        nc = self.nc
        sems = self.sems
        qs = self.q

        def replay(e, lst):
            for (waits, fn, s, inc) in lst:
                for (ws, v) in waits:
                    e.wait_ge(sems[ws], v)
                if fn is None:
                    continue
                fn(e).then_inc(sems[s], inc)

        with nc.Block() as block:
            @block.tensor
            def _(e):
                replay(e, qs['pe'])

            @block.scalar
            def _(e):
                replay(e, qs['act'])

            @block.vector
            def _(e):
                replay(e, qs['dve'])

            @block.gpsimd
            def _(e):
                replay(e, qs['pool'])

            @block.sync
            def _(e):
                replay(e, qs['sp'])


class T:
    def __init__(self, t, name=''):
        self.t = t
        self.b = Buf(name)

    def __getitem__(self, k):
        return self.t[k]


def build_nc(stage=99, ntile_dbg=None, nb_dbg=None):
    nc = bass.Bass("TRN2", target_bir_lowering=False)

    def din(name, shape, dt):
        return nc.dram_tensor(name, shape, dt, kind="ExternalInput").ap()

    def dscr(name, shape, dt):
        return nc.dram_tensor(name, shape, dt, kind="Internal").ap()

    x = din("x", [NB * SEQ, D], F32)
    cT = din("cT", [128, 16], F32)
    posT = din("posT", [NB, 128, NT], I32)
    w_ada = din("w_ada", [D, 6 * D], F32)
    b_adaT = din("b_adaT", [128, 48], F32)
    gvec = din("gvec", [128, 24], F32)
    g_final = din("g_final", [1, D], F32)
    g_kv = din("g_kv", [1, 128], F32)
    b_forget = din("b_forget", [1, 8], F32)
    w_in = din("w_in", [D, NCOL], F32)
    w_uk = din("w_uk", [128, 384], F32)
    w_uv = din("w_uv", [128, 512], F32)
    w_out = din("w_out", [D, D], F32)
    w_pq = din("w_pq", [D, D], F32)
    kT = din("kT", [128, 128], F32)
    uT = din("uT", [D, 16384], F32)
    pv = din("pv", [16384, D], F32)
    y = nc.dram_tensor("y", [NB * SEQ, D], F32, kind="ExternalOutput").ap()

    QAT = dscr("QAT", [NB, 128, 4, SEQ], BF16)
    KATs = dscr("KATs", [NB, 128, 4, SEQ], BF16)
    VAs = dscr("VAs", [NB, 128, NT, 512], BF16)
    IQT = dscr("IQT", [NB, 128, 3, SEQ], BF16)
    IKT = dscr("IKT", [NB, 128, SEQ], BF16)
    SGN = dscr("SGN", [NB, 128, NT, 8], F32)
    QBT = dscr("QBT", [NB, 128, 4, SEQ], BF16)
    KBTs = dscr("KBTs", [NB, 128, 4, SEQ], BF16)
    VBs = dscr("VBs", [NB, 128, NT, 512], BF16)
    YA = dscr("YA", [NB * SEQ, D], F32)
    X1 = dscr("X1", [NB * SEQ, D], F32)
    NCUM = dscr("NCUM", [NB, 8, SEQ], F32)
    uTb = dscr("uTb", [D, 16384], BF16)
    pvb = dscr("pvb", [16384, D], BF16)
    DBG = {}
    dbg_stage = stage

    with ExitStack() as ctx:
        S = Sched(nc, ctx)
        counter = [0]

        def sb(ctxx, shape, dt, name=None):
            counter[0] += 1
            nm = f"{name or 't'}_{counter[0]}"
            return T(ctxx.enter_context(nc.sbuf_tensor(nm, shape, dt)), nm)

        def psb(ctxx, shape, dt, name=None):
            counter[0] += 1
            nm = f"{name or 'p'}_{counter[0]}"
            return T(ctxx.enter_context(nc.psum_tensor(nm, shape, dt)), nm)

        def bs(*ts):
            return [t.b for t in ts]

        def ts_(out, in0, s1, s2, op0, op1=None, accum_out=None):
            if op1 is None:
                return lambda e: e.tensor_scalar(out=out, in0=in0, scalar1=s1, scalar2=None, op0=op0)
            if accum_out is not None:
                return lambda e: e.tensor_scalar(out=out, in0=in0, scalar1=s1, scalar2=s2, op0=op0, op1=op1,
                                                 accum_out=accum_out)
            return lambda e: e.tensor_scalar(out=out, in0=in0, scalar1=s1, scalar2=s2, op0=op0, op1=op1)

        def tt_(out, in0, in1, op):
            return lambda e: e.tensor_tensor(out=out, in0=in0, in1=in1, op=op)

        def stt_(out, in0, scalar, in1, op0, op1):
            return lambda e: e.scalar_tensor_tensor(out=out, in0=in0, scalar=scalar, in1=in1, op0=op0, op1=op1)

        def cp_(out, in_):
            return lambda e: e.tensor_copy(out=out, in_=in_)

        def act_(out, in_, func, bias=None, scale=None, accum_out=None):
            kw = {}
            if bias is not None:
                kw['bias'] = bias
            if scale is not None:
                kw['scale'] = scale
            if accum_out is not None:
                kw['accum_out'] = accum_out
            return lambda e: e.activation(out=out, in_=in_, func=func, **kw)

        def max_(out, in_):
            return lambda e: e.max(out=out, in_=in_)

        def mr_(out, repl, vals, imm):
            return lambda e: e.match_replace(out=out, in_to_replace=repl, in_values=vals, imm_value=imm)

        def red_(out, in_, op):
            return lambda e: e.tensor_reduce(out=out, in_=in_, axis=AX.X, op=op)

        def recip_(out, in_):
            return lambda e: e.reciprocal(out=out, in_=in_)

        def memset_(ap, v):
            return lambda e: e.memset(ap, v)

        def mm_(out, lhsT, rhs, start, stop, sgc=False):
            if sgc:
                return lambda e: e.matmul(out, lhsT=lhsT, rhs=rhs, start=start, stop=stop, skip_group_check=True)
            return lambda e: e.matmul(out, lhsT=lhsT, rhs=rhs, start=start, stop=stop)

        def tr_(out, in_, ident):
            return lambda e: e.transpose(out=out, in_=in_, identity=ident)

        G = ctx
        idi = sb(G, [128, 128], I32)
        pidi = sb(G, [128, 1], I32)
        idf = sb(G, [128, 128], F32)
        pidf = sb(G, [128, 1], F32)
        ident_f = sb(G, [128, 128], F32, "ident_f")
        ident_b = sb(G, [128, 128], BF16, "ident_b")
        cmaskL = sb(G, [128, 128], F32, "cmaskL")
        cmaskS = sb(G, [128, 128], F32, "cmaskS")
        tri_f = sb(G, [128, 128], F32, "tri_f")
        ones_f = sb(G, [128, 128], F32, "ones_f")
        invf = sb(G, [128, 24], F32, "invf")
        cact = sb(G, [128, 16], F32, "cact")
        badaT = sb(G, [128, 48], F32, "badaT")
        gv = sb(G, [128, 24], F32, "gv")
        mod = sb(G, [128, 48, 2], F32, "mod")
        mv = [sb(G, [128, 48], F32, f"mv{b}") for b in range(NB)]
        gkvbc = sb(G, [128, 128], F32, "gkvbc")
        bfbc = sb(G, [128, 8], F32, "bfbc")
        lsall = sb(G, [128, NT, 8], F32, "lsall")
        dgt = sb(G, [128, 128], F32, "dgt")
        rt = sb(G, [128, NT, 24], F32, "rt")
        small = sb(G, [128, 64], F32, "small")
        stgf = sb(G, [128, 1024], F32, "stgf")
        castrot = [0]

        def load_cast(dst_ap, dst_t, src_ap, n):
            S.dma('sp', stgf[:, 0:n], src_ap, W=bs(stgf))
            castrot[0] += 1
            if castrot[0] % 2 == 0:
                S.op('act', act_(dst_ap, stgf[:, 0:n], AF.Copy), R=bs(stgf), W=bs(dst_t))
            else:
                S.op('dve', cp_(dst_ap, stgf[:, 0:n]), R=bs(stgf), W=bs(dst_t))

        PS = [psb(G, [128, 512], F32, f"PS{i}") for i in range(8)]

        def psbf(i):
            return PS[i][:].bitcast(BF16)

        S.op('pool', lambda e: e.iota(idi[:], pattern=[[1, 128]], base=0, channel_multiplier=0), W=bs(idi))
        S.op('pool', lambda e: e.iota(pidi[:], pattern=[[0, 1]], base=0, channel_multiplier=1), W=bs(pidi))
        S.op('dve', cp_(idf[:], idi[:]), R=bs(idi), W=bs(idf))
        S.op('dve', cp_(pidf[:], pidi[:]), R=bs(pidi), W=bs(pidf))
        S.op('dve', ts_(ident_f[:], idf[:], pidf[:, 0:1], None, ALU.is_equal), R=bs(idf, pidf), W=bs(ident_f))
        S.op('dve', cp_(ident_b[:], ident_f[:]), R=bs(ident_f), W=bs(ident_b))
        S.op('dve', ts_(cmaskL[:], idf[:], pidf[:, 0:1], NEG, ALU.is_gt, ALU.mult), R=bs(idf, pidf), W=bs(cmaskL))
        S.op('dve', ts_(cmaskS[:], idf[:], pidf[:, 0:1], NEGS, ALU.is_gt, ALU.mult), R=bs(idf, pidf), W=bs(cmaskS))
        S.op('dve', ts_(tri_f[:], idf[:], pidf[:, 0:1], None, ALU.is_ge), R=bs(idf, pidf), W=bs(tri_f))
        S.op('dve', memset_(ones_f[:], 1.0), W=bs(ones_f))
        fr = [THETA ** (-(2.0 * k) / 16.0) for k in range(8)] + [THETA ** (-(2.0 * k) / 8.0) for k in range(4)]
        for rep in range(2):
            for k in range(12):
                S.op('dve', (lambda e, c=rep * 12 + k, v=float(np.float32(fr[k])): e.memset(invf[:, c:c + 1], v)),
                     W=bs(invf))
        S.dma('sp', cact[:], cT, W=bs(cact))
        S.dma('sp', badaT[:], b_adaT, W=bs(badaT))
        S.dma('sp', gv[:], gvec, W=bs(gv))
        S.dma('sp', gkvbc[:], g_kv.partition_broadcast(128), W=bs(gkvbc))
        S.dma('sp', bfbc[:], b_forget.partition_broadcast(128), W=bs(bfbc))
        S.op('act', act_(cact[:], cact[:], AF.Silu), R=bs(cact), W=bs(cact))

        with ExitStack() as P0:
            wada = [sb(P0, [128, 8, D], F32, f"wada{i}") for i in range(2)]
            for g in range(6):
                wt_ = wada[g % 2]
                S.dma('sp', wt_[:], w_ada[:, g * D:(g + 1) * D].rearrange("(k p) n -> p k n", p=128), W=bs(wt_))
                for jc in range(8):
                    j = g * 8 + jc
                    for k in range(8):
                        S.op('pe', mm_(PS[0][:, j * 2:(j + 1) * 2], wt_[:, k, jc * 128:(jc + 1) * 128],
                                       cact[:, k * 2:(k + 1) * 2], k == 0, k == 7), R=bs(wt_, cact), W=bs(PS[0]))
            S.op('dve', tt_(mod[:], PS[0][:, 0:96].rearrange("p (j b) -> p j b", b=2),
                            badaT[:].unsqueeze(2).to_broadcast([128, 48, 2]), ALU.add), R=bs(PS[0], badaT), W=bs(mod))
            for b in range(NB):
                m = mv[b]
                S.op('dve', cp_(m[:, 0:8], mod[:, 0:8, b]), R=bs(mod), W=bs(m))
                S.op('dve', stt_(m[:, 8:16], mod[:, 8:16, b], 1.0, gv[:, 0:8], ALU.add, ALU.mult), R=bs(mod, gv), W=bs(m))
                S.op('dve', cp_(m[:, 16:24], mod[:, 16:24, b]), R=bs(mod), W=bs(m))
                S.op('dve', cp_(m[:, 24:32], mod[:, 24:32, b]), R=bs(mod), W=bs(m))
                S.op('dve', stt_(m[:, 32:40], mod[:, 32:40, b], 1.0, gv[:, 8:16], ALU.add, ALU.mult), R=bs(mod, gv), W=bs(m))
                S.op('dve', cp_(m[:, 40:48], mod[:, 40:48, b]), R=bs(mod), W=bs(m))
        S.barrier()

        if stage >= 4:
            with ExitStack() as P0:
                CW = 8192
                cf = [sb(P0, [128, CW], F32, f"cf{i}") for i in range(2)]
                cb = [sb(P0, [128, CW], BF16, f"cb{i}") for i in range(2)]
                i = 0
                for (src, dst) in ((uT, uTb), (pv, pvb)):
                    sv = src.rearrange("(p a) n -> p (a n)", p=128)
                    dv = dst.rearrange("(p a) n -> p (a n)", p=128)
                    tot = sv.shape[1]
                    for c in range(tot // CW):
                        f_, t_ = cf[i % 2], cb[i % 2]
                        S.dma('sp', f_[:], sv[:, c * CW:(c + 1) * CW], W=bs(f_))
                        if i % 3 == 0:
                            S.op('act', act_(t_[:], f_[:], AF.Copy), R=bs(f_), W=bs(t_))
                        elif i % 3 == 1:
                            S.op('dve', cp_(t_[:], f_[:]), R=bs(f_), W=bs(t_))
                        else:
                            S.op('pool', cp_(t_[:], f_[:]), R=bs(f_), W=bs(t_))
                        S.dma('sp', dv[:, c * CW:(c + 1) * CW], t_[:], R=bs(t_))
                        i += 1
            S.barrier()

        def build_gate_bc(m, src0, dst):
            for k in range(8):
                S.op('dve', ts_(dgt[:], ident_f[:], m[:, src0 + k:src0 + k + 1], None, ALU.mult),
                     R=bs(ident_f, m), W=bs(dgt))
                pb = PS[1 + k // 4]
                S.op('pe', mm_(pb[:, (k % 4) * 128:(k % 4 + 1) * 128], ones_f[:], dgt[:], True, True),
                     R=bs(ones_f, dgt), W=bs(pb))
            S.op('act', act_(dst[:, 0:512], PS[1][:], AF.Copy), R=bs(PS[1]), W=bs(dst))
            S.op('act', act_(dst[:, 512:1024], PS[2][:], AF.Copy), R=bs(PS[2]), W=bs(dst))

        def rms_rstd(ctxx, src_ap, n, rstd_ap, junk_ap, src_b, junk_b, sm_b, eps=1e-6):
            S.op('act', act_(junk_ap, src_ap, AF.Square, accum_out=small[:, 60:61]), R=src_b, W=junk_b + bs(small))
            S.op('dve', ts_(small[:, 61:62], small[:, 60:61], 1.0 / n, eps, ALU.mult, ALU.add), R=bs(small), W=bs(small))
            S.op('act', act_(small[:, 62:63], small[:, 61:62], AF.Sqrt), R=bs(small), W=bs(small))
            S.op('dve', recip_(rstd_ap, small[:, 62:63]), R=bs(small), W=sm_b)

        tiles = list(range(NT)) if ntile_dbg is None else list(range(ntile_dbg))
        for b in range(NB if ntile_dbg is None else (nb_dbg or 1)):
            m = mv[b]
            with ExitStack() as P0:
                posi = sb(P0, [128, NT], I32)
                posf = sb(P0, [128, NT], F32)
                ang = sb(P0, [128, NT, 24], F32)
                kf = sb(P0, [128, NT, 24], F32)
                ki = sb(P0, [128, NT, 24], I32)
                S.dma('sp', posi[:], posT[b], W=bs(posi))
                S.op('dve', cp_(posf[:], posi[:]), R=bs(posi), W=bs(posf))
                S.op('dve', tt_(ang[:], posf[:].unsqueeze(2).to_broadcast([128, NT, 24]),
                                invf[:].unsqueeze(1).to_broadcast([128, NT, 24]), ALU.mult), R=bs(posf, invf), W=bs(ang))
                S.op('dve', ts_(ang[:, :, 12:24], ang[:, :, 12:24], math.pi / 2, None, ALU.add), R=bs(ang), W=bs(ang))
                S.op('dve', ts_(kf[:], ang[:], 1.0 / (2 * math.pi), None, ALU.mult), R=bs(ang), W=bs(kf))
                S.op('dve', cp_(ki[:], kf[:]), R=bs(kf), W=bs(ki))
                S.op('dve', cp_(kf[:], ki[:]), R=bs(ki), W=bs(kf))
                C1 = 6.28125
                C2 = 2 * math.pi - C1
                S.op('dve', stt_(ang[:].rearrange("p a b -> p (a b)"), kf[:].rearrange("p a b -> p (a b)"), -C1,
                                 ang[:].rearrange("p a b -> p (a b)"), ALU.mult, ALU.add), R=bs(kf, ang), W=bs(ang))
                S.op('dve', stt_(ang[:].rearrange("p a b -> p (a b)"), kf[:].rearrange("p a b -> p (a b)"), -C2,
                                 ang[:].rearrange("p a b -> p (a b)"), ALU.mult, ALU.add), R=bs(kf, ang), W=bs(ang))
                S.op('dve', ts_(ang[:], ang[:], -math.pi, math.pi, ALU.max, ALU.min), R=bs(ang), W=bs(ang))
                S.op('act', act_(rt[:], ang[:], AF.Sin), R=bs(ang), W=bs(rt))
            S.barrier()

            with ExitStack() as PP:
                winb = sb(PP, [128, 8, NCOL], BF16, "winb")
                wukb = sb(PP, [128, 384], BF16, "wukb")
                wuvb = sb(PP, [128, 512], BF16, "wuvb")
                for k in range(8):
                    for (c0, c1) in ((0, 1024), (1024, 2048), (2048, NCOL)):
                        load_cast(winb[:, k, c0:c1], winb, w_in[k * 128:(k + 1) * 128, c0:c1], c1 - c0)
                load_cast(wukb[:], wukb, w_uk, 384)
                load_cast(wuvb[:], wuvb, w_uv, 512)
                xt = [sb(PP, [128, D], F32, f"xt{i}") for i in range(2)]
                junk = sb(PP, [128, D], F32, "junk")
                xh = sb(PP, [128, D], F32, "xh")
                hT = sb(PP, [128, 8, 128], BF16, "hT")
                pr = sb(PP, [128, NCOL], F32, "pr")
                qa_b = sb(PP, [128, 512], BF16, "qa_b")
                qb_b = sb(PP, [128, 512], BF16, "qb_b")
                kb_b = sb(PP, [128, 512], BF16, "kb_b")
                vb_b = sb(PP, [128, 512], BF16, "vb_b")
                va_b = sb(PP, [128, 512], BF16, "va_b")
                ka_b = sb(PP, [128, 8, 64], BF16, "ka_b")
                kr_b = sb(PP, [128, 16], BF16, "kr_b")
                iq_f = sb(PP, [128, 8, 32], F32, "iq_f")
                iq_b = sb(PP, [128, 3, 4, 32], BF16, "iq_b")
                S.op('dve', memset_(iq_b[:], 0.0), W=bs(iq_b))
                ik_b = sb(PP, [128, 4, 32], BF16, "ik_b")
                ik_f = sb(PP, [128, 32], F32, "ik_f")
                latn = sb(PP, [128, 128], BF16, "latn")
                latnT = sb(PP, [128, 128], BF16, "latnT")
                rtmp = [sb(PP, [128, 8, 8], F32, f"rtmp{i}") for i in range(4)]
                wabs = sb(PP, [128, 8], F32, "wabs")
                sgn_t = sb(PP, [128, 8], F32, "sgn_t")
                fz = sb(PP, [128, 8], F32, "fz")
                oT = sb(PP, [128, 4, 128], BF16, "oT")
                oT2 = sb(PP, [128, 4, 128], BF16, "oT2")
                oT3 = sb(PP, [128, 4, 128], BF16, "oT3")
                oT4 = sb(PP, [128, 4, 128], BF16, "oT4")
                ncs = sb(PP, [8, 512], F32, "ncs")

                def rope(dst3, src3, H, half, sinap, cosap, Rb, Wb):
                    cb_ = cosap.unsqueeze(1).to_broadcast([128, H, half])
                    sb_ = sinap.unsqueeze(1).to_broadcast([128, H, half])
                    x1 = src3[:, :, 0:half]
                    x2 = src3[:, :, half:2 * half]
                    t = [r[:, 0:H, 0:half] for r in rtmp]
                    S.op('dve', tt_(t[0], x1, cb_, ALU.mult), R=Rb + bs(rt), W=bs(rtmp[0]))
                    S.op('dve', tt_(t[1], x2, sb_, ALU.mult), R=Rb + bs(rt), W=bs(rtmp[1]))
                    S.op('dve', tt_(t[2], x2, cb_, ALU.mult), R=Rb + bs(rt), W=bs(rtmp[2]))
                    S.op('dve', tt_(t[3], x1, sb_, ALU.mult), R=Rb + bs(rt), W=bs(rtmp[3]))
                    S.op('dve', tt_(dst3[:, :, 0:half], t[0], t[1], ALU.subtract), R=bs(rtmp[0], rtmp[1]), W=Wb)
                    S.op('dve', tt_(dst3[:, :, half:2 * half], t[2], t[3], ALU.add), R=bs(rtmp[2], rtmp[3]), W=Wb)

                for tt in tiles:
                    r0 = b * SEQ + tt * 128
                    X = xt[tt % 2]
                    S.dma('sp', X[:], x[r0:r0 + 128, :], W=bs(X))
                    rms_rstd(PP, X[:], D, small[:, 0:1], junk[:], bs(X), bs(junk), bs(small))
                    S.op('dve', ts_(xh[:], X[:], small[:, 0:1], None, ALU.mult), R=bs(X, small), W=bs(xh))
                    for k in range(8):
                        pb = PS[k // 4]
                        S.op('pe', tr_(pb[:, (k % 4) * 128:(k % 4 + 1) * 128], xh[:, k * 128:(k + 1) * 128], ident_f[:]),
                             R=bs(xh, ident_f), W=bs(pb))
                    for k in range(8):
                        pb = PS[k // 4]
                        src = pb[:, (k % 4) * 128:(k % 4 + 1) * 128]
                        if k // 4 == 0:
                            S.op('dve', ts_(hT[:, k, :], src, m[:, 8 + k:9 + k], m[:, k:k + 1], ALU.mult, ALU.add),
                                 R=bs(pb, m), W=bs(hT))
                        else:
                            S.op('act', act_(hT[:, k, :], src, AF.Identity, bias=m[:, k:k + 1], scale=m[:, 8 + k:9 + k]),
                                 R=bs(pb, m), W=bs(hT))
                    cols = [(0, 512), (512, 1024), (1024, 1536), (1536, 2048), (2048, NCOL)]
                    for ci, (c0, c1) in enumerate(cols):
                        pb = PS[2 + ci]
                        for k in range(8):
                            S.op('pe', mm_(pb[:, 0:c1 - c0], hT[:, k, :], winb[:, k, c0:c1], k == 0, k == 7),
                                 R=bs(hT, winb), W=bs(pb))
                        if ci % 2 == 0:
                            S.op('act', act_(pr[:, c0:c1], pb[:, 0:c1 - c0], AF.Copy), R=bs(pb), W=bs(pr))
                        else:
                            S.op('dve', cp_(pr[:, c0:c1], pb[:, 0:c1 - c0]), R=bs(pb), W=bs(pr))
                    sinA, sinI = rt[:, tt, 0:8], rt[:, tt, 8:12]
                    cosA, cosI = rt[:, tt, 12:20], rt[:, tt, 20:24]
                    S.op('act', act_(qa_b[:], pr[:, 0:512], AF.Copy), R=bs(pr), W=bs(qa_b))
                    rope(qa_b[:].rearrange("p (h d) -> p h d", h=8), pr[:, 0:512].rearrange("p (h d) -> p h d", h=8),
                         8, 8, sinA, cosA, bs(pr), bs(qa_b))
                    rope(kr_b[:].unsqueeze(1), pr[:, 640:656].unsqueeze(1), 1, 8, sinA, cosA, bs(pr), bs(kr_b))
                    rms_rstd(PP, pr[:, 512:640], 128, small[:, 1:2], junk[:, 0:128], bs(pr), bs(junk), bs(small))
                    S.op('dve', stt_(latn[:], pr[:, 512:640], small[:, 1:2], gkvbc[:], ALU.mult, ALU.mult),
                         R=bs(pr, small, gkvbc), W=bs(latn))
                    S.op('pe', tr_(psbf(7)[:, 0:128], latn[:], ident_b[:]), R=bs(latn, ident_b), W=bs(PS[7]))
                    S.op('act', act_(latnT[:], psbf(7)[:, 0:128], AF.Copy), R=bs(PS[7]), W=bs(latnT))
                    S.op('pe', mm_(PS[0][:, 0:384], latnT[:], wukb[:], True, True), R=bs(latnT, wukb), W=bs(PS[0]))
                    S.op('pe', mm_(PS[1][:, 0:512], latnT[:], wuvb[:], True, True), R=bs(latnT, wuvb), W=bs(PS[1]))
                    S.op('act', act_(ka_b[:, :, 16:64], PS[0][:, 0:384].rearrange("p (h d) -> p h d", h=8), AF.Copy),
                         R=bs(PS[0]), W=bs(ka_b))
                    S.op('dve', cp_(ka_b[:, :, 0:16], kr_b[:].unsqueeze(1).to_broadcast([128, 8, 16])),
                         R=bs(kr_b), W=bs(ka_b))
                    S.op('dve', cp_(va_b[:], PS[1][:, 0:512]), R=bs(PS[1]), W=bs(va_b))
                    S.dma('sp', VAs[b, :, tt, :], va_b[:], R=bs(va_b))
                    S.op('act', act_(wabs[:], pr[:, 944:952], AF.Abs, scale=(8.0 ** -0.5) * (32.0 ** -0.5)),
                         R=bs(pr), W=bs(wabs))
                    S.op('dve', ts_(sgn_t[:], pr[:, 944:952], 0.0, 2.0, ALU.is_ge, ALU.mult), R=bs(pr), W=bs(sgn_t))
                    S.op('dve', ts_(sgn_t[:], sgn_t[:], -1.0, None, ALU.add), R=bs(sgn_t), W=bs(sgn_t))
                    S.dma('sp', SGN[b, :, tt, :], sgn_t[:], R=bs(sgn_t))
                    S.op('dve', cp_(iq_f[:], pr[:, 656:912].rearrange("p (h d) -> p h d", h=8)), R=bs(pr), W=bs(iq_f))
                    rope(iq_f[:], pr[:, 656:912].rearrange("p (h d) -> p h d", h=8), 8, 4, sinI, cosI, bs(pr), bs(iq_f))
                    for k3 in range(3):
                        nh = 3 if k3 < 2 else 2
                        S.op('dve', tt_(iq_b[:, k3, 0:nh, :], iq_f[:, 3 * k3:3 * k3 + nh, :],
                                        wabs[:, 3 * k3:3 * k3 + nh].unsqueeze(2).to_broadcast([128, nh, 32]), ALU.mult),
                             R=bs(iq_f, wabs), W=bs(iq_b))
                    S.op('dve', cp_(ik_f[:], pr[:, 912:944]), R=bs(pr), W=bs(ik_f))
                    rope(ik_f[:].unsqueeze(1), pr[:, 912:944].unsqueeze(1), 1, 4, sinI, cosI, bs(pr), bs(ik_f))
                    S.op('dve', cp_(ik_b[:], ik_f[:].unsqueeze(1).to_broadcast([128, 4, 32])), R=bs(ik_f), W=bs(ik_b))
                    S.op('act', act_(qb_b[:], pr[:, 952:1464], AF.Copy), R=bs(pr), W=bs(qb_b))
                    S.op('dve', cp_(kb_b[:], pr[:, 1464:1976]), R=bs(pr), W=bs(kb_b))
                    S.op('act', act_(vb_b[:], pr[:, 1976:2488], AF.Copy), R=bs(pr), W=bs(vb_b))
                    S.dma('sp', VBs[b, :, tt, :], vb_b[:], R=bs(vb_b))
                    S.op('dve', tt_(fz[:], pr[:, 2488:2496], bfbc[:], ALU.add), R=bs(pr, bfbc), W=bs(fz))
                    S.op('act', act_(fz[:], fz[:], AF.Exp, scale=-1.0), R=bs(fz), W=bs(fz))
                    S.op('act', act_(lsall[:, tt, :], fz[:], AF.Ln, bias=1.0), R=bs(fz), W=bs(lsall))
                    jobs = [(qa_b, 4, QAT[b], oT, 0), (ka_b, 4, KATs[b], oT2, 1), (qb_b, 4, QBT[b], oT3, 2),
                            (kb_b, 4, KBTs[b], oT4, 3)]
                    for (src, nblk, dst, stg, pbi) in jobs:
                        sv = src[:] if len(src.t.shape) == 2 else src[:].rearrange("p h d -> p (h d)")
                        pbb = psbf(pbi)
                        for k in range(nblk):
                            S.op('pe', tr_(pbb[:, k * 128:(k + 1) * 128], sv[:, k * 128:(k + 1) * 128], ident_b[:]),
                                 R=bs(src, ident_b), W=bs(PS[pbi]))
                        eng = 'act' if pbi % 2 == 0 else 'dve'
                        if eng == 'act':
                            S.op('act', act_(stg[:].rearrange("p a t -> p (a t)"), pbb[:, 0:512], AF.Copy), R=bs(PS[pbi]), W=bs(stg))
                        else:
                            S.op('dve', cp_(stg[:].rearrange("p a t -> p (a t)"), pbb[:, 0:512]), R=bs(PS[pbi]), W=bs(stg))
                        S.dma('sp', dst[:, :, tt * 128:(tt + 1) * 128], stg[:], R=bs(stg))
                    pbb = psbf(4)
                    for k in range(3):
                        S.op('pe', tr_(pbb[:, k * 128:(k + 1) * 128], iq_b[:, k, :, :].rearrange("p a d -> p (a d)"), ident_b[:]),
                             R=bs(iq_b, ident_b), W=bs(PS[4]))
                    S.op('pe', tr_(pbb[:, 384:512], ik_b[:].rearrange("p a d -> p (a d)"), ident_b[:]),
                         R=bs(ik_b, ident_b), W=bs(PS[4]))
                    S.op('act', act_(oT[:].rearrange("p a t -> p (a t)"), pbb[:, 0:512], AF.Copy), R=bs(PS[4]), W=bs(oT))
                    S.dma('sp', IQT[b, :, :, tt * 128:(tt + 1) * 128], oT[:, 0:3, :], R=bs(oT))
                    S.dma('sp', IKT[b, :, tt * 128:(tt + 1) * 128], oT[:, 3, :], R=bs(oT))
                ntl = len(tiles)
                for T0 in range(0, ntl, 4):
                    pb = PS[5 + (T0 // 4) % 2]
                    for Tt in range(T0, min(T0 + 4, ntl)):
                        oc = (Tt - T0) * 128
                        for J in range(Tt + 1):
                            S.op('pe', mm_(pb[0:8, oc:oc + 128], lsall[:, J, :], (tri_f if J == Tt else ones_f)[:],
                                           J == 0, J == Tt), R=bs(lsall, tri_f, ones_f), W=bs(pb))
                    n = (min(T0 + 4, ntl) - T0) * 128
                    S.op('act', act_(ncs[0:8, 0:n], pb[0:8, 0:n], AF.Copy, scale=8.0), R=bs(pb), W=bs(ncs))
                    S.dma('sp', NCUM[b, :, T0 * 128:T0 * 128 + n], ncs[0:8, 0:n], R=bs(ncs))
            S.barrier()
            if dbg_stage == 1:
                continue

            for grp in (0, 1):
                with ExitStack() as PA:
                    KT = sb(PA, [128, 4, SEQ], BF16, "KT")
                    VV = sb(PA, [128, NT, 512], BF16, "VV")
                    L_all = len(tiles) * 128
                    for p4 in range(4):
                        S.dma('sp', KT[:, p4, 0:L_all], (KATs if grp == 0 else KBTs)[b, :, p4, 0:L_all], W=bs(KT))
                    S.dma('sp', VV[:, 0:len(tiles), :], (VAs if grp == 0 else VBs)[b, :, 0:len(tiles), :], W=bs(VV))
                    woutb = sb(PA, [128, 4, D], BF16, "woutb")
                    for k in range(4):
                        load_cast(woutb[:, k, :], woutb, w_out[grp * 512 + k * 128:grp * 512 + (k + 1) * 128, :], D)
                    QT = [sb(PA, [128, 4, 128], BF16, f"QT{i}") for i in range(2)]
                    lgs = [sb(PA, [128, SEQ], F32, f"lg{i}") for i in range(2)]
                    PT = [sb(PA, [128, 1024], BF16, f"PT{i}") for i in range(2)]
                    oall = sb(PA, [128, 512], F32, "oall")
                    onb = sb(PA, [128, 512], BF16, "onb")
                    mixT = sb(PA, [128, 4, 128], BF16, "mixT")
                    junk2 = sb(PA, [128, 512], F32, "junk2")
                    yt = sb(PA, [128, D], F32, "yt")
                    stat = sb(PA, [128, 32], F32, "stat")
                    if grp == 0:
                        IK = sb(PA, [128, SEQ], BF16, "IK")
                        S.dma('sp', IK[:, 0:L_all], IKT[b, :, 0:L_all], W=bs(IK))
                        IQ = [sb(PA, [128, 3, 128], BF16, f"IQ{i}") for i in range(2)]
                        sg = [sb(PA, [128, 8], F32, f"sg{i}") for i in range(2)]
                        sc = sb(PA, [128, SEQ], F32, "sc")
                        wk = lgs[1]
                        Pm1 = sb(PA, [128, SEQ], BF16, "Pm1")
                        Pms = [(sc[:].bitcast(BF16), sc), (Pm1[:], Pm1)]
                        mb = sb(PA, [128, SEQ], BF16, "mb")
                        rl = [sb(PA, [128, 512], F32, f"rl{i}") for i in range(2)]
                        m8 = sb(PA, [128, 8], F32, "m8")
                        bis = sb(PA, [128, 8], F32, "bis")
                        tau = sb(PA, [128, 1], F32, "tau")
                    else:
                        Pm0 = sb(PA, [128, SEQ], BF16, "Pm0")
                        Pm1 = sb(PA, [128, SEQ], BF16, "Pm1")
                        Pms = [(Pm0[:], Pm0), (Pm1[:], Pm1)]
                        fbts = [sb(PA, [128, SEQ], F32, f"fbt{i}") for i in range(2)]
                        xres = sb(PA, [128, D], F32, "xres")
                        yat = sb(PA, [128, D], F32, "yat")
                        g1bc = sb(PA, [128, D], F32, "g1bc")
                        build_gate_bc(m, 16, g1bc)
                    rotA = [0]
                    rotB = [0]
                    rotT = [0]
                    statb = [Buf() for _ in range(8)]

                    def nextA():
                        rotA[0] = (rotA[0] + 1) % 3
                        return 1 + rotA[0]

                    def nextB():
                        rotB[0] = (rotB[0] + 1) % 4
                        return 4 + rotB[0]

                    for qt in tiles:
                        r0 = b * SEQ + qt * 128
                        L = (qt + 1) * 128
                        nch = (L + 511) // 512
                        Q = QT[qt % 2]
                        S.dma('sp', Q[:], (QAT if grp == 0 else QBT)[b, :, :, qt * 128:(qt + 1) * 128], W=bs(Q))
                        if grp == 0:
                            iq = IQ[qt % 2]
                            sgt = sg[qt % 2]
                            S.dma('sp', iq[:], IQT[b, :, :, qt * 128:(qt + 1) * 128], W=bs(iq))
                            S.dma('sp', sgt[:], SGN[b, :, qt, :], W=bs(sgt))
                            for h in range(8):
                                pr0 = 32 * (h % 3)
                                for c in range(nch):
                                    c0, c1 = c * 512, min(L, (c + 1) * 512)
                                    pb = PS[nextB()]
                                    S.op('pe', mm_(pb[:, 0:c1 - c0], iq[pr0:pr0 + 32, h // 3, :], IK[pr0:pr0 + 32, c0:c1],
                                                   True, True), R=bs(iq, IK), W=bs(pb))
                                    r_ = rl[(h + c) % 2]
                                    S.op('act', act_(r_[:, 0:c1 - c0], pb[:, 0:c1 - c0], AF.Relu), R=bs(pb), W=bs(r_))
                                    if h == 0:
                                        S.op('dve', ts_(sc[:, c0:c1], r_[:, 0:c1 - c0], sgt[:, 0:1], None, ALU.mult),
                                             R=bs(r_, sgt), W=bs(sc))
                                    else:
                                        S.op('dve', stt_(sc[:, c0:c1], r_[:, 0:c1 - c0], sgt[:, h:h + 1], sc[:, c0:c1],
                                                         ALU.mult, ALU.add), R=bs(r_, sgt, sc), W=bs(sc))
                            if qt >= 2:
                                S.op('dve', red_(bis[:, 5:6], sc[:, 0:L], ALU.max), R=bs(sc), W=bs(bis))
                                S.op('dve', red_(bis[:, 0:1], sc[:, 0:L], ALU.min), R=bs(sc), W=bs(bis))
                                S.op('dve', tt_(bis[:, 1:2], bis[:, 5:6], bis[:, 0:1], ALU.subtract), R=bs(bis), W=bs(bis))
                            S.op('dve', tt_(sc[:, L - 128:L], sc[:, L - 128:L], cmaskS[:], ALU.add), R=bs(sc, cmaskS), W=bs(sc))
                            if qt >= 2:
                                NBIS = 22
                                for k in range(NBIS):
                                    stp = 2.0 ** -(k + 1)
                                    S.op('dve', stt_(bis[:, 2:3], bis[:, 1:2], stp, bis[:, 0:1], ALU.mult, ALU.add), R=bs(bis), W=bs(bis))
                                    S.op('dve', ts_(wk[:, 0:L], sc[:, 0:L], bis[:, 2:3], 0.0, ALU.is_ge, ALU.add, accum_out=bis[:, 3:4]),
                                         R=bs(sc, bis), W=bs(wk, bis))
                                    S.op('dve', ts_(bis[:, 4:5], bis[:, 3:4], 255.5, stp, ALU.is_ge, ALU.mult), R=bs(bis), W=bs(bis))
                                    S.op('dve', stt_(bis[:, 0:1], bis[:, 4:5], bis[:, 1:2], bis[:, 0:1], ALU.mult, ALU.add), R=bs(bis), W=bs(bis))
                                S.op('dve', ts_(tau[:], bis[:, 0:1], -1.0e29, None, ALU.max), R=bs(bis), W=bs(tau))
                            else:
                                S.op('dve', memset_(tau[:], -1.0e29), W=bs(tau))
                            S.op('dve', ts_(mb[:, 0:L], sc[:, 0:L], tau[:, 0:1], NEG, ALU.is_lt, ALU.mult), R=bs(sc, tau), W=bs(mb))
                        def head_S1(h):
                            p4, hf = h // 2, (h % 2) * 64
                            lg = lgs[h % 2]
                            if grp == 1:
                                fbt = fbts[h % 2]
                                S.dma('sp', fbt[:, 0:L], NCUM[b, h:h + 1, 0:L].partition_broadcast(128), W=bs(fbt))
                                bias_t = fbt
                            else:
                                bias_t = mb
                            for c in range(nch):
                                c0, c1 = c * 512, min(L, (c + 1) * 512)
                                pb = PS[nextB()]
                                S.op('pe', mm_(pb[:, 0:c1 - c0], Q[hf:hf + 64, p4, :], KT[hf:hf + 64, p4, c0:c1], True, True),
                                     R=bs(Q, KT), W=bs(pb))
                                S.op('dve', tt_(lg[:, c0:c1], pb[:, 0:c1 - c0], bias_t[:, c0:c1], ALU.add),
                                     R=bs(pb, bias_t), W=bs(lg))
                            if grp == 1:
                                S.op('pool', tt_(lg[:, L - 128:L], lg[:, L - 128:L], cmaskL[:], ALU.add), R=bs(lg, cmaskL), W=bs(lg))
                            S.op('dve', red_(stat[:, 16 + h:17 + h], lg[:, 0:L], ALU.max), R=bs(lg), W=[statb[h]])
                            S.op('dve', ts_(stat[:, 24 + h:25 + h], stat[:, 16 + h:17 + h], -0.125, None, ALU.mult), R=[statb[h]], W=[statb[h]])

                        def head_S2(h):
                            lg = lgs[h % 2]
                            Pm_ap, Pm_b = Pms[h % 2]
                            S.op('act', act_(Pm_ap[:, 0:L], lg[:, 0:L], AF.Exp, bias=stat[:, 24 + h:25 + h], scale=0.125,
                                             accum_out=stat[:, h:h + 1]), R=bs(lg) + [statb[h]], W=bs(Pm_b) + [statb[h]])
                            nkb = qt + 1
                            for g0 in range(0, nkb, 8):
                                g1 = min(nkb, g0 + 8)
                                pi = nextA()
                                pbb = psbf(pi)
                                for kb in range(g0, g1):
                                    S.op('pe', tr_(pbb[:, (kb - g0) * 128:(kb - g0 + 1) * 128], Pm_ap[:, kb * 128:(kb + 1) * 128], ident_b[:]),
                                         R=bs(Pm_b, ident_b), W=bs(PS[pi]))
                                rotT[0] += 1
                                ptt = PT[rotT[0] % 2]
                                n = (g1 - g0) * 128
                                if rotT[0] % 2 == 0:
                                    S.op('act', act_(ptt[:, 0:n], pbb[:, 0:n], AF.Copy), R=bs(PS[pi]), W=bs(ptt))
                                else:
                                    S.op('pool' if False else 'dve', cp_(ptt[:, 0:n], pbb[:, 0:n]), R=bs(PS[pi]), W=bs(ptt))
                                for kb in range(g0, g1):
                                    S.op('pe', mm_(PS[0][:, h * 64:(h + 1) * 64], ptt[:, (kb - g0) * 128:(kb - g0 + 1) * 128],
                                                   VV[:, kb, h * 64:(h + 1) * 64], kb == 0, kb == nkb - 1),
                                         R=bs(ptt, VV), W=bs(PS[0]))

                        head_S1(0)
                        for h in range(8):
                            if h + 1 < 8:
                                head_S1(h + 1)
                            head_S2(h)
                        S.op('dve', recip_(stat[:, 8:16], stat[:, 0:8]), R=statb, W=bs(stat))
                        S.op('dve', tt_(oall[:].rearrange("p (h d) -> p h d", h=8), PS[0][:].rearrange("p (h d) -> p h d", h=8),
                                        stat[:, 8:16].unsqueeze(2).to_broadcast([128, 8, 64]), ALU.mult), R=bs(PS[0], stat), W=bs(oall))
                        rms_rstd(PA, oall[:], 512, small[:, 2:3], junk2[:], bs(oall), bs(junk2), bs(small))
                        S.op('dve', ts_(onb[:], oall[:], small[:, 2:3], None, ALU.mult), R=bs(oall, small), W=bs(onb))
                        pbb = psbf(1)
                        for k in range(4):
                            S.op('pe', tr_(pbb[:, k * 128:(k + 1) * 128], onb[:, k * 128:(k + 1) * 128], ident_b[:]),
                                 R=bs(onb, ident_b), W=bs(PS[1]))
                        for k in range(4):
                            S.op('dve', ts_(mixT[:, k, :], pbb[:, k * 128:(k + 1) * 128], gv[:, 16 + grp * 4 + k:17 + grp * 4 + k], None, ALU.mult),
                                 R=bs(PS[1], gv), W=bs(mixT))
                        for dh in range(2):
                            for k in range(4):
                                S.op('pe', mm_(PS[2 + dh][:], mixT[:, k, :], woutb[:, k, dh * 512:(dh + 1) * 512], k == 0, k == 3),
                                     R=bs(mixT, woutb), W=bs(PS[2 + dh]))
                        if grp == 0:
                            S.op('act', act_(yt[:, 0:512], PS[2][:], AF.Copy), R=bs(PS[2]), W=bs(yt))
                            S.op('dve', cp_(yt[:, 512:1024], PS[3][:]), R=bs(PS[3]), W=bs(yt))
                            S.dma('sp', YA[r0:r0 + 128, :], yt[:], R=bs(yt))
                        else:
                            S.dma('sp', yat[:], YA[r0:r0 + 128, :], W=bs(yat))
                            S.dma('sp', xres[:], x[r0:r0 + 128, :], W=bs(xres))
                            for dh in range(2):
                                sl = slice(dh * 512, (dh + 1) * 512)
                                S.op('dve', tt_(yt[:, sl], PS[2 + dh][:], yat[:, sl], ALU.add), R=bs(PS[2 + dh], yat), W=bs(yt))
                            S.op('pool', tt_(yt[:], yt[:], g1bc[:], ALU.mult), R=bs(yt, g1bc), W=bs(yt))
                            S.op('pool', tt_(yt[:], yt[:], xres[:], ALU.add), R=bs(yt, xres), W=bs(yt))
                            S.dma('sp', X1[r0:r0 + 128, :], yt[:], R=bs(yt))
                S.barrier()
            if dbg_stage == 3:
                nrow = len(tiles) * 128
                S.dma('sp', y[b * SEQ:b * SEQ + nrow, :], X1[b * SEQ:b * SEQ + nrow, :])
                S.barrier()
            if dbg_stage <= 4:
                continue

            with ExitStack() as PE_:
                TG = 1
                TT = TG * 128
                NI = 4
                EB = NI * 128
                nblk = 16384 // EB
                wpqb = sb(PE_, [128, 8, D], BF16, "wpqb")
                for k in range(8):
                    load_cast(wpqb[:, k, :], wpqb, w_pq[k * 128:(k + 1) * 128, :], D)
                kTb = sb(PE_, [128, 128], BF16, "kTb")
                load_cast(kTb[:], kTb, kT, 128)
                g2bc = sb(PE_, [128, D], F32, "g2bc")
                gfbc = sb(PE_, [128, D], F32, "gfbc")
                build_gate_bc(m, 40, g2bc)
                S.dma('sp', gfbc[:], g_final.partition_broadcast(128), W=bs(gfbc))
                x1t = [sb(PE_, [128, D], F32, f"x1t{i}") for i in range(TG)]
                xh = sb(PE_, [128, D], F32, "xhE")
                junk = xh
                h2T = sb(PE_, [128, 8, TT], BF16, "h2T")
                pqT = sb(PE_, [128, 8, 128], BF16, "pqT")
                s1 = [sb(PE_, [128, 8, 128], F32, f"s1_{i}") for i in range(TG)]
                s2 = [sb(PE_, [128, 8, 128], F32, f"s2_{i}") for i in range(TG)]
                w1 = sb(PE_, [128, 8, 128], F32, "w1")
                v1 = sb(PE_, [128, 8, 16], F32, "v1")
                v2 = sb(PE_, [128, 8, 16], F32, "v2")
                cand = sb(PE_, [128, 8, 256], F32, "cand")
                best = sb(PE_, [128, 8, 16], F32, "best")
                et = sb(PE_, [128, 8, 16], F32, "et")
                zs = [sb(PE_, [128, 32], F32, f"zs{i}") for i in range(TG)]
                Sg = [sb(PE_, [128, NI, 128], F32, f"Sg{i}") for i in range(4)]
                Ee = [sb(PE_, [128, EB], F32, f"Ee{i}") for i in range(2)]
                Gh2 = [[sb(PE_, [128, 8, EB], BF16, f"Gh{i}_{j}") for i in range(TG)] for j in range(2)]
                gAT = [sb(PE_, [128, NI * 128], BF16, f"gAT{i}") for i in range(4)]
                WTt = [sb(PE_, [128, 128], BF16, f"WT{i}") for i in range(4)]
                uch = [sb(PE_, [128, 8, EB], BF16, f"uch{i}") for i in range(3)]
                vch = [sb(PE_, [128, NI, D], BF16, f"vch{i}") for i in range(3)]
                po = sb(PE_, [128, D], F32, "po")
                rot = {'sg': 0, 'ee': 0, 'ga': 0, 'wt': 0, 'pa': 0, 'pg': 0, 'w': 0}
                PAb = [Buf(), Buf()]
                PGb = [Buf() for _ in range(4)]
                for g0 in range(0, len(tiles), TG):
                    grp_tiles = tiles[g0:g0 + TG]
                    for ti, tt in enumerate(grp_tiles):
                        r0 = b * SEQ + tt * 128
                        X = x1t[ti]
                        S.dma('sp', X[:], X1[r0:r0 + 128, :], W=bs(X))
                        rms_rstd(PE_, X[:], D, small[:, 3:4], junk[:], bs(X), bs(junk), bs(small))
                        S.op('dve', ts_(xh[:], X[:], small[:, 3:4], None, ALU.mult), R=bs(X, small), W=bs(xh))
                        for k in range(8):
                            pb = PS[k // 4]
                            S.op('pe', tr_(pb[:, (k % 4) * 128:(k % 4 + 1) * 128], xh[:, k * 128:(k + 1) * 128], ident_f[:]),
                                 R=bs(xh, ident_f), W=bs(pb))
                        for k in range(8):
                            pb = PS[k // 4]
                            src = pb[:, (k % 4) * 128:(k % 4 + 1) * 128]
                            S.op('act', act_(h2T[:, k, ti * 128:(ti + 1) * 128], src, AF.Identity, bias=m[:, 24 + k:25 + k],
                                             scale=m[:, 32 + k:33 + k]), R=bs(pb, m), W=bs(h2T))
                        for h in range(8):
                            pb = PS[2 + h // 4]
                            for k in range(8):
                                S.op('pe', mm_(pb[:, (h % 4) * 128:(h % 4 + 1) * 128], wpqb[:, k, h * 128:(h + 1) * 128],
                                               h2T[:, k, ti * 128:(ti + 1) * 128], k == 0, k == 7), R=bs(wpqb, h2T), W=bs(pb))
                        S.op('act', act_(pqT[:, 0:4, :].rearrange("p a t -> p (a t)"), PS[2][:], AF.Copy), R=bs(PS[2]), W=bs(pqT))
                        S.op('dve', cp_(pqT[:, 4:8, :].rearrange("p a t -> p (a t)"), PS[3][:]), R=bs(PS[3]), W=bs(pqT))
                        for h in range(8):
                            pa, pb2 = PS[4 + h // 4], PS[6 + h // 4]
                            S.op('pe', mm_(pa[:, (h % 4) * 128:(h % 4 + 1) * 128], pqT[0:64, h, :], kTb[0:64, :], True, True),
                                 R=bs(pqT, kTb), W=bs(pa))
                            S.op('pe', mm_(pb2[:, (h % 4) * 128:(h % 4 + 1) * 128], pqT[64:128, h, :], kTb[64:128, :], True, True),
                                 R=bs(pqT, kTb), W=bs(pb2))
                        s1t, s2t, zst = s1[ti], s2[ti], zs[ti]
                        S.op('act', act_(s1t[:, 0:4, :].rearrange("p a t -> p (a t)"), PS[4][:], AF.Copy), R=bs(PS[4]), W=bs(s1t))
                        S.op('dve', cp_(s1t[:, 4:8, :].rearrange("p a t -> p (a t)"), PS[5][:]), R=bs(PS[5]), W=bs(s1t))
                        S.op('act', act_(s2t[:, 0:4, :].rearrange("p a t -> p (a t)"), PS[6][:], AF.Copy), R=bs(PS[6]), W=bs(s2t))
                        S.op('dve', cp_(s2t[:, 4:8, :].rearrange("p a t -> p (a t)"), PS[7][:]), R=bs(PS[7]), W=bs(s2t))
                        for (sx, vx) in ((s1t, v1), (s2t, v2)):
                            for h in range(8):
                                S.op('dve', max_(vx[:, h, 0:8], sx[:, h, :]), R=bs(sx), W=bs(vx))
                                S.op('dve', mr_(w1[:, h, :], vx[:, h, 0:8], sx[:, h, :], NEGS), R=bs(sx, vx), W=bs(w1))
                                S.op('dve', max_(vx[:, h, 8:16], w1[:, h, :]), R=bs(w1), W=bs(vx))
                        for h in range(8):
                            S.op('dve', tt_(cand[:, h, :].rearrange("p (a c) -> p a c", a=16),
                                             v1[:, h, :].unsqueeze(2).to_broadcast([128, 16, 16]),
                                             v2[:, h, :].unsqueeze(1).to_broadcast([128, 16, 16]), ALU.add), R=bs(v1, v2), W=bs(cand))
                        for h in range(8):
                            S.op('dve', max_(best[:, h, 0:8], cand[:, h, :]), R=bs(cand), W=bs(best))
                            S.op('dve', mr_(cand[:, h, :], best[:, h, 0:8], cand[:, h, :], NEGS), R=bs(best, cand), W=bs(cand))
                            S.op('dve', max_(best[:, h, 8:16], cand[:, h, :]), R=bs(cand), W=bs(best))
                        S.op('dve', tt_(et[:], best[:], best[:, :, 0:1].to_broadcast([128, 8, 16]), ALU.subtract), R=bs(best), W=bs(et))
                        S.op('act', act_(et[:], et[:], AF.Exp), R=bs(et), W=bs(et))
                        S.op('dve', red_(zst[:, 0:8], et[:], ALU.add), R=bs(et), W=bs(zst))
                        S.op('act', act_(zst[:, 8:16], zst[:, 0:8], AF.Ln), R=bs(zst), W=bs(zst))
                        S.op('dve', tt_(zst[:, 16:24], zst[:, 8:16], best[:, :, 0], ALU.add), R=bs(zst, best), W=bs(zst))
                        S.op('dve', ts_(zst[:, 16:24], zst[:, 16:24], -1.0, None, ALU.mult), R=bs(zst), W=bs(zst))
                        S.op('dve', cp_(zst[:, 24:32], best[:, :, 15]), R=bs(best), W=bs(zst))
                    ntg = len(grp_tiles)
                    NTT = ntg * 128
                    pend = []

                    def flush_pend(keep):
                        while len(pend) > keep:
                            fn_, R_, W_ = pend.pop(0)
                            S.op('dve', fn_, R=R_, W=W_)

                    def emit_gate_unit(ib, ti, h):
                        Gh = Gh2[ib % 2]
                        u_ = rot['sg']
                        rot['sg'] += 1
                        pX = PS[6 + u_ % 2]
                        in0 = s1[ti][:, h, ib * NI:(ib + 1) * NI].unsqueeze(2).to_broadcast([128, NI, 128])
                        in1 = s2[ti][:, h, :].unsqueeze(1).to_broadcast([128, NI, 128])
                        nb_ap = zs[ti][:, 16 + h:17 + h]
                        tau_ap = zs[ti][:, 24 + h:25 + h]
                        if u_ % 4 != 0:
                            sg_ = Sg[rot['w'] % 4]
                            rot['w'] += 1
                            S.op('pool', tt_(sg_[:], in0, in1, ALU.add), R=bs(s1[ti], s2[ti]), W=bs(sg_))
                            S.op('act', act_(pX[:], sg_[:].rearrange("p a c -> p (a c)"), AF.Exp, bias=nb_ap),
                                 R=bs(sg_, zs[ti]), W=bs(pX))
                            pend.append((stt_(Gh[ti][:, h, :], sg_[:].rearrange("p a c -> p (a c)"), tau_ap, pX[:],
                                              ALU.is_ge, ALU.mult), bs(sg_, zs[ti], pX), bs(Gh[ti])))
                        else:
                            ee_ = Ee[rot['ee'] % 2]
                            rot['ee'] += 1
                            S.op('dve', tt_(pX[:].rearrange("p (a c) -> p a c", a=NI), in0, in1, ALU.add),
                                 R=bs(s1[ti], s2[ti]), W=bs(pX))
                            S.op('act', act_(ee_[:], pX[:], AF.Exp, bias=nb_ap), R=bs(pX, zs[ti]), W=bs(ee_))
                            pend.append((stt_(Gh[ti][:, h, :], pX[:], tau_ap, ee_[:], ALU.is_ge, ALU.mult),
                                         bs(pX, zs[ti], ee_), bs(Gh[ti])))
                        flush_pend(1)

                    units = [(ti, h) for ti in range(ntg) for h in range(8)]
                    st = {}

                    def issue_loads(ib):
                        e0 = ib * EB
                        uc = uch[ib % 3]
                        vcb = vch[ib % 3]
                        S.dma('sp', uc[:], uTb[:, e0:e0 + EB].rearrange("(k p) n -> p k n", p=128), W=bs(uc))
                        S.dma('sp', vcb[:], pvb[e0:e0 + EB, :].rearrange("(a p) n -> p a n", p=128), W=bs(vcb))

                    def stage_A(ibs):
                        for ib in ibs:
                            uc = uch[ib % 3]
                            pAt = PS[2 + ib % 2]
                            for blk in range(NI):
                                bsl = slice(blk * 128, (blk + 1) * 128)
                                for k in range(8):
                                    S.op('pe', mm_(pAt[:, blk * 128:(blk + 1) * 128], uc[:, k, bsl], h2T[:, k, 0:128], k == 0, k == 7),
                                         R=bs(uc, h2T), W=bs(pAt))
                        for ib in ibs:
                            pAt = PS[2 + ib % 2]
                            ga = gAT[ib % 4]
                            S.op('act', act_(ga[:], pAt[:, 0:NI * 128], AF.Gelu), R=bs(pAt), W=bs(ga))

                    def stage_G(ib, blk):
                        Gh = Gh2[ib % 2]
                        bsl = slice(blk * 128, (blk + 1) * 128)
                        ga = gAT[ib % 4]
                        pGt = PS[4 + rot['pg'] % 2]
                        rot['pg'] += 1
                        pG = pGt[:, 0:128]
                        for h in range(8):
                            S.op('pe', mm_(pG, Gh[0][:, h, bsl], ident_b[:], h == 0, h == 7), R=bs(Gh[0], ident_b), W=bs(pGt))
                        wt_ = WTt[rot['wt'] % 4]
                        rot['wt'] += 1
                        S.op('dve', tt_(wt_[:], pG, ga[:, bsl], ALU.mult), R=bs(pGt, ga), W=bs(wt_))
                        st[(ib, blk)] = wt_

                    def stage_O(ib, blk):
                        vcb = vch[ib % 3]
                        first = (ib == 0 and blk == 0)
                        last = (ib == nblk - 1 and blk == NI - 1)
                        wt_ = st.pop((ib, blk))
                        for dh in range(2):
                            S.op('pe', mm_(PS[dh][:], wt_[:], vcb[:, blk, dh * 512:(dh + 1) * 512], first, last),
                                 R=bs(wt_, vcb), W=bs(PS[dh]))

                    assert TG == 1
                    for (ti, h) in units:
                        emit_gate_unit(0, ti, h)
                    issue_loads(0)
                    stage_A([0])
                    upb = (len(units) + NI - 1) // NI
                    prev = None
                    for ib in range(nblk):
                        nxt = [j for j in (ib + 1, ib + 2) if j < nblk] if ib % 2 == 0 else []
                        for blk in range(NI):
                            if blk == 0:
                                flush_pend(0)
                            stage_G(ib, blk)
                            if prev is not None:
                                stage_O(*prev)
                            prev = (ib, blk)
                            if blk == 0:
                                for j in nxt:
                                    issue_loads(j)
                            if ib + 1 < nblk:
                                for (ti, h) in units[blk * upb:(blk + 1) * upb]:
                                    emit_gate_unit(ib + 1, ti, h)
                            if blk == 1 and nxt:
                                stage_A(nxt)
                    flush_pend(0)
                    stage_O(*prev)
                    for ti, tt in enumerate(grp_tiles):
                        r0 = b * SEQ + tt * 128
                        X = x1t[ti]
                        for dh in range(2):
                            sl = slice(dh * 512, (dh + 1) * 512)
                            S.op('dve', tt_(po[:, sl], PS[ti * 2 + dh][:], g2bc[:, sl], ALU.mult), R=bs(PS[ti * 2 + dh], g2bc), W=bs(po))
                        S.op('pool', tt_(po[:], po[:], X[:], ALU.add), R=bs(po, X), W=bs(po))
                        rms_rstd(PE_, po[:], D, small[:, 4:5], junk[:], bs(po), bs(junk), bs(small))
                        S.op('dve', stt_(po[:], po[:], small[:, 4:5], gfbc[:], ALU.mult, ALU.mult), R=bs(po, small, gfbc), W=bs(po))
                        S.dma('sp', y[r0:r0 + 128, :], po[:], R=bs(po))
            S.barrier()
        S.barrier()
        S.emit()
    return nc


def _prep_inputs(inp, core):
    f = np.float32
    bsl = slice(core * NB, (core + 1) * NB)
    c = np.asarray(inp['c'], f)[bsl]
    cT = np.ascontiguousarray(c.reshape(NB, 8, 128).transpose(2, 1, 0)).reshape(128, 16)
    pos = np.asarray(inp['positions'], np.int32)[bsl]
    posT = np.ascontiguousarray(pos.reshape(NB, NT, 128).transpose(0, 2, 1))
    gcat = np.concatenate([np.asarray(inp['g_out_a'], f)[0], np.asarray(inp['g_out_b'], f)[0]])
    gvec = np.concatenate([np.asarray(inp['g_mix'], f)[0].reshape(8, 128).T,
                           np.asarray(inp['g_ffn'], f)[0].reshape(8, 128).T,
                           gcat.reshape(8, 128).T], axis=1)
    kT = np.concatenate([np.asarray(inp['peer_keys1'], f)[0].T, np.asarray(inp['peer_keys2'], f)[0].T], axis=0)
    return {
        "x": np.ascontiguousarray(np.asarray(inp['x'], f)[bsl].reshape(NB * SEQ, D)),
        "cT": np.ascontiguousarray(cT),
        "posT": posT,
        "w_ada": np.ascontiguousarray(np.asarray(inp['w_ada'], f)[0]),
        "b_adaT": np.ascontiguousarray(np.asarray(inp['b_ada'], f)[0].reshape(48, 128).T),
        "gvec": np.ascontiguousarray(gvec),
        "g_final": np.ascontiguousarray(np.asarray(inp['g_final'], f).reshape(1, D)),
        "g_kv": np.ascontiguousarray(np.asarray(inp['g_kv'], f)[0].reshape(1, 128)),
        "b_forget": np.ascontiguousarray(np.asarray(inp['b_forget'], f)[0].reshape(1, 8)),
        "w_in": np.ascontiguousarray(np.asarray(inp['w_in'], f)[0]),
        "w_uk": np.ascontiguousarray(np.asarray(inp['w_uk'], f)[0].reshape(128, 384)),
        "w_uv": np.ascontiguousarray(np.asarray(inp['w_uv'], f)[0].reshape(128, 512)),
        "w_out": np.ascontiguousarray(np.asarray(inp['w_out'], f)[0]),
        "w_pq": np.ascontiguousarray(np.asarray(inp['w_peer_q'], f)[0]),
        "kT": np.ascontiguousarray(kT),
    }


def kernel(**inputs):
    f = np.float32
    nc = build_nc()
    uT = np.ascontiguousarray(np.asarray(inputs['peer_u'], f)[0].T)
    pv = np.ascontiguousarray(np.asarray(inputs['peer_v'], f)[0])
    in_maps = []
    for core in range(8):
        d = _prep_inputs(inputs, core)
        d["uT"] = uT
        d["pv"] = pv
        in_maps.append(d)
    res = run_bass_kernel_spmd(nc, in_maps, core_ids=list(range(8)))
    out = np.concatenate([np.asarray(r["y"], f).reshape(NB, SEQ, D) for r in res.results], axis=0)
    return out
```

```python
import math
import numpy as np
from contextlib import ExitStack
import concourse.bass as bass
import concourse.mybir as mybir
from concourse.bass_utils import run_bass_kernel_spmd

F32 = mybir.dt.float32
BF16 = mybir.dt.bfloat16
I32 = mybir.dt.int32
AF = mybir.ActivationFunctionType
ALU = mybir.AluOpType
AX = mybir.AxisListType

ENGS = ('pe', 'act', 'dve', 'pool', 'sp')
EPOCH = 30000
N_EPOCH = 12
N_DMA = 24

NEG = -30000.0
NEGS = -1.0e30
SEQ = 4096
NT = 32
NB = 2
D = 1024
NCOL = 2496
THETA = 500000.0


class Buf:
    __slots__ = ('w', 'r', 'name')

    def __init__(self, name=''):
        self.w = None
        self.r = []
        self.name = name


class Sched:
    def __init__(self, nc, ctx):
        self.nc = nc
        self.q = {e: [] for e in ENGS}
        self.sems = {}
        for e in ENGS:
            if e == 'sp':
                continue
            for k in range(N_EPOCH):
                self.sems[(e, k)] = ctx.enter_context(nc.semaphore(f"s_{e}{k}"))
        for k in range(N_DMA):
            self.sems[('dma', k)] = ctx.enter_context(nc.semaphore(f"s_dma{k}"))
        self.epoch = {e: 0 for e in ENGS}
        self.cnt = {e: 0 for e in ENGS}
        self.dcnt = [0] * N_DMA
        self.dnext = 0
        self.known = {e: {} for e in ENGS}
        self.nops = 0

    def _need(self, eng, deps):
        waits = []
        kn = self.known[eng]
        best = {}
        for (s, v) in deps:
            if best.get(s, 0) < v:
                best[s] = v
        for s, v in best.items():
            if kn.get(s, 0) < v:
                kn[s] = v
                waits.append((s, v))
        return waits

    @staticmethod
    def _deps(R, W):
        deps = []
        for b in R:
            if b.w is not None:
                deps.append(b.w)
        for b in W:
            if b.w is not None:
                deps.append(b.w)
            deps.extend(b.r)
        return deps

    def op(self, eng, fn, R=(), W=()):
        deps = self._deps(R, W)
        if eng == 'pe':
            deps = [d for d in deps if d[0][0] != 'pe']
        waits = self._need(eng, deps)
        if self.cnt[eng] >= EPOCH:
            self.epoch[eng] += 1
            self.cnt[eng] = 0
        self.cnt[eng] += 1
        tok = ((eng, self.epoch[eng]), self.cnt[eng])
        self.q[eng].append((waits, fn, tok[0], 1))
        for b in R:
            b.r.append(tok)
        for b in W:
            b.w = tok
            b.r = []
        self.nops += 1
        return tok

    def dma(self, q, out, in_, R=(), W=(), **kw):
        slot = self.dnext
        self.dnext = (self.dnext + 1) % N_DMA
        s = ('dma', slot)
        deps = self._deps(R, W)
        if self.dcnt[slot] > 0:
            deps.append((s, self.dcnt[slot]))
        waits = self._need(q, deps)
        self.dcnt[slot] += 16
        tok = (s, self.dcnt[slot])

        def fn(e, out=out, in_=in_, kw=kw):
            return e.dma_start(out=out, in_=in_, **kw)
        self.q[q].append((waits, fn, s, 16))
        for b in R:
            b.r.append(tok)
        for b in W:
            b.w = tok
            b.r = []
        self.nops += 1
        return tok

    def barrier(self):
        toks = []
        for e in ENGS:
            if e == 'sp':
                continue
            if self.cnt[e] > 0:
                toks.append(((e, self.epoch[e]), self.cnt[e]))
        for k in range(N_DMA):
            if self.dcnt[k] > 0:
                toks.append((('dma', k), self.dcnt[k]))
        for e in ENGS:
            waits = self._need(e, toks)
            if waits:
                self.q[e].append((waits, None, None, 0))

    def emit(self):
        nc = self.nc
        sems = self.sems
        qs = self.q

        def replay(e, lst):
            for (waits, fn, s, inc) in lst:
                for (ws, v) in waits:
                    e.wait_ge(sems[ws], v)
                if fn is None:
                    continue
                fn(e).then_inc(sems[s], inc)

        with nc.Block() as block:
            @block.tensor
            def _(e):
                replay(e, qs['pe'])

            @block.scalar
            def _(e):
                replay(e, qs['act'])

            @block.vector
            def _(e):
                replay(e, qs['dve'])

            @block.gpsimd
            def _(e):
                replay(e, qs['pool'])

            @block.sync
            def _(e):
                replay(e, qs['sp'])


class T:
    def __init__(self, t, name=''):
        self.t = t
        self.b = Buf(name)

    def __getitem__(self, k):
        return self.t[k]


def build_nc(stage=99, ntile_dbg=None, nb_dbg=None):
    nc = bass.Bass("TRN2", target_bir_lowering=False)

    def din(name, shape, dt):
        return nc.dram_tensor(name, shape, dt, kind="ExternalInput").ap()

    def dscr(name, shape, dt):
        return nc.dram_tensor(name, shape, dt, kind="Internal").ap()

    x = din("x", [NB * SEQ, D], F32)
    cT = din("cT", [128, 16], F32)
    posT = din("posT", [NB, 128, NT], I32)
    w_ada = din("w_ada", [D, 6 * D], F32)
    b_adaT = din("b_adaT", [128, 48], F32)
    gvec = din("gvec", [128, 24], F32)
    g_final = din("g_final", [1, D], F32)
    g_kv = din("g_kv", [1, 128], F32)
    b_forget = din("b_forget", [1, 8], F32)
    w_in = din("w_in", [D, NCOL], F32)
    w_uk = din("w_uk", [128, 384], F32)
    w_uv = din("w_uv", [128, 512], F32)
    w_out = din("w_out", [D, D], F32)
    w_pq = din("w_pq", [D, D], F32)
    kT = din("kT", [128, 128], F32)
    uT = din("uT", [D, 16384], F32)
    pv = din("pv", [16384, D], F32)
    y = nc.dram_tensor("y", [NB * SEQ, D], F32, kind="ExternalOutput").ap()

    QAT = dscr("QAT", [NB, 128, 4, SEQ], BF16)
    KATs = dscr("KATs", [NB, 128, 4, SEQ], BF16)
    VAs = dscr("VAs", [NB, 128, NT, 512], BF16)
    IQT = dscr("IQT", [NB, 128, 3, SEQ], BF16)
    IKT = dscr("IKT", [NB, 128, SEQ], BF16)
    SGN = dscr("SGN", [NB, 128, NT, 8], F32)
    QBT = dscr("QBT", [NB, 128, 4, SEQ], BF16)
    KBTs = dscr("KBTs", [NB, 128, 4, SEQ], BF16)
    VBs = dscr("VBs", [NB, 128, NT, 512], BF16)
    YA = dscr("YA", [NB * SEQ, D], F32)
    X1 = dscr("X1", [NB * SEQ, D], F32)
    NCUM = dscr("NCUM", [NB, 8, SEQ], F32)
    uTb = dscr("uTb", [D, 16384], BF16)
    pvb = dscr("pvb", [16384, D], BF16)
    DBG = {}
    dbg_stage = stage

    with ExitStack() as ctx:
        S = Sched(nc, ctx)
        counter = [0]

        def sb(ctxx, shape, dt, name=None):
            counter[0] += 1
            nm = f"{name or 't'}_{counter[0]}"
            return T(ctxx.enter_context(nc.sbuf_tensor(nm, shape, dt)), nm)

        def psb(ctxx, shape, dt, name=None):
            counter[0] += 1
            nm = f"{name or 'p'}_{counter[0]}"
            return T(ctxx.enter_context(nc.psum_tensor(nm, shape, dt)), nm)

        def bs(*ts):
            return [t.b for t in ts]

        def ts_(out, in0, s1, s2, op0, op1=None, accum_out=None):
            if op1 is None:
                return lambda e: e.tensor_scalar(out=out, in0=in0, scalar1=s1, scalar2=None, op0=op0)
            if accum_out is not None:
                return lambda e: e.tensor_scalar(out=out, in0=in0, scalar1=s1, scalar2=s2, op0=op0, op1=op1,
                                                 accum_out=accum_out)
            return lambda e: e.tensor_scalar(out=out, in0=in0, scalar1=s1, scalar2=s2, op0=op0, op1=op1)

        def tt_(out, in0, in1, op):
            return lambda e: e.tensor_tensor(out=out, in0=in0, in1=in1, op=op)

        def stt_(out, in0, scalar, in1, op0, op1):
            return lambda e: e.scalar_tensor_tensor(out=out, in0=in0, scalar=scalar, in1=in1, op0=op0, op1=op1)

        def cp_(out, in_):
            return lambda e: e.tensor_copy(out=out, in_=in_)

        def act_(out, in_, func, bias=None, scale=None, accum_out=None):
            kw = {}
            if bias is not None:
                kw['bias'] = bias
            if scale is not None:
                kw['scale'] = scale
            if accum_out is not None:
                kw['accum_out'] = accum_out
            return lambda e: e.activation(out=out, in_=in_, func=func, **kw)

        def max_(out, in_):
            return lambda e: e.max(out=out, in_=in_)

        def mr_(out, repl, vals, imm):
            return lambda e: e.match_replace(out=out, in_to_replace=repl, in_values=vals, imm_value=imm)

        def red_(out, in_, op):
            return lambda e: e.tensor_reduce(out=out, in_=in_, axis=AX.X, op=op)

        def recip_(out, in_):
            return lambda e: e.reciprocal(out=out, in_=in_)

        def memset_(ap, v):
            return lambda e: e.memset(ap, v)

        def mm_(out, lhsT, rhs, start, stop, sgc=False):
            if sgc:
                return lambda e: e.matmul(out, lhsT=lhsT, rhs=rhs, start=start, stop=stop, skip_group_check=True)
            return lambda e: e.matmul(out, lhsT=lhsT, rhs=rhs, start=start, stop=stop)

        def tr_(out, in_, ident):
            return lambda e: e.transpose(out=out, in_=in_, identity=ident)

        G = ctx
        idi = sb(G, [128, 128], I32)
        pidi = sb(G, [128, 1], I32)
        idf = sb(G, [128, 128], F32)
        pidf = sb(G, [128, 1], F32)
        ident_f = sb(G, [128, 128], F32, "ident_f")
        ident_b = sb(G, [128, 128], BF16, "ident_b")
        cmaskL = sb(G, [128, 128], F32, "cmaskL")
        cmaskS = sb(G, [128, 128], F32, "cmaskS")
        tri_f = sb(G, [128, 128], F32, "tri_f")
        ones_f = sb(G, [128, 128], F32, "ones_f")
        invf = sb(G, [128, 24], F32, "invf")
        cact = sb(G, [128, 16], F32, "cact")
        badaT = sb(G, [128, 48], F32, "badaT")
        gv = sb(G, [128, 24], F32, "gv")
        mod = sb(G, [128, 48, 2], F32, "mod")
        mv = [sb(G, [128, 48], F32, f"mv{b}") for b in range(NB)]
        gkvbc = sb(G, [128, 128], F32, "gkvbc")
        bfbc = sb(G, [128, 8], F32, "bfbc")
        lsall = sb(G, [128, NT, 8], F32, "lsall")
        dgt = sb(G, [128, 128], F32, "dgt")
        rt = sb(G, [128, NT, 24], F32, "rt")
        small = sb(G, [128, 64], F32, "small")
        stgf = sb(G, [128, 1024], F32, "stgf")
        castrot = [0]

        def load_cast(dst_ap, dst_t, src_ap, n):
            S.dma('sp', stgf[:, 0:n], src_ap, W=bs(stgf))
            castrot[0] += 1
            if castrot[0] % 2 == 0:
                S.op('act', act_(dst_ap, stgf[:, 0:n], AF.Copy), R=bs(stgf), W=bs(dst_t))
            else:
                S.op('dve', cp_(dst_ap, stgf[:, 0:n]), R=bs(stgf), W=bs(dst_t))

        PS = [psb(G, [128, 512], F32, f"PS{i}") for i in range(8)]

        def psbf(i):
            return PS[i][:].bitcast(BF16)

        S.op('pool', lambda e: e.iota(idi[:], pattern=[[1, 128]], base=0, channel_multiplier=0), W=bs(idi))
        S.op('pool', lambda e: e.iota(pidi[:], pattern=[[0, 1]], base=0, channel_multiplier=1), W=bs(pidi))
        S.op('dve', cp_(idf[:], idi[:]), R=bs(idi), W=bs(idf))
        S.op('dve', cp_(pidf[:], pidi[:]), R=bs(pidi), W=bs(pidf))
        S.op('dve', ts_(ident_f[:], idf[:], pidf[:, 0:1], None, ALU.is_equal), R=bs(idf, pidf), W=bs(ident_f))
        S.op('dve', cp_(ident_b[:], ident_f[:]), R=bs(ident_f), W=bs(ident_b))
        S.op('dve', ts_(cmaskL[:], idf[:], pidf[:, 0:1], NEG, ALU.is_gt, ALU.mult), R=bs(idf, pidf), W=bs(cmaskL))
        S.op('dve', ts_(cmaskS[:], idf[:], pidf[:, 0:1], NEGS, ALU.is_gt, ALU.mult), R=bs(idf, pidf), W=bs(cmaskS))
        S.op('dve', ts_(tri_f[:], idf[:], pidf[:, 0:1], None, ALU.is_ge), R=bs(idf, pidf), W=bs(tri_f))
        S.op('dve', memset_(ones_f[:], 1.0), W=bs(ones_f))
        fr = [THETA ** (-(2.0 * k) / 16.0) for k in range(8)] + [THETA ** (-(2.0 * k) / 8.0) for k in range(4)]
        for rep in range(2):
            for k in range(12):
                S.op('dve', (lambda e, c=rep * 12 + k, v=float(np.float32(fr[k])): e.memset(invf[:, c:c + 1], v)),
                     W=bs(invf))
        S.dma('sp', cact[:], cT, W=bs(cact))
        S.dma('sp', badaT[:], b_adaT, W=bs(badaT))
        S.dma('sp', gv[:], gvec, W=bs(gv))
        S.dma('sp', gkvbc[:], g_kv.partition_broadcast(128), W=bs(gkvbc))
        S.dma('sp', bfbc[:], b_forget.partition_broadcast(128), W=bs(bfbc))
        S.op('act', act_(cact[:], cact[:], AF.Silu), R=bs(cact), W=bs(cact))

        with ExitStack() as P0:
            wada = [sb(P0, [128, 8, D], F32, f"wada{i}") for i in range(2)]
            for g in range(6):
                wt_ = wada[g % 2]
                S.dma('sp', wt_[:], w_ada[:, g * D:(g + 1) * D].rearrange("(k p) n -> p k n", p=128), W=bs(wt_))
                for jc in range(8):
                    j = g * 8 + jc
                    for k in range(8):
                        S.op('pe', mm_(PS[0][:, j * 2:(j + 1) * 2], wt_[:, k, jc * 128:(jc + 1) * 128],
                                       cact[:, k * 2:(k + 1) * 2], k == 0, k == 7), R=bs(wt_, cact), W=bs(PS[0]))
            S.op('dve', tt_(mod[:], PS[0][:, 0:96].rearrange("p (j b) -> p j b", b=2),
                            badaT[:].unsqueeze(2).to_broadcast([128, 48, 2]), ALU.add), R=bs(PS[0], badaT), W=bs(mod))
            for b in range(NB):
                m = mv[b]
                S.op('dve', cp_(m[:, 0:8], mod[:, 0:8, b]), R=bs(mod), W=bs(m))
                S.op('dve', stt_(m[:, 8:16], mod[:, 8:16, b], 1.0, gv[:, 0:8], ALU.add, ALU.mult), R=bs(mod, gv), W=bs(m))
                S.op('dve', cp_(m[:, 16:24], mod[:, 16:24, b]), R=bs(mod), W=bs(m))
                S.op('dve', cp_(m[:, 24:32], mod[:, 24:32, b]), R=bs(mod), W=bs(m))
                S.op('dve', stt_(m[:, 32:40], mod[:, 32:40, b], 1.0, gv[:, 8:16], ALU.add, ALU.mult), R=bs(mod, gv), W=bs(m))
                S.op('dve', cp_(m[:, 40:48], mod[:, 40:48, b]), R=bs(mod), W=bs(m))
        S.barrier()

        if stage >= 4:
            with ExitStack() as P0:
                CW = 8192
                cf = [sb(P0, [128, CW], F32, f"cf{i}") for i in range(2)]
                cb = [sb(P0, [128, CW], BF16, f"cb{i}") for i in range(2)]
                i = 0
                for (src, dst) in ((uT, uTb), (pv, pvb)):
                    sv = src.rearrange("(p a) n -> p (a n)", p=128)
                    dv = dst.rearrange("(p a) n -> p (a n)", p=128)
                    tot = sv.shape[1]
                    for c in range(tot // CW):
                        f_, t_ = cf[i % 2], cb[i % 2]
                        S.dma('sp', f_[:], sv[:, c * CW:(c + 1) * CW], W=bs(f_))
                        if i % 3 == 0:
                            S.op('act', act_(t_[:], f_[:], AF.Copy), R=bs(f_), W=bs(t_))
                        elif i % 3 == 1:
                            S.op('dve', cp_(t_[:], f_[:]), R=bs(f_), W=bs(t_))
                        else:
                            S.op('pool', cp_(t_[:], f_[:]), R=bs(f_), W=bs(t_))
                        S.dma('sp', dv[:, c * CW:(c + 1) * CW], t_[:], R=bs(t_))
                        i += 1
            S.barrier()

        def build_gate_bc(m, src0, dst):
            for k in range(8):
                S.op('dve', ts_(dgt[:], ident_f[:], m[:, src0 + k:src0 + k + 1], None, ALU.mult),
                     R=bs(ident_f, m), W=bs(dgt))
                pb = PS[1 + k // 4]
                S.op('pe', mm_(pb[:, (k % 4) * 128:(k % 4 + 1) * 128], ones_f[:], dgt[:], True, True),
                     R=bs(ones_f, dgt), W=bs(pb))
            S.op('act', act_(dst[:, 0:512], PS[1][:], AF.Copy), R=bs(PS[1]), W=bs(dst))
            S.op('act', act_(dst[:, 512:1024], PS[2][:], AF.Copy), R=bs(PS[2]), W=bs(dst))

        def rms_rstd(ctxx, src_ap, n, rstd_ap, junk_ap, src_b, junk_b, sm_b, eps=1e-6):
            S.op('act', act_(junk_ap, src_ap, AF.Square, accum_out=small[:, 60:61]), R=src_b, W=junk_b + bs(small))
            S.op('dve', ts_(small[:, 61:62], small[:, 60:61], 1.0 / n, eps, ALU.mult, ALU.add), R=bs(small), W=bs(small))
            S.op('act', act_(small[:, 62:63], small[:, 61:62], AF.Sqrt), R=bs(small), W=bs(small))
            S.op('dve', recip_(rstd_ap, small[:, 62:63]), R=bs(small), W=sm_b)

        tiles = list(range(NT)) if ntile_dbg is None else list(range(ntile_dbg))
        for b in range(NB if ntile_dbg is None else (nb_dbg or 1)):
            m = mv[b]
            with ExitStack() as P0:
                posi = sb(P0, [128, NT], I32)
                posf = sb(P0, [128, NT], F32)
                ang = sb(P0, [128, NT, 24], F32)
                kf = sb(P0, [128, NT, 24], F32)
                ki = sb(P0, [128, NT, 24], I32)
                S.dma('sp', posi[:], posT[b], W=bs(posi))
                S.op('dve', cp_(posf[:], posi[:]), R=bs(posi), W=bs(posf))
                S.op('dve', tt_(ang[:], posf[:].unsqueeze(2).to_broadcast([128, NT, 24]),
                                invf[:].unsqueeze(1).to_broadcast([128, NT, 24]), ALU.mult), R=bs(posf, invf), W=bs(ang))
                S.op('dve', ts_(ang[:, :, 12:24], ang[:, :, 12:24], math.pi / 2, None, ALU.add), R=bs(ang), W=bs(ang))
                S.op('dve', ts_(kf[:], ang[:], 1.0 / (2 * math.pi), None, ALU.mult), R=bs(ang), W=bs(kf))
                S.op('dve', cp_(ki[:], kf[:]), R=bs(kf), W=bs(ki))
                S.op('dve', cp_(kf[:], ki[:]), R=bs(ki), W=bs(kf))
                C1 = 6.28125
                C2 = 2 * math.pi - C1
                S.op('dve', stt_(ang[:].rearrange("p a b -> p (a b)"), kf[:].rearrange("p a b -> p (a b)"), -C1,
                                 ang[:].rearrange("p a b -> p (a b)"), ALU.mult, ALU.add), R=bs(kf, ang), W=bs(ang))
                S.op('dve', stt_(ang[:].rearrange("p a b -> p (a b)"), kf[:].rearrange("p a b -> p (a b)"), -C2,
                                 ang[:].rearrange("p a b -> p (a b)"), ALU.mult, ALU.add), R=bs(kf, ang), W=bs(ang))
                S.op('dve', ts_(ang[:], ang[:], -math.pi, math.pi, ALU.max, ALU.min), R=bs(ang), W=bs(ang))
                S.op('act', act_(rt[:], ang[:], AF.Sin), R=bs(ang), W=bs(rt))
            S.barrier()

            with ExitStack() as PP:
                winb = sb(PP, [128, 8, NCOL], BF16, "winb")
                wukb = sb(PP, [128, 384], BF16, "wukb")
                wuvb = sb(PP, [128, 512], BF16, "wuvb")
                for k in range(8):
                    for (c0, c1) in ((0, 1024), (1024, 2048), (2048, NCOL)):
                        load_cast(winb[:, k, c0:c1], winb, w_in[k * 128:(k + 1) * 128, c0:c1], c1 - c0)
                load_cast(wukb[:], wukb, w_uk, 384)
                load_cast(wuvb[:], wuvb, w_uv, 512)
                xt = [sb(PP, [128, D], F32, f"xt{i}") for i in range(2)]
                junk = sb(PP, [128, D], F32, "junk")
                xh = sb(PP, [128, D], F32, "xh")
                hT = sb(PP, [128, 8, 128], BF16, "hT")
                pr = sb(PP, [128, NCOL], F32, "pr")
                qa_b = sb(PP, [128, 512], BF16, "qa_b")
                qb_b = sb(PP, [128, 512], BF16, "qb_b")
                kb_b = sb(PP, [128, 512], BF16, "kb_b")
                vb_b = sb(PP, [128, 512], BF16, "vb_b")
                va_b = sb(PP, [128, 512], BF16, "va_b")
                ka_b = sb(PP, [128, 8, 64], BF16, "ka_b")
                kr_b = sb(PP, [128, 16], BF16, "kr_b")
                iq_f = sb(PP, [128, 8, 32], F32, "iq_f")
                iq_b = sb(PP, [128, 3, 4, 32], BF16, "iq_b")
                S.op('dve', memset_(iq_b[:], 0.0), W=bs(iq_b))
                ik_b = sb(PP, [128, 4, 32], BF16, "ik_b")
                ik_f = sb(PP, [128, 32], F32, "ik_f")
                latn = sb(PP, [128, 128], BF16, "latn")
                latnT = sb(PP, [128, 128], BF16, "latnT")
                rtmp = [sb(PP, [128, 8, 8], F32, f"rtmp{i}") for i in range(4)]
                wabs = sb(PP, [128, 8], F32, "wabs")
                sgn_t = sb(PP, [128, 8], F32, "sgn_t")
                fz = sb(PP, [128, 8], F32, "fz")
                oT = sb(PP, [128, 4, 128], BF16, "oT")
                oT2 = sb(PP, [128, 4, 128], BF16, "oT2")
                oT3 = sb(PP, [128, 4, 128], BF16, "oT3")
                oT4 = sb(PP, [128, 4, 128], BF16, "oT4")
                ncs = sb(PP, [8, 512], F32, "ncs")

                def rope(dst3, src3, H, half, sinap, cosap, Rb, Wb):
                    cb_ = cosap.unsqueeze(1).to_broadcast([128, H, half])
                    sb_ = sinap.unsqueeze(1).to_broadcast([128, H, half])
                    x1 = src3[:, :, 0:half]
                    x2 = src3[:, :, half:2 * half]
                    t = [r[:, 0:H, 0:half] for r in rtmp]
                    S.op('dve', tt_(t[0], x1, cb_, ALU.mult), R=Rb + bs(rt), W=bs(rtmp[0]))
                    S.op('dve', tt_(t[1], x2, sb_, ALU.mult), R=Rb + bs(rt), W=bs(rtmp[1]))
                    S.op('dve', tt_(t[2], x2, cb_, ALU.mult), R=Rb + bs(rt), W=bs(rtmp[2]))
                    S.op('dve', tt_(t[3], x1, sb_, ALU.mult), R=Rb + bs(rt), W=bs(rtmp[3]))
                    S.op('dve', tt_(dst3[:, :, 0:half], t[0], t[1], ALU.subtract), R=bs(rtmp[0], rtmp[1]), W=Wb)
                    S.op('dve', tt_(dst3[:, :, half:2 * half], t[2], t[3], ALU.add), R=bs(rtmp[2], rtmp[3]), W=Wb)

                for tt in tiles:
                    r0 = b * SEQ + tt * 128
                    X = xt[tt % 2]
                    S.dma('sp', X[:], x[r0:r0 + 128, :], W=bs(X))
                    rms_rstd(PP, X[:], D, small[:, 0:1], junk[:], bs(X), bs(junk), bs(small))
                    S.op('dve', ts_(xh[:], X[:], small[:, 0:1], None, ALU.mult), R=bs(X, small), W=bs(xh))
                    for k in range(8):
                        pb = PS[k // 4]
                        S.op('pe', tr_(pb[:, (k % 4) * 128:(k % 4 + 1) * 128], xh[:, k * 128:(k + 1) * 128], ident_f[:]),
                             R=bs(xh, ident_f), W=bs(pb))
                    for k in range(8):
                        pb = PS[k // 4]
                        src = pb[:, (k % 4) * 128:(k % 4 + 1) * 128]
                        if k // 4 == 0:
                            S.op('dve', ts_(hT[:, k, :], src, m[:, 8 + k:9 + k], m[:, k:k + 1], ALU.mult, ALU.add),
                                 R=bs(pb, m), W=bs(hT))
                        else:
                            S.op('act', act_(hT[:, k, :], src, AF.Identity, bias=m[:, k:k + 1], scale=m[:, 8 + k:9 + k]),
                                 R=bs(pb, m), W=bs(hT))
                    cols = [(0, 512), (512, 1024), (1024, 1536), (1536, 2048), (2048, NCOL)]
                    for ci, (c0, c1) in enumerate(cols):
                        pb = PS[2 + ci]
                        for k in range(8):
                            S.op('pe', mm_(pb[:, 0:c1 - c0], hT[:, k, :], winb[:, k, c0:c1], k == 0, k == 7),
                                 R=bs(hT, winb), W=bs(pb))
                        if ci % 2 == 0:
                            S.op('act', act_(pr[:, c0:c1], pb[:, 0:c1 - c0], AF.Copy), R=bs(pb), W=bs(pr))
                        else:
                            S.op('dve', cp_(pr[:, c0:c1], pb[:, 0:c1 - c0]), R=bs(pb), W=bs(pr))
                    sinA, sinI = rt[:, tt, 0:8], rt[:, tt, 8:12]
                    cosA, cosI = rt[:, tt, 12:20], rt[:, tt, 20:24]
                    S.op('act', act_(qa_b[:], pr[:, 0:512], AF.Copy), R=bs(pr), W=bs(qa_b))
                    rope(qa_b[:].rearrange("p (h d) -> p h d", h=8), pr[:, 0:512].rearrange("p (h d) -> p h d", h=8),
                         8, 8, sinA, cosA, bs(pr), bs(qa_b))
                    rope(kr_b[:].unsqueeze(1), pr[:, 640:656].unsqueeze(1), 1, 8, sinA, cosA, bs(pr), bs(kr_b))
                    rms_rstd(PP, pr[:, 512:640], 128, small[:, 1:2], junk[:, 0:128], bs(pr), bs(junk), bs(small))
                    S.op('dve', stt_(latn[:], pr[:, 512:640], small[:, 1:2], gkvbc[:], ALU.mult, ALU.mult),
                         R=bs(pr, small, gkvbc), W=bs(latn))
                    S.op('pe', tr_(psbf(7)[:, 0:128], latn[:], ident_b[:]), R=bs(latn, ident_b), W=bs(PS[7]))
                    S.op('act', act_(latnT[:], psbf(7)[:, 0:128], AF.Copy), R=bs(PS[7]), W=bs(latnT))
                    S.op('pe', mm_(PS[0][:, 0:384], latnT[:], wukb[:], True, True), R=bs(latnT, wukb), W=bs(PS[0]))
                    S.op('pe', mm_(PS[1][:, 0:512], latnT[:], wuvb[:], True, True), R=bs(latnT, wuvb), W=bs(PS[1]))
                    S.op('act', act_(ka_b[:, :, 16:64], PS[0][:, 0:384].rearrange("p (h d) -> p h d", h=8), AF.Copy),
                         R=bs(PS[0]), W=bs(ka_b))
                    S.op('dve', cp_(ka_b[:, :, 0:16], kr_b[:].unsqueeze(1).to_broadcast([128, 8, 16])),
                         R=bs(kr_b), W=bs(ka_b))
                    S.op('dve', cp_(va_b[:], PS[1][:, 0:512]), R=bs(PS[1]), W=bs(va_b))
                    S.dma('sp', VAs[b, :, tt, :], va_b[:], R=bs(va_b))
                    S.op('act', act_(wabs[:], pr[:, 944:952], AF.Abs, scale=(8.0 ** -0.5) * (32.0 ** -0.5)),
                         R=bs(pr), W=bs(wabs))
                    S.op('dve', ts_(sgn_t[:], pr[:, 944:952], 0.0, 2.0, ALU.is_ge, ALU.mult), R=bs(pr), W=bs(sgn_t))
                    S.op('dve', ts_(sgn_t[:], sgn_t[:], -1.0, None, ALU.add), R=bs(sgn_t), W=bs(sgn_t))
                    S.dma('sp', SGN[b, :, tt, :], sgn_t[:], R=bs(sgn_t))
                    S.op('dve', cp_(iq_f[:], pr[:, 656:912].rearrange("p (h d) -> p h d", h=8)), R=bs(pr), W=bs(iq_f))
                    rope(iq_f[:], pr[:, 656:912].rearrange("p (h d) -> p h d", h=8), 8, 4, sinI, cosI, bs(pr), bs(iq_f))
                    for k3 in range(3):
                        nh = 3 if k3 < 2 else 2
                        S.op('dve', tt_(iq_b[:, k3, 0:nh, :], iq_f[:, 3 * k3:3 * k3 + nh, :],
                                        wabs[:, 3 * k3:3 * k3 + nh].unsqueeze(2).to_broadcast([128, nh, 32]), ALU.mult),
                             R=bs(iq_f, wabs), W=bs(iq_b))
                    S.op('dve', cp_(ik_f[:], pr[:, 912:944]), R=bs(pr), W=bs(ik_f))
                    rope(ik_f[:].unsqueeze(1), pr[:, 912:944].unsqueeze(1), 1, 4, sinI, cosI, bs(pr), bs(ik_f))
                    S.op('dve', cp_(ik_b[:], ik_f[:].unsqueeze(1).to_broadcast([128, 4, 32])), R=bs(ik_f), W=bs(ik_b))
                    S.op('act', act_(qb_b[:], pr[:, 952:1464], AF.Copy), R=bs(pr), W=bs(qb_b))
                    S.op('dve', cp_(kb_b[:], pr[:, 1464:1976]), R=bs(pr), W=bs(kb_b))
                    S.op('act', act_(vb_b[:], pr[:, 1976:2488], AF.Copy), R=bs(pr), W=bs(vb_b))
                    S.dma('sp', VBs[b, :, tt, :], vb_b[:], R=bs(vb_b))
                    S.op('dve', tt_(fz[:], pr[:, 2488:2496], bfbc[:], ALU.add), R=bs(pr, bfbc), W=bs(fz))
                    S.op('act', act_(fz[:], fz[:], AF.Exp, scale=-1.0), R=bs(fz), W=bs(fz))
                    S.op('act', act_(lsall[:, tt, :], fz[:], AF.Ln, bias=1.0), R=bs(fz), W=bs(lsall))
                    jobs = [(qa_b, 4, QAT[b], oT, 0), (ka_b, 4, KATs[b], oT2, 1), (qb_b, 4, QBT[b], oT3, 2),
                            (kb_b, 4, KBTs[b], oT4, 3)]
                    for (src, nblk, dst, stg, pbi) in jobs:
                        sv = src[:] if len(src.t.shape) == 2 else src[:].rearrange("p h d -> p (h d)")
                        pbb = psbf(pbi)
                        for k in range(nblk):
                            S.op('pe', tr_(pbb[:, k * 128:(k + 1) * 128], sv[:, k * 128:(k + 1) * 128], ident_b[:]),
                                 R=bs(src, ident_b), W=bs(PS[pbi]))
                        eng = 'act' if pbi % 2 == 0 else 'dve'
                        if eng == 'act':
                            S.op('act', act_(stg[:].rearrange("p a t -> p (a t)"), pbb[:, 0:512], AF.Copy), R=bs(PS[pbi]), W=bs(stg))
                        else:
                            S.op('dve', cp_(stg[:].rearrange("p a t -> p (a t)"), pbb[:, 0:512]), R=bs(PS[pbi]), W=bs(stg))
                        S.dma('sp', dst[:, :, tt * 128:(tt + 1) * 128], stg[:], R=bs(stg))
                    pbb = psbf(4)
                    for k in range(3):
                        S.op('pe', tr_(pbb[:, k * 128:(k + 1) * 128], iq_b[:, k, :, :].rearrange("p a d -> p (a d)"), ident_b[:]),
                             R=bs(iq_b, ident_b), W=bs(PS[4]))
                    S.op('pe', tr_(pbb[:, 384:512], ik_b[:].rearrange("p a d -> p (a d)"), ident_b[:]),
                         R=bs(ik_b, ident_b), W=bs(PS[4]))
                    S.op('act', act_(oT[:].rearrange("p a t -> p (a t)"), pbb[:, 0:512], AF.Copy), R=bs(PS[4]), W=bs(oT))
                    S.dma('sp', IQT[b, :, :, tt * 128:(tt + 1) * 128], oT[:, 0:3, :], R=bs(oT))
                    S.dma('sp', IKT[b, :, tt * 128:(tt + 1) * 128], oT[:, 3, :], R=bs(oT))
                ntl = len(tiles)
                for T0 in range(0, ntl, 4):
                    pb = PS[5 + (T0 // 4) % 2]
                    for Tt in range(T0, min(T0 + 4, ntl)):
                        oc = (Tt - T0) * 128
                        for J in range(Tt + 1):
                            S.op('pe', mm_(pb[0:8, oc:oc + 128], lsall[:, J, :], (tri_f if J == Tt else ones_f)[:],
                                           J == 0, J == Tt), R=bs(lsall, tri_f, ones_f), W=bs(pb))
                    n = (min(T0 + 4, ntl) - T0) * 128
                    S.op('act', act_(ncs[0:8, 0:n], pb[0:8, 0:n], AF.Copy, scale=8.0), R=bs(pb), W=bs(ncs))
                    S.dma('sp', NCUM[b, :, T0 * 128:T0 * 128 + n], ncs[0:8, 0:n], R=bs(ncs))
            S.barrier()
            if dbg_stage == 1:
                continue

            for grp in (0, 1):
                with ExitStack() as PA:
                    KT = sb(PA, [128, 4, SEQ], BF16, "KT")
                    VV = sb(PA, [128, NT, 512], BF16, "VV")
                    L_all = len(tiles) * 128
                    for p4 in range(4):
                        S.dma('sp', KT[:, p4, 0:L_all], (KATs if grp == 0 else KBTs)[b, :, p4, 0:L_all], W=bs(KT))
                    S.dma('sp', VV[:, 0:len(tiles), :], (VAs if grp == 0 else VBs)[b, :, 0:len(tiles), :], W=bs(VV))
                    woutb = sb(PA, [128, 4, D], BF16, "woutb")
                    for k in range(4):
                        load_cast(woutb[:, k, :], woutb, w_out[grp * 512 + k * 128:grp * 512 + (k + 1) * 128, :], D)
                    QT = [sb(PA, [128, 4, 128], BF16, f"QT{i}") for i in range(2)]
                    lgs = [sb(PA, [128, SEQ], F32, f"lg{i}") for i in range(2)]
                    PT = [sb(PA, [128, 1024], BF16, f"PT{i}") for i in range(2)]
                    oall = sb(PA, [128, 512], F32, "oall")
                    onb = sb(PA, [128, 512], BF16, "onb")
                    mixT = sb(PA, [128, 4, 128], BF16, "mixT")
                    junk2 = sb(PA, [128, 512], F32, "junk2")
                    yt = sb(PA, [128, D], F32, "yt")
                    stat = sb(PA, [128, 32], F32, "stat")
                    if grp == 0:
                        IK = sb(PA, [128, SEQ], BF16, "IK")
                        S.dma('sp', IK[:, 0:L_all], IKT[b, :, 0:L_all], W=bs(IK))
                        IQ = [sb(PA, [128, 3, 128], BF16, f"IQ{i}") for i in range(2)]
                        sg = [sb(PA, [128, 8], F32, f"sg{i}") for i in range(2)]
                        sc = sb(PA, [128, SEQ], F32, "sc")
                        wk = lgs[1]
                        Pm1 = sb(PA, [128, SEQ], BF16, "Pm1")
                        Pms = [(sc[:].bitcast(BF16), sc), (Pm1[:], Pm1)]
                        mb = sb(PA, [128, SEQ], BF16, "mb")
                        rl = [sb(PA, [128, 512], F32, f"rl{i}") for i in range(2)]
                        m8 = sb(PA, [128, 8], F32, "m8")
                        bis = sb(PA, [128, 8], F32, "bis")
                        tau = sb(PA, [128, 1], F32, "tau")
                    else:
                        Pm0 = sb(PA, [128, SEQ], BF16, "Pm0")
                        Pm1 = sb(PA, [128, SEQ], BF16, "Pm1")
                        Pms = [(Pm0[:], Pm0), (Pm1[:], Pm1)]
                        fbts = [sb(PA, [128, SEQ], F32, f"fbt{i}") for i in range(2)]
                        xres = sb(PA, [128, D], F32, "xres")
                        yat = sb(PA, [128, D], F32, "yat")
                        g1bc = sb(PA, [128, D], F32, "g1bc")
                        build_gate_bc(m, 16, g1bc)
                    rotA = [0]
                    rotB = [0]
                    rotT = [0]
                    statb = [Buf() for _ in range(8)]

                    def nextA():
                        rotA[0] = (rotA[0] + 1) % 3
                        return 1 + rotA[0]

                    def nextB():
                        rotB[0] = (rotB[0] + 1) % 4
                        return 4 + rotB[0]

                    for qt in tiles:
                        r0 = b * SEQ + qt * 128
                        L = (qt + 1) * 128
                        nch = (L + 511) // 512
                        Q = QT[qt % 2]
                        S.dma('sp', Q[:], (QAT if grp == 0 else QBT)[b, :, :, qt * 128:(qt + 1) * 128], W=bs(Q))
                        if grp == 0:
                            iq = IQ[qt % 2]
                            sgt = sg[qt % 2]
                            S.dma('sp', iq[:], IQT[b, :, :, qt * 128:(qt + 1) * 128], W=bs(iq))
                            S.dma('sp', sgt[:], SGN[b, :, qt, :], W=bs(sgt))
                            for h in range(8):
                                pr0 = 32 * (h % 3)
                                for c in range(nch):
                                    c0, c1 = c * 512, min(L, (c + 1) * 512)
                                    pb = PS[nextB()]
                                    S.op('pe', mm_(pb[:, 0:c1 - c0], iq[pr0:pr0 + 32, h // 3, :], IK[pr0:pr0 + 32, c0:c1],
                                                   True, True), R=bs(iq, IK), W=bs(pb))
                                    r_ = rl[(h + c) % 2]
                                    S.op('act', act_(r_[:, 0:c1 - c0], pb[:, 0:c1 - c0], AF.Relu), R=bs(pb), W=bs(r_))
                                    if h == 0:
                                        S.op('dve', ts_(sc[:, c0:c1], r_[:, 0:c1 - c0], sgt[:, 0:1], None, ALU.mult),
                                             R=bs(r_, sgt), W=bs(sc))
                                    else:
                                        S.op('dve', stt_(sc[:, c0:c1], r_[:, 0:c1 - c0], sgt[:, h:h + 1], sc[:, c0:c1],
                                                         ALU.mult, ALU.add), R=bs(r_, sgt, sc), W=bs(sc))
                            if qt >= 2:
                                S.op('dve', red_(bis[:, 5:6], sc[:, 0:L], ALU.max), R=bs(sc), W=bs(bis))
                                S.op('dve', red_(bis[:, 0:1], sc[:, 0:L], ALU.min), R=bs(sc), W=bs(bis))
                                S.op('dve', tt_(bis[:, 1:2], bis[:, 5:6], bis[:, 0:1], ALU.subtract), R=bs(bis), W=bs(bis))
                            S.op('dve', tt_(sc[:, L - 128:L], sc[:, L - 128:L], cmaskS[:], ALU.add), R=bs(sc, cmaskS), W=bs(sc))
                            if qt >= 2:
                                NBIS = 21
                                for k in range(NBIS):
                                    stp = 2.0 ** -(k + 1)
                                    S.op('dve', stt_(bis[:, 2:3], bis[:, 1:2], stp, bis[:, 0:1], ALU.mult, ALU.add), R=bs(bis), W=bs(bis))
                                    S.op('dve', ts_(wk[:, 0:L], sc[:, 0:L], bis[:, 2:3], 0.0, ALU.is_ge, ALU.add, accum_out=bis[:, 3:4]),
                                         R=bs(sc, bis), W=bs(wk, bis))
                                    S.op('dve', ts_(bis[:, 4:5], bis[:, 3:4], 255.5, stp, ALU.is_ge, ALU.mult), R=bs(bis), W=bs(bis))
                                    S.op('dve', stt_(bis[:, 0:1], bis[:, 4:5], bis[:, 1:2], bis[:, 0:1], ALU.mult, ALU.add), R=bs(bis), W=bs(bis))
                                S.op('dve', ts_(tau[:], bis[:, 0:1], -1.0e29, None, ALU.max), R=bs(bis), W=bs(tau))
                            else:
                                S.op('dve', memset_(tau[:], -1.0e29), W=bs(tau))
                            S.op('dve', ts_(mb[:, 0:L], sc[:, 0:L], tau[:, 0:1], NEG, ALU.is_lt, ALU.mult), R=bs(sc, tau), W=bs(mb))
                        def head_S1(h):
                            p4, hf = h // 2, (h % 2) * 64
                            lg = lgs[h % 2]
                            if grp == 1:
                                fbt = fbts[h % 2]
                                S.dma('sp', fbt[:, 0:L], NCUM[b, h:h + 1, 0:L].partition_broadcast(128), W=bs(fbt))
                                S.op('dve', tt_(fbt[:, L - 128:L], fbt[:, L - 128:L], cmaskL[:], ALU.add), R=bs(fbt, cmaskL), W=bs(fbt))
                                bias_t = fbt
                            else:
                                bias_t = mb
                            for c in range(nch):
                                c0, c1 = c * 512, min(L, (c + 1) * 512)
                                pb = PS[nextB()]
                                S.op('pe', mm_(pb[:, 0:c1 - c0], Q[hf:hf + 64, p4, :], KT[hf:hf + 64, p4, c0:c1], True, True),
                                     R=bs(Q, KT), W=bs(pb))
                                S.op('dve', tt_(lg[:, c0:c1], pb[:, 0:c1 - c0], bias_t[:, c0:c1], ALU.add),
                                     R=bs(pb, bias_t), W=bs(lg))
                            S.op('dve', red_(stat[:, 16 + h:17 + h], lg[:, 0:L], ALU.max), R=bs(lg), W=[statb[h]])
                            S.op('dve', ts_(stat[:, 24 + h:25 + h], stat[:, 16 + h:17 + h], -0.125, None, ALU.mult), R=[statb[h]], W=[statb[h]])

                        def head_S2(h):
                            lg = lgs[h % 2]
                            Pm_ap, Pm_b = Pms[h % 2]
                            S.op('act', act_(Pm_ap[:, 0:L], lg[:, 0:L], AF.Exp, bias=stat[:, 24 + h:25 + h], scale=0.125,
                                             accum_out=stat[:, h:h + 1]), R=bs(lg) + [statb[h]], W=bs(Pm_b) + [statb[h]])
                            nkb = qt + 1
                            for g0 in range(0, nkb, 8):
                                g1 = min(nkb, g0 + 8)
                                pi = nextA()
                                pbb = psbf(pi)
                                for kb in range(g0, g1):
                                    S.op('pe', tr_(pbb[:, (kb - g0) * 128:(kb - g0 + 1) * 128], Pm_ap[:, kb * 128:(kb + 1) * 128], ident_b[:]),
                                         R=bs(Pm_b, ident_b), W=bs(PS[pi]))
                                rotT[0] += 1
                                ptt = PT[rotT[0] % 2]
                                n = (g1 - g0) * 128
                                S.op('act', act_(ptt[:, 0:n], pbb[:, 0:n], AF.Copy), R=bs(PS[pi]), W=bs(ptt))
                                for kb in range(g0, g1):
                                    S.op('pe', mm_(PS[0][:, h * 64:(h + 1) * 64], ptt[:, (kb - g0) * 128:(kb - g0 + 1) * 128],
                                                   VV[:, kb, h * 64:(h + 1) * 64], kb == 0, kb == nkb - 1),
                                         R=bs(ptt, VV), W=bs(PS[0]))

                        head_S1(0)
                        for h in range(8):
                            if h + 1 < 8:
                                head_S1(h + 1)
                            head_S2(h)
                        S.op('dve', recip_(stat[:, 8:16], stat[:, 0:8]), R=statb, W=bs(stat))
                        S.op('dve', tt_(oall[:].rearrange("p (h d) -> p h d", h=8), PS[0][:].rearrange("p (h d) -> p h d", h=8),
                                        stat[:, 8:16].unsqueeze(2).to_broadcast([128, 8, 64]), ALU.mult), R=bs(PS[0], stat), W=bs(oall))
                        rms_rstd(PA, oall[:], 512, small[:, 2:3], junk2[:], bs(oall), bs(junk2), bs(small))
                        S.op('dve', ts_(onb[:], oall[:], small[:, 2:3], None, ALU.mult), R=bs(oall, small), W=bs(onb))
                        pbb = psbf(1)
                        for k in range(4):
                            S.op('pe', tr_(pbb[:, k * 128:(k + 1) * 128], onb[:, k * 128:(k + 1) * 128], ident_b[:]),
                                 R=bs(onb, ident_b), W=bs(PS[1]))
                        for k in range(4):
                            S.op('dve', ts_(mixT[:, k, :], pbb[:, k * 128:(k + 1) * 128], gv[:, 16 + grp * 4 + k:17 + grp * 4 + k], None, ALU.mult),
                                 R=bs(PS[1], gv), W=bs(mixT))
                        for dh in range(2):
                            for k in range(4):
                                S.op('pe', mm_(PS[2 + dh][:], mixT[:, k, :], woutb[:, k, dh * 512:(dh + 1) * 512], k == 0, k == 3),
                                     R=bs(mixT, woutb), W=bs(PS[2 + dh]))
                        if grp == 0:
                            S.op('act', act_(yt[:, 0:512], PS[2][:], AF.Copy), R=bs(PS[2]), W=bs(yt))
                            S.op('dve', cp_(yt[:, 512:1024], PS[3][:]), R=bs(PS[3]), W=bs(yt))
                            S.dma('sp', YA[r0:r0 + 128, :], yt[:], R=bs(yt))
                        else:
                            S.dma('sp', yat[:], YA[r0:r0 + 128, :], W=bs(yat))
                            S.dma('sp', xres[:], x[r0:r0 + 128, :], W=bs(xres))
                            for dh in range(2):
                                sl = slice(dh * 512, (dh + 1) * 512)
                                S.op('dve', tt_(yt[:, sl], PS[2 + dh][:], yat[:, sl], ALU.add), R=bs(PS[2 + dh], yat), W=bs(yt))
                            S.op('pool', tt_(yt[:], yt[:], g1bc[:], ALU.mult), R=bs(yt, g1bc), W=bs(yt))
                            S.op('pool', tt_(yt[:], yt[:], xres[:], ALU.add), R=bs(yt, xres), W=bs(yt))
                            S.dma('sp', X1[r0:r0 + 128, :], yt[:], R=bs(yt))
                S.barrier()
            if dbg_stage == 3:
                nrow = len(tiles) * 128
                S.dma('sp', y[b * SEQ:b * SEQ + nrow, :], X1[b * SEQ:b * SEQ + nrow, :])
                S.barrier()
            if dbg_stage <= 4:
                continue

            with ExitStack() as PE_:
                TG = 1
                TT = TG * 128
                NI = 4
                EB = NI * 128
                nblk = 16384 // EB
                wpqb = sb(PE_, [128, 8, D], BF16, "wpqb")
                for k in range(8):
                    load_cast(wpqb[:, k, :], wpqb, w_pq[k * 128:(k + 1) * 128, :], D)
                kTb = sb(PE_, [128, 128], BF16, "kTb")
                load_cast(kTb[:], kTb, kT, 128)
                g2bc = sb(PE_, [128, D], F32, "g2bc")
                gfbc = sb(PE_, [128, D], F32, "gfbc")
                build_gate_bc(m, 40, g2bc)
                S.dma('sp', gfbc[:], g_final.partition_broadcast(128), W=bs(gfbc))
                x1t = [sb(PE_, [128, D], F32, f"x1t{i}") for i in range(TG)]
                xh = sb(PE_, [128, D], F32, "xhE")
                junk = xh
                h2T = sb(PE_, [128, 8, TT], BF16, "h2T")
                pqT = sb(PE_, [128, 8, 128], BF16, "pqT")
                s1 = [sb(PE_, [128, 8, 128], F32, f"s1_{i}") for i in range(TG)]
                s2 = [sb(PE_, [128, 8, 128], F32, f"s2_{i}") for i in range(TG)]
                w1 = sb(PE_, [128, 8, 128], F32, "w1")
                v1 = sb(PE_, [128, 8, 16], F32, "v1")
                v2 = sb(PE_, [128, 8, 16], F32, "v2")
                cand = sb(PE_, [128, 8, 256], F32, "cand")
                best = sb(PE_, [128, 8, 16], F32, "best")
                et = sb(PE_, [128, 8, 16], F32, "et")
                zs = [sb(PE_, [128, 32], F32, f"zs{i}") for i in range(TG)]
                Sg = [sb(PE_, [128, NI, 128], F32, f"Sg{i}") for i in range(4)]
                Ee = [sb(PE_, [128, EB], F32, f"Ee{i}") for i in range(2)]
                Gh2 = [[sb(PE_, [128, 8, EB], BF16, f"Gh{i}_{j}") for i in range(TG)] for j in range(2)]
                gAT = [sb(PE_, [128, NI * 128], BF16, f"gAT{i}") for i in range(4)]
                WTt = [sb(PE_, [128, 128], BF16, f"WT{i}") for i in range(4)]
                uch = [sb(PE_, [128, 8, EB], BF16, f"uch{i}") for i in range(3)]
                vch = [sb(PE_, [128, NI, D], BF16, f"vch{i}") for i in range(3)]
                po = sb(PE_, [128, D], F32, "po")
                rot = {'sg': 0, 'ee': 0, 'ga': 0, 'wt': 0, 'pa': 0, 'pg': 0, 'w': 0}
                PAb = [Buf(), Buf()]
                PGb = [Buf() for _ in range(4)]
                for g0 in range(0, len(tiles), TG):
                    grp_tiles = tiles[g0:g0 + TG]
                    for ti, tt in enumerate(grp_tiles):
                        r0 = b * SEQ + tt * 128
                        X = x1t[ti]
                        S.dma('sp', X[:], X1[r0:r0 + 128, :], W=bs(X))
                        rms_rstd(PE_, X[:], D, small[:, 3:4], junk[:], bs(X), bs(junk), bs(small))
                        S.op('dve', ts_(xh[:], X[:], small[:, 3:4], None, ALU.mult), R=bs(X, small), W=bs(xh))
                        for k in range(8):
                            pb = PS[k // 4]
                            S.op('pe', tr_(pb[:, (k % 4) * 128:(k % 4 + 1) * 128], xh[:, k * 128:(k + 1) * 128], ident_f[:]),
                                 R=bs(xh, ident_f), W=bs(pb))
                        for k in range(8):
                            pb = PS[k // 4]
                            src = pb[:, (k % 4) * 128:(k % 4 + 1) * 128]
                            S.op('act', act_(h2T[:, k, ti * 128:(ti + 1) * 128], src, AF.Identity, bias=m[:, 24 + k:25 + k],
                                             scale=m[:, 32 + k:33 + k]), R=bs(pb, m), W=bs(h2T))
                        for h in range(8):
                            pb = PS[2 + h // 4]
                            for k in range(8):
                                S.op('pe', mm_(pb[:, (h % 4) * 128:(h % 4 + 1) * 128], wpqb[:, k, h * 128:(h + 1) * 128],
                                               h2T[:, k, ti * 128:(ti + 1) * 128], k == 0, k == 7), R=bs(wpqb, h2T), W=bs(pb))
                        S.op('act', act_(pqT[:, 0:4, :].rearrange("p a t -> p (a t)"), PS[2][:], AF.Copy), R=bs(PS[2]), W=bs(pqT))
                        S.op('dve', cp_(pqT[:, 4:8, :].rearrange("p a t -> p (a t)"), PS[3][:]), R=bs(PS[3]), W=bs(pqT))
                        for h in range(8):
                            pa, pb2 = PS[4 + h // 4], PS[6 + h // 4]
                            S.op('pe', mm_(pa[:, (h % 4) * 128:(h % 4 + 1) * 128], pqT[0:64, h, :], kTb[0:64, :], True, True),
                                 R=bs(pqT, kTb), W=bs(pa))
                            S.op('pe', mm_(pb2[:, (h % 4) * 128:(h % 4 + 1) * 128], pqT[64:128, h, :], kTb[64:128, :], True, True),
                                 R=bs(pqT, kTb), W=bs(pb2))
                        s1t, s2t, zst = s1[ti], s2[ti], zs[ti]
                        S.op('act', act_(s1t[:, 0:4, :].rearrange("p a t -> p (a t)"), PS[4][:], AF.Copy), R=bs(PS[4]), W=bs(s1t))
                        S.op('dve', cp_(s1t[:, 4:8, :].rearrange("p a t -> p (a t)"), PS[5][:]), R=bs(PS[5]), W=bs(s1t))
                        S.op('act', act_(s2t[:, 0:4, :].rearrange("p a t -> p (a t)"), PS[6][:], AF.Copy), R=bs(PS[6]), W=bs(s2t))
                        S.op('dve', cp_(s2t[:, 4:8, :].rearrange("p a t -> p (a t)"), PS[7][:]), R=bs(PS[7]), W=bs(s2t))
                        for (sx, vx) in ((s1t, v1), (s2t, v2)):
                            for h in range(8):
                                S.op('dve', max_(vx[:, h, 0:8], sx[:, h, :]), R=bs(sx), W=bs(vx))
                                S.op('dve', mr_(w1[:, h, :], vx[:, h, 0:8], sx[:, h, :], NEGS), R=bs(sx, vx), W=bs(w1))
                                S.op('dve', max_(vx[:, h, 8:16], w1[:, h, :]), R=bs(w1), W=bs(vx))
                        for h in range(8):
                            S.op('dve', tt_(cand[:, h, :].rearrange("p (a c) -> p a c", a=16),
                                             v1[:, h, :].unsqueeze(2).to_broadcast([128, 16, 16]),
                                             v2[:, h, :].unsqueeze(1).to_broadcast([128, 16, 16]), ALU.add), R=bs(v1, v2), W=bs(cand))
                        for h in range(8):
                            S.op('dve', max_(best[:, h, 0:8], cand[:, h, :]), R=bs(cand), W=bs(best))
                            S.op('dve', mr_(cand[:, h, :], best[:, h, 0:8], cand[:, h, :], NEGS), R=bs(best, cand), W=bs(cand))
                            S.op('dve', max_(best[:, h, 8:16], cand[:, h, :]), R=bs(cand), W=bs(best))
                        S.op('dve', tt_(et[:], best[:], best[:, :, 0:1].to_broadcast([128, 8, 16]), ALU.subtract), R=bs(best), W=bs(et))
                        S.op('act', act_(et[:], et[:], AF.Exp), R=bs(et), W=bs(et))
                        S.op('dve', red_(zst[:, 0:8], et[:], ALU.add), R=bs(et), W=bs(zst))
                        S.op('act', act_(zst[:, 8:16], zst[:, 0:8], AF.Ln), R=bs(zst), W=bs(zst))
                        S.op('dve', tt_(zst[:, 16:24], zst[:, 8:16], best[:, :, 0], ALU.add), R=bs(zst, best), W=bs(zst))
                        S.op('dve', ts_(zst[:, 16:24], zst[:, 16:24], -1.0, None, ALU.mult), R=bs(zst), W=bs(zst))
                        S.op('dve', cp_(zst[:, 24:32], best[:, :, 15]), R=bs(best), W=bs(zst))
                    ntg = len(grp_tiles)
                    NTT = ntg * 128
                    pend = []

                    def flush_pend(keep):
                        while len(pend) > keep:
                            fn_, R_, W_ = pend.pop(0)
                            S.op('dve', fn_, R=R_, W=W_)

                    def emit_gate_unit(ib, ti, h):
                        Gh = Gh2[ib % 2]
                        u_ = rot['sg']
                        rot['sg'] += 1
                        pX = PS[6 + u_ % 2]
                        in0 = s1[ti][:, h, ib * NI:(ib + 1) * NI].unsqueeze(2).to_broadcast([128, NI, 128])
                        in1 = s2[ti][:, h, :].unsqueeze(1).to_broadcast([128, NI, 128])
                        nb_ap = zs[ti][:, 16 + h:17 + h]
                        tau_ap = zs[ti][:, 24 + h:25 + h]
                        if u_ % 4 != 0:
                            sg_ = Sg[rot['w'] % 4]
                            rot['w'] += 1
                            S.op('pool', tt_(sg_[:], in0, in1, ALU.add), R=bs(s1[ti], s2[ti]), W=bs(sg_))
                            S.op('act', act_(pX[:], sg_[:].rearrange("p a c -> p (a c)"), AF.Exp, bias=nb_ap),
                                 R=bs(sg_, zs[ti]), W=bs(pX))
                            pend.append((stt_(Gh[ti][:, h, :], sg_[:].rearrange("p a c -> p (a c)"), tau_ap, pX[:],
                                              ALU.is_ge, ALU.mult), bs(sg_, zs[ti], pX), bs(Gh[ti])))
                        else:
                            ee_ = Ee[rot['ee'] % 2]
                            rot['ee'] += 1
                            S.op('dve', tt_(pX[:].rearrange("p (a c) -> p a c", a=NI), in0, in1, ALU.add),
                                 R=bs(s1[ti], s2[ti]), W=bs(pX))
                            S.op('act', act_(ee_[:], pX[:], AF.Exp, bias=nb_ap), R=bs(pX, zs[ti]), W=bs(ee_))
                            pend.append((stt_(Gh[ti][:, h, :], pX[:], tau_ap, ee_[:], ALU.is_ge, ALU.mult),
                                         bs(pX, zs[ti], ee_), bs(Gh[ti])))
                        flush_pend(1)

                    units = [(ti, h) for ti in range(ntg) for h in range(8)]
                    st = {}

                    def issue_loads(ib):
                        e0 = ib * EB
                        uc = uch[ib % 3]
                        vcb = vch[ib % 3]
                        S.dma('sp', uc[:], uTb[:, e0:e0 + EB].rearrange("(k p) n -> p k n", p=128), W=bs(uc))
                        S.dma('sp', vcb[:], pvb[e0:e0 + EB, :].rearrange("(a p) n -> p a n", p=128), W=bs(vcb))

                    def stage_A(ibs):
                        for ib in ibs:
                            uc = uch[ib % 3]
                            pAt = PS[2 + ib % 2]
                            for blk in range(NI):
                                bsl = slice(blk * 128, (blk + 1) * 128)
                                for k in range(8):
                                    S.op('pe', mm_(pAt[:, blk * 128:(blk + 1) * 128], uc[:, k, bsl], h2T[:, k, 0:128], k == 0, k == 7),
                                         R=bs(uc, h2T), W=bs(pAt))
                        for ib in ibs:
                            pAt = PS[2 + ib % 2]
                            ga = gAT[ib % 4]
                            S.op('act', act_(ga[:], pAt[:, 0:NI * 128], AF.Gelu), R=bs(pAt), W=bs(ga))

                    def stage_G(ib, blk):
                        Gh = Gh2[ib % 2]
                        bsl = slice(blk * 128, (blk + 1) * 128)
                        ga = gAT[ib % 4]
                        pGt = PS[4 + rot['pg'] % 2]
                        rot['pg'] += 1
                        pG = pGt[:, 0:128]
                        for h in range(8):
                            S.op('pe', mm_(pG, Gh[0][:, h, bsl], ident_b[:], h == 0, h == 7), R=bs(Gh[0], ident_b), W=bs(pGt))
                        wt_ = WTt[rot['wt'] % 4]
                        rot['wt'] += 1
                        S.op('dve', tt_(wt_[:], pG, ga[:, bsl], ALU.mult), R=bs(pGt, ga), W=bs(wt_))
                        st[(ib, blk)] = wt_

                    def stage_O(ib, blk):
                        vcb = vch[ib % 3]
                        first = (ib == 0 and blk == 0)
                        last = (ib == nblk - 1 and blk == NI - 1)
                        wt_ = st.pop((ib, blk))
                        for dh in range(2):
                            S.op('pe', mm_(PS[dh][:], wt_[:], vcb[:, blk, dh * 512:(dh + 1) * 512], first, last),
                                 R=bs(wt_, vcb), W=bs(PS[dh]))

                    assert TG == 1
                    for (ti, h) in units:
                        emit_gate_unit(0, ti, h)
                    issue_loads(0)
                    stage_A([0])
                    upb = (len(units) + NI - 1) // NI
                    prev = None
                    for ib in range(nblk):
                        nxt = [j for j in (ib + 1, ib + 2) if j < nblk] if ib % 2 == 0 else []
                        for blk in range(NI):
                            if blk == 0:
                                flush_pend(0)
                            stage_G(ib, blk)
                            if prev is not None:
                                stage_O(*prev)
                            prev = (ib, blk)
                            if blk == 0:
                                for j in nxt:
                                    issue_loads(j)
                            if ib + 1 < nblk:
                                for (ti, h) in units[blk * upb:(blk + 1) * upb]:
                                    emit_gate_unit(ib + 1, ti, h)
                            if blk == 1 and nxt:
                                stage_A(nxt)
                    flush_pend(0)
                    stage_O(*prev)
                    for ti, tt in enumerate(grp_tiles):
                        r0 = b * SEQ + tt * 128
                        X = x1t[ti]
                        for dh in range(2):
                            sl = slice(dh * 512, (dh + 1) * 512)
                            S.op('dve', tt_(po[:, sl], PS[ti * 2 + dh][:], g2bc[:, sl], ALU.mult), R=bs(PS[ti * 2 + dh], g2bc), W=bs(po))
                        S.op('pool', tt_(po[:], po[:], X[:], ALU.add), R=bs(po, X), W=bs(po))
                        rms_rstd(PE_, po[:], D, small[:, 4:5], junk[:], bs(po), bs(junk), bs(small))
                        S.op('dve', stt_(po[:], po[:], small[:, 4:5], gfbc[:], ALU.mult, ALU.mult), R=bs(po, small, gfbc), W=bs(po))
                        S.dma('sp', y[r0:r0 + 128, :], po[:], R=bs(po))
            S.barrier()
        S.barrier()
        S.emit()
    return nc


def _prep_inputs(inp, core):
    f = np.float32
    bsl = slice(core * NB, (core + 1) * NB)
    c = np.asarray(inp['c'], f)[bsl]
    cT = np.ascontiguousarray(c.reshape(NB, 8, 128).transpose(2, 1, 0)).reshape(128, 16)
    pos = np.asarray(inp['positions'], np.int32)[bsl]
    posT = np.ascontiguousarray(pos.reshape(NB, NT, 128).transpose(0, 2, 1))
    gcat = np.concatenate([np.asarray(inp['g_out_a'], f)[0], np.asarray(inp['g_out_b'], f)[0]])
    gvec = np.concatenate([np.asarray(inp['g_mix'], f)[0].reshape(8, 128).T,
                           np.asarray(inp['g_ffn'], f)[0].reshape(8, 128).T,
                           gcat.reshape(8, 128).T], axis=1)
    kT = np.concatenate([np.asarray(inp['peer_keys1'], f)[0].T, np.asarray(inp['peer_keys2'], f)[0].T], axis=0)
    return {
        "x": np.ascontiguousarray(np.asarray(inp['x'], f)[bsl].reshape(NB * SEQ, D)),
        "cT": np.ascontiguousarray(cT),
        "posT": posT,
        "w_ada": np.ascontiguousarray(np.asarray(inp['w_ada'], f)[0]),
        "b_adaT": np.ascontiguousarray(np.asarray(inp['b_ada'], f)[0].reshape(48, 128).T),
        "gvec": np.ascontiguousarray(gvec),
        "g_final": np.ascontiguousarray(np.asarray(inp['g_final'], f).reshape(1, D)),
        "g_kv": np.ascontiguousarray(np.asarray(inp['g_kv'], f)[0].reshape(1, 128)),
        "b_forget": np.ascontiguousarray(np.asarray(inp['b_forget'], f)[0].reshape(1, 8)),
        "w_in": np.ascontiguousarray(np.asarray(inp['w_in'], f)[0]),
        "w_uk": np.ascontiguousarray(np.asarray(inp['w_uk'], f)[0].reshape(128, 384)),
        "w_uv": np.ascontiguousarray(np.asarray(inp['w_uv'], f)[0].reshape(128, 512)),
        "w_out": np.ascontiguousarray(np.asarray(inp['w_out'], f)[0]),
        "w_pq": np.ascontiguousarray(np.asarray(inp['w_peer_q'], f)[0]),
        "kT": np.ascontiguousarray(kT),
    }


def kernel(**inputs):
    f = np.float32
    nc = build_nc()
    uT = np.ascontiguousarray(np.asarray(inputs['peer_u'], f)[0].T)
    pv = np.ascontiguousarray(np.asarray(inputs['peer_v'], f)[0])
    in_maps = []
    for core in range(8):
        d = _prep_inputs(inputs, core)
        d["uT"] = uT
        d["pv"] = pv
        in_maps.append(d)
    res = run_bass_kernel_spmd(nc, in_maps, core_ids=list(range(8)))
    out = np.concatenate([np.asarray(r["y"], f).reshape(NB, SEQ, D) for r in res.results], axis=0)
    return out
```
